# Optimizing a Trainium2 kernel written in Bass

```python
import jax, jax.numpy as jnp
from jax import lax
import numpy as np

D_MODEL = 1024
BATCH = 8
SEQ = 4096
DEPTH = 4

GRID_W = 64
CTX_LEN = 256
N_Q_HEADS = 8
N_KV_HEADS = 2
HEAD_DIM = 64
D_ATTN = N_Q_HEADS * HEAD_DIM
D_KV = N_KV_HEADS * HEAD_DIM
Q_BLOCK = 128
ROPE_THETA = 10000.0
D_CONF = 512
CONF_WIDTH = 31
D_SHORT = 512
SHORT_WIDTH = 3
D_POOL = 512
POOL_WINDOWS = (2, 4, 8, 16)
POOL_GROUP = D_POOL // len(POOL_WINDOWS)
N_EXPERTS = 16
CAPACITY_FACTOR = 2
D_EXPERT = 2048
EPS = 1e-6
EVEN_IN = 2 * D_CONF + D_ATTN + 2 * D_KV
EVEN_MIX = D_CONF + D_ATTN
ODD_IN = 3 * D_SHORT + D_POOL
ODD_MIX = D_SHORT + D_POOL

kernel_name = 'hybrid_flow_backbone'


def rmsnorm(x, g):
    x32 = x.astype(jnp.float32)
    y = x32 * lax.rsqrt(jnp.mean(x32 * x32, axis=-1, keepdims=True) + EPS)
    return y * g.astype(jnp.float32)


def modulate(x, g, shift, scale):
    y = rmsnorm(x, g) * (1.0 + scale.astype(jnp.float32)) + shift.astype(jnp.float32)
    return y.astype(x.dtype)


def rms_heads(x, g):
    return rmsnorm(x, g).astype(x.dtype)


def rope_tables(n_tok):
    rows = n_tok // GRID_W
    row = jnp.repeat(jnp.arange(rows), GRID_W)
    col = jnp.tile(jnp.arange(GRID_W), rows)
    n_freq = HEAD_DIM // 4
    inv = ROPE_THETA ** (-jnp.arange(n_freq, dtype=jnp.float32) / n_freq)
    ang_r = row.astype(jnp.float32)[:, None] * inv
    ang_c = col.astype(jnp.float32)[:, None] * inv
    return (jnp.cos(ang_r), jnp.sin(ang_r), jnp.cos(ang_c), jnp.sin(ang_c))


def rope_2d(x, cos_r, sin_r, cos_c, sin_c):
    x32 = x.astype(jnp.float32)
    half = HEAD_DIM // 2
    quarter = HEAD_DIM // 4

    def rot(xp, cos, sin):
        x1, x2 = xp[..., :quarter], xp[..., quarter:]
        cs, sn = cos[None, :, None, :], sin[None, :, None, :]
        return jnp.concatenate([x1 * cs - x2 * sn, x2 * cs + x1 * sn], axis=-1)

    out = jnp.concatenate([rot(x32[..., :half], cos_r, sin_r), rot(x32[..., half:], cos_c, sin_c)], axis=-1)
    return out.astype(x.dtype)


def block_attention(q, k, v):
    b, lq, hq, hd = q.shape
    hkv = k.shape[2]
    grp = hq // hkv
    nb = lq // Q_BLOCK
    qb = q.reshape(b, nb, Q_BLOCK, hkv, grp, hd).transpose(1, 0, 2, 3, 4, 5)
    scale = hd ** -0.5

    def one_block(qi):
        s = jnp.einsum('bqhgd,bnhd->bhgqn', qi, k).astype(jnp.float32) * scale
        p = jax.nn.softmax(s, axis=-1).astype(v.dtype)
        return jnp.einsum('bhgqn,bnhd->bqhgd', p, v)

    o = lax.map(one_block, qb)
    return o.transpose(1, 0, 2, 3, 4, 5).reshape(b, lq, hq * hd)


def depthwise_conv(u, w):
    pad = w.shape[0] // 2
    return lax.conv_general_dilated(u, w.astype(u.dtype)[:, None, :], window_strides=(1,),
                                    padding=[(pad, pad)], dimension_numbers=('NWC', 'WIO', 'NWC'),
                                    feature_group_count=u.shape[-1])


def conformer_conv(a, g, conv_w, conv_b, ln_g, ln_b):
    u = a * jax.nn.sigmoid(g)
    u = depthwise_conv(u, conv_w) + conv_b
    u32 = u.astype(jnp.float32)
    mu = jnp.mean(u32, axis=-1, keepdims=True)
    var = jnp.mean(jnp.square(u32 - mu), axis=-1, keepdims=True)
    un = (u32 - mu) * lax.rsqrt(var + EPS) * ln_g.astype(jnp.float32) + ln_b.astype(jnp.float32)
    return jax.nn.silu(un).astype(a.dtype)


def split_heads(t, n_heads):
    return t.reshape(t.shape[0], t.shape[1], n_heads, HEAD_DIM)


def even_mixer(h_lat, h_ctx, w_in, conv_w, conv_b, ln_g, ln_b, q_g, k_g, w_out, rope, with_ctx_out):
    kv0 = 2 * D_CONF + D_ATTN
    p = h_lat @ w_in
    a, g, q, k, v = jnp.split(p, [D_CONF, 2 * D_CONF, kv0, kv0 + D_KV], axis=-1)
    pc = h_ctx @ (w_in if with_ctx_out else w_in[:, kv0:])
    kc = rms_heads(split_heads(pc[..., -2 * D_KV:-D_KV], N_KV_HEADS), k_g)
    vc = split_heads(pc[..., -D_KV:], N_KV_HEADS)
    q = rope_2d(rms_heads(split_heads(q, N_Q_HEADS), q_g), *rope)
    k = rope_2d(rms_heads(split_heads(k, N_KV_HEADS), k_g), *rope)
    v = split_heads(v, N_KV_HEADS)
    attn = block_attention(q, jnp.concatenate([kc, k], axis=1), jnp.concatenate([vc, v], axis=1))
    conv = conformer_conv(a, g, conv_w, conv_b, ln_g, ln_b)
    y_lat = jnp.concatenate([conv, attn], axis=-1) @ w_out
    y_ctx = None
    if with_ctx_out:
        ac, gc, qc = jnp.split(pc[..., :kv0], [D_CONF, 2 * D_CONF], axis=-1)
        qc = rms_heads(split_heads(qc, N_Q_HEADS), q_g)
        attn_c = block_attention(qc, kc, vc)
        conv_c = conformer_conv(ac, gc, conv_w, conv_b, ln_g, ln_b)
        y_ctx = jnp.concatenate([conv_c, attn_c], axis=-1) @ w_out
    return y_lat, y_ctx


def multiscale_pool(u, pool_w, pool_scale):
    b, n, ch = u.shape
    u32 = u.astype(jnp.float32)
    csum = jnp.concatenate([jnp.zeros((b, 1, ch), jnp.float32), jnp.cumsum(u32, axis=1)], axis=1)
    t = np.arange(n)
    outs = []
    for gi, w in enumerate(POOL_WINDOWS):
        lo = np.clip(t - w // 2, 0, n)
        hi = np.clip(t + w // 2, 0, n)
        cnt = jnp.asarray((hi - lo).astype(np.float32))
        sl = slice(gi * POOL_GROUP, (gi + 1) * POOL_GROUP)
        cg = csum[..., sl]
        mean = (cg[:, hi] - cg[:, lo]) / cnt[None, :, None]
        diff = (mean - u32[..., sl]).astype(u.dtype)
        outs.append(diff @ pool_w[gi])
    return jnp.concatenate(outs, axis=-1) * pool_scale


def odd_mixer(h, w_in, conv_w, pool_w, pool_scale, w_out):
    u, gate_b, gate_c, pin = jnp.split(h @ w_in, [D_SHORT, 2 * D_SHORT, 3 * D_SHORT], axis=-1)
    short = gate_b * depthwise_conv(gate_c * u, conv_w)
    pool = multiscale_pool(pin, pool_w, pool_scale)
    return jnp.concatenate([short, pool], axis=-1) @ w_out


def expert_choice_ffn(h, w_router, w_gate, w_up, w_down):
    n_tok, d = h.shape[1], h.shape[2]
    cap = CAPACITY_FACTOR * n_tok // N_EXPERTS
    aff = jax.nn.softmax(jnp.einsum('bnd,de->bne', h, w_router).astype(jnp.float32), axis=-1)
    top_aff, top_idx = lax.top_k(jnp.swapaxes(aff, 1, 2), cap)
    xe = jax.vmap(lambda hb, ib: hb[ib])(h, top_idx)
    g = jnp.einsum('becd,edf->becf', xe, w_gate)
    u = jnp.einsum('becd,edf->becf', xe, w_up)
    ye = jnp.einsum('becf,efd->becd', jax.nn.silu(g) * u, w_down) * top_aff[..., None].astype(h.dtype)

    def combine(ib, yb):
        return jnp.zeros((n_tok, d), yb.dtype).at[ib.reshape(-1)].add(yb.reshape(-1, d))

    return jax.vmap(combine)(top_idx, ye)


def setup_inputs(seed: int = 0) -> dict:
    key = jax.random.key(seed)
    ks = iter(jax.random.split(key, 40))
    n_even = (DEPTH + 1) // 2
    n_odd = DEPTH // 2
    f32 = jnp.float32

    def nrm(shape, s):
        return jax.random.normal(next(ks), shape, f32) * s

    d = D_MODEL
    return {
        'x': nrm((BATCH, SEQ, d), 1.0),
        'c': nrm((BATCH, d), 1.0),
        'ctx': nrm((BATCH, CTX_LEN, d), 1.0),
        'c_ctx': nrm((d,), 1.0),
        'norm_mix_g': 1.0 + nrm((DEPTH, d), 0.05),
        'norm_ffn_g': 1.0 + nrm((DEPTH, d), 0.05),
        'w_mod': nrm((DEPTH, d, 6 * d), 0.5 * d ** -0.5),
        'b_mod': nrm((DEPTH, 6 * d), 0.01),
        'ev_w_in': nrm((n_even, d, EVEN_IN), d ** -0.5),
        'ev_conv_w': nrm((n_even, CONF_WIDTH, D_CONF), CONF_WIDTH ** -0.5),
        'ev_conv_b': nrm((n_even, D_CONF), 0.01),
        'ev_ln_g': 1.0 + nrm((n_even, D_CONF), 0.05),
        'ev_ln_b': nrm((n_even, D_CONF), 0.01),
        'ev_q_norm_g': 1.0 + nrm((n_even, HEAD_DIM), 0.05),
        'ev_k_norm_g': 1.0 + nrm((n_even, HEAD_DIM), 0.05),
        'ev_w_out': nrm((n_even, EVEN_MIX, d), EVEN_MIX ** -0.5),
        'od_w_in': nrm((n_odd, d, ODD_IN), d ** -0.5),
        'od_conv_w': nrm((n_odd, SHORT_WIDTH, D_SHORT), SHORT_WIDTH ** -0.5),
        'od_pool_w': nrm((n_odd, len(POOL_WINDOWS), POOL_GROUP, POOL_GROUP), POOL_GROUP ** -0.5),
        'od_pool_scale': 1.0 + nrm((n_odd, D_POOL), 0.1),
        'od_w_out': nrm((n_odd, ODD_MIX, d), ODD_MIX ** -0.5),
        'w_router': nrm((DEPTH, d, N_EXPERTS), d ** -0.5),
        'w_gate': nrm((DEPTH, N_EXPERTS, d, D_EXPERT), d ** -0.5),
        'w_up': nrm((DEPTH, N_EXPERTS, d, D_EXPERT), d ** -0.5),
        'w_down': nrm((DEPTH, N_EXPERTS, D_EXPERT, d), D_EXPERT ** -0.5),
    }


def reference(x, c, ctx, c_ctx, norm_mix_g, norm_ffn_g, w_mod, b_mod,
              ev_w_in, ev_conv_w, ev_conv_b, ev_ln_g, ev_ln_b, ev_q_norm_g, ev_k_norm_g, ev_w_out,
              od_w_in, od_conv_w, od_pool_w, od_pool_scale, od_w_out,
              w_router, w_gate, w_up, w_down):
    rope = rope_tables(x.shape[1])
    last_even = ((DEPTH - 1) // 2) * 2
    for i in range(DEPTH):
        j = i // 2
        is_even = i % 2 == 0
        ctx_live = i < last_even
        mods = jnp.split(jax.nn.silu(c) @ w_mod[i] + b_mod[i], 6, axis=-1)
        sh1, sc1, g1, sh2, sc2, g2 = [t[:, None, :] for t in mods]
        if is_even or ctx_live:
            sh1c, sc1c, g1c, sh2c, sc2c, g2c = jnp.split(jax.nn.silu(c_ctx) @ w_mod[i] + b_mod[i], 6, axis=-1)
        h = modulate(x, norm_mix_g[i], sh1, sc1)
        if is_even:
            hc = modulate(ctx, norm_mix_g[i], sh1c, sc1c)
            y, yc = even_mixer(h, hc, ev_w_in[j], ev_conv_w[j], ev_conv_b[j], ev_ln_g[j], ev_ln_b[j],
                               ev_q_norm_g[j], ev_k_norm_g[j], ev_w_out[j], rope, ctx_live)
        else:
            y = odd_mixer(h, od_w_in[j], od_conv_w[j], od_pool_w[j], od_pool_scale[j], od_w_out[j])
            yc = None
            if ctx_live:
                hc = modulate(ctx, norm_mix_g[i], sh1c, sc1c)
                yc = odd_mixer(hc, od_w_in[j], od_conv_w[j], od_pool_w[j], od_pool_scale[j], od_w_out[j])
        x = x + g1 * y
        x = x + g2 * expert_choice_ffn(modulate(x, norm_ffn_g[i], sh2, sc2),
                                       w_router[i], w_gate[i], w_up[i], w_down[i])
        if ctx_live:
            ctx = ctx + g1c * yc
            ctx = ctx + g2c * expert_choice_ffn(modulate(ctx, norm_ffn_g[i], sh2c, sc2c),
                                                w_router[i], w_gate[i], w_up[i], w_down[i])
    return x
```

```python
import os
import numpy as np
import concourse.bass as bass
import concourse.mybir as mybir
from concourse.bass_utils import run_bass_kernel_spmd

F32 = mybir.dt.float32
BF16 = mybir.dt.bfloat16
I32 = mybir.dt.int32
U32 = mybir.dt.uint32
ALU = mybir.AluOpType
AF = mybir.ActivationFunctionType

KDBG = set(os.environ.get("KDBG", "").split(","))
PE, ACT, DVE, POOL, SP = "pe", "act", "dve", "pool", "sp"
ENGS = [PE, ACT, DVE, POOL, SP]
STRICT_SAME = {PE: False, ACT: True, DVE: True, POOL: True, SP: False}


class _Stop(Exception):
    pass


class Buf:
    __slots__ = ("name", "w", "r")

    def __init__(self, name):
        self.name = name
        self.w = []
        self.r = []


class Prog:
    def __init__(self, nc):
        self.nc = nc
        self.q = {e: [] for e in ENGS}
        self.sems = {}
        self.cnt = {}
        self.waited = {e: {} for e in ENGS}
        for e in (PE, ACT, DVE, POOL):
            self._mksem("e_" + e)
        nd = {SP: 16, ACT: 4, POOL: 16}
        self.dma_pool = {}
        self.dma_rr = {}
        for e, n in nd.items():
            self.dma_pool[e] = [self._mksem("d_%s%d" % (e, i)) for i in range(n)]
            self.dma_rr[e] = 0
        self.n_ops = 0

    def _mksem(self, key):
        self.sems[key] = self.nc.alloc_semaphore(key)
        self.cnt[key] = 0
        return key

    def buf(self, name):
        return Buf(name)

    def _deps(self, eng, reads, writes, same_ok=False, stream=False):
        need = {}

        def add(t):
            k, v, e = t[0], t[1], t[2]
            if e == eng and (same_ok or not STRICT_SAME[eng] or (stream and len(t) > 3 and t[3])):
                return
            if need.get(k, 0) < v:
                need[k] = v

        for b in reads:
            for t in b.w:
                add(t)
        for b in writes:
            for t in b.w:
                add(t)
            for t in b.r:
                add(t)
        out = []
        wd = self.waited[eng]
        for k, v in need.items():
            if wd.get(k, 0) < v:
                wd[k] = v
                out.append((k, v))
        return out

    def _commit(self, toks, reads, writes):
        for b in reads:
            b.r.extend(toks)
            if len(b.r) > 64:
                mx = {}
                for t in b.r:
                    k, v = t[0], t[1]
                    if k not in mx or mx[k][1] < v:
                        mx[k] = (k, v, t[2])
                b.r = list(mx.values())
        for b in writes:
            b.w = list(toks)
            b.r = []

    def op(self, eng, fn, reads=(), writes=(), same_ok=False, stream=False):
        stream = False
        waits = self._deps(eng, reads, writes, same_ok, stream)
        k = "e_" + eng
        self.cnt[k] += 1
        tok = (k, self.cnt[k], eng, stream)
        sems = self.sems

        def thunk(e, waits=waits, fn=fn, s=sems[k]):
            for (wk, wv) in waits:
                e.wait_ge(sems[wk], wv)
            fn(e).then_inc(s, 1)

        self.q[eng].append(thunk)
        self._commit([tok], reads, writes)
        self.n_ops += 1
        return tok

    def dma(self, eng, fns, reads=(), writes=()):
        if not isinstance(fns, (list, tuple)):
            fns = [fns]
        waits = self._deps(eng, reads, writes)
        wd = self.waited[eng]
        pool = self.dma_pool[eng]
        toks = []
        items = []
        for fn in fns:
            k = pool[self.dma_rr[eng] % len(pool)]
            self.dma_rr[eng] += 1
            prev = self.cnt[k]
            if prev > 0 and wd.get(k, 0) < prev:
                wd[k] = prev
                waits.append((k, prev))
            self.cnt[k] += 16
            toks.append((k, self.cnt[k], "dma"))
            items.append((fn, self.sems[k]))
        sems = self.sems

        def thunk(e, waits=waits, items=items):
            for (wk, wv) in waits:
                e.wait_ge(sems[wk], wv)
            for fn, s in items:
                fn(e).then_inc(s, 16)

        self.q[eng].append(thunk)
        self._commit(toks, reads, writes)
        self.n_ops += 1
        return toks

    def finish(self):
        nc = self.nc
        sems = self.sems
        cnt = dict(self.cnt)

        def fin_thunk(e):
            for k, v in cnt.items():
                if v > 0:
                    e.wait_ge(sems[k], v)

        self.q[SP].append(fin_thunk)
        q = self.q
        with nc.Block() as block:
            @block.tensor
            def _(e):
                for t in q[PE]:
                    t(e)

            @block.scalar
            def _(e):
                for t in q[ACT]:
                    t(e)

            @block.vector
            def _(e):
                for t in q[DVE]:
                    t(e)

            @block.gpsimd
            def _(e):
                for t in q[POOL]:
                    t(e)

            @block.sync
            def _(e):
                for t in q[SP]:
                    t(e)


D = 1024
NL = 4096
NCX = 256
NT = NL + NCX
PAD = 15
L0 = PAD
C0 = PAD + NL + 2 * PAD
CTW = 4416
NCT = 12
NRING = 7
EPS = 1e-6
DEPTH = 4
NEXP = 16
CAP_L = 512
CAP_C = 32
NSLOT = CAP_L + CAP_C
BLKS = [(L0 + 512 * j, 512 * j, 512) for j in range(8)] + [(C0, NL, NCX)]

V_NMG = 0
V_NFG = 32
V_BMOD = 64
V_EV = 256
V_OD = 532
V_CC = 564
NV = 580


def build_program(n_layers=DEPTH, debug=False, stop=None, small_moe=False, start_layer=0, moe_decl=None):
    nc = bass.Bass("TRN2", target_bir_lowering=False)
    P = Prog(nc)

    def din(name, shape, dt=F32):
        return nc.dram_tensor(name, list(shape), dt, kind="ExternalInput")

    xT = din("xT", [D, NL])
    ctxT = din("ctxT", [D, NCX])
    vecs = din("vecs", [128, NV])
    cmats = din("cmats", [128, 5 * 128])
    rope_cs = din("rope_cs", [128, 2 * NL])
    pool_fix = din("pool_fix", [128, 64])
    zeros_d = din("zeros_d", [NT, D])
    w_mod = din("w_mod", [DEPTH, D, 6 * D])
    ev_w_in = din("ev_w_in", [2, D, 1792])
    ev_w_out = din("ev_w_out", [2, D, D])
    od_w_in = din("od_w_in", [2, D, 2048])
    od_w_out = din("od_w_out", [2, D, D])
    od_pool_w = din("od_pool_w", [2, 4, 128, 128])
    w_router = din("w_router", [DEPTH, D, NEXP])
    mshape = [1, 1] if small_moe else [DEPTH if moe_decl is None else len(moe_decl), NEXP]
    mmap = {l: l for l in range(DEPTH)} if moe_decl is None else {l: n for n, l in enumerate(moe_decl)}
    w_gate = din("w_gate", mshape + [D, 2048])
    w_up = din("w_up", mshape + [D, 2048])
    w_down = din("w_down", mshape + [2048, D])
    outT = nc.dram_tensor("outT", [D, NL], F32, kind="ExternalOutput")
    XA = nc.dram_tensor("XA", [D, NT], F32)
    HFTOK = nc.dram_tensor("HFTOK", [NT, D], BF16)
    MOQ = [nc.dram_tensor("MO%d" % q, [NT, 256], F32) for q in range(4)]
    dbg = {}
    if debug:
        dbg["xa"] = nc.dram_tensor("dbg_xa", [D, NT], F32, kind="ExternalOutput")

    XAb = [[P.buf("xa%d_%d" % (c, j)) for j in range(9)] for c in range(8)]
    HFb = [P.buf("hftok%d" % t) for t in range(34)]
    MOb = [P.buf("mo%d" % q) for q in range(4)]
    OUTb = P.buf("out")

    CT = nc.alloc_sbuf_tensor("ct", [128, NCT * CTW], BF16)
    CTF = CT.bitcast(F32)
    CTb = [[[P.buf("ct%d_%d_%d" % (i, j, h)) for h in range(2)] for j in range(9)] for i in range(NCT)]
    RING = nc.alloc_sbuf_tensor("ring", [128, NRING * 4096], BF16)
    RINGb = [P.buf("ring%d" % s) for s in range(NRING)]
    RSTD = nc.alloc_sbuf_tensor("rstd", [128, NT], F32)
    RSTDb = [P.buf("rstd%d" % j) for j in range(9)]
    WT = nc.alloc_sbuf_tensor("wt", [128, 12 * 512], F32)
    WTH = WT.bitcast(BF16)
    WTI = WT.bitcast(I32)
    WTU = WT.bitcast(U32)
    WTb = [P.buf("wt%d" % k) for k in range(12)]
    CM = nc.alloc_sbuf_tensor("cm", [128, 4 * 128], BF16)
    CMb = P.buf("cm")
    IDF = nc.alloc_sbuf_tensor("idf", [128, 128], F32)
    IDFb = P.buf("idf")
    ONEF = nc.alloc_sbuf_tensor("onef", [16, 16], F32)
    ONEFb = P.buf("onef")
    VEC = nc.alloc_sbuf_tensor("vec", [128, NV], F32)
    VECb = P.buf("vec")
    MODS = nc.alloc_sbuf_tensor("mods", [128, DEPTH * 96], F32)
    MODSb = P.buf("mods")
    DER = nc.alloc_sbuf_tensor("der", [128, 96], F32)
    DERb = P.buf("der")
    SCC = nc.alloc_sbuf_tensor("scc", [128, 16], BF16)
    SCCb = P.buf("scc")
    WR = nc.alloc_sbuf_tensor("wr", [128, 8 * NEXP], F32)
    WRb = P.buf("wr")
    AFFT = nc.alloc_sbuf_tensor("afft", [128, 5 * NEXP], F32)
    AFFTb = P.buf("afft")
    IDXT = nc.alloc_sbuf_tensor("idxt", [128, 5 * NEXP], I32)
    IDXTb = P.buf("idxt")
    PFIX = nc.alloc_sbuf_tensor("pfix", [128, 64], F32)
    PFIXb = P.buf("pfix")
    PSD = [nc.alloc_psum_tensor("psd%d" % k, [128, 1024], F32) for k in range(4)]
    PSDH = [p.bitcast(BF16) for p in PSD]
    PS = [PSD[k // 2][:, (k % 2) * 512:(k % 2 + 1) * 512] for k in range(8)]
    PSH = [PSDH[k // 2][:, (k % 2) * 1024:(k % 2 + 1) * 1024] for k in range(8)]
    PSb = [P.buf("ps%d" % k) for k in range(8)]

    def ct(i, a, b):
        return CT[:, i * CTW + a: i * CTW + b]

    def ctp(i, a, b, p0, p1):
        return CT[p0:p1, i * CTW + a: i * CTW + b]

    def cb(i, j):
        return CTb[i][j]

    def wt(k, w=512):
        return WT[:, k * 512: k * 512 + w]

    def wth(k, a=0, b=1024):
        return WTH[:, k * 1024 + a: k * 1024 + b]

    def ring(s, a=0, b=4096):
        return RING[:, s * 4096 + a: s * 4096 + b]

    def ring3(s, c):
        return RING[:, s * 4096:(s + 1) * 4096].rearrange("p (c n) -> p c n", c=c)

    def cm(k):
        return CM[:, k * 128:(k + 1) * 128]

    IDB, ONESB, BLK1, ROT = 0, 1, 2, 3

    def vcol(k, n=1):
        return VEC[:, k:k + n]

    def MM(out, pairs, reads, writes):
        def fn(e, out=out, pairs=pairs):
            n = len(pairs)
            last = None
            for t, (l, r) in enumerate(pairs):
                last = e.matmul(out, lhsT=l, rhs=r, start=(t == 0), stop=(t == n - 1))
            return last
        return P.op(PE, fn, reads, writes)

    def TR(out, in_, ident, reads, writes):
        return P.op(PE, lambda e: e.transpose(out=out, in_=in_, identity=ident), reads, writes)

    def TRS(items, reads, writes):
        def fn(e, items=items):
            last = None
            for (o, i_, idn) in items:
                last = e.transpose(out=o, in_=i_, identity=idn)
            return last
        return P.op(PE, fn, reads, writes)

    def big(ap):
        sh = ap.shape
        return len(sh) == 2 and sh[1] >= 256

    def A(out, in_, func, reads, writes, scale=1.0, bias=0.0, accum=None):
        if accum is None:
            return P.op(ACT, lambda e: e.activation(out=out, in_=in_, func=func, bias=bias, scale=scale), reads, writes,
                        stream=big(out) and big(in_))
        return P.op(ACT, lambda e: e.activation(out=out, in_=in_, func=func, bias=bias, scale=scale, accum_out=accum), reads, writes)

    def TT(out, in0, in1, op, reads, writes, eng=DVE):
        return P.op(eng, lambda e: e.tensor_tensor(out=out, in0=in0, in1=in1, op=op), reads, writes,
                    stream=(eng == DVE) and big(out) and big(in0) and big(in1))

    def TS(out, in0, s1, s2, op0, op1, reads, writes, eng=DVE):
        if s2 is None:
            return P.op(eng, lambda e: e.tensor_scalar(out, in0, s1, None, op0=op0), reads, writes, stream=(eng == DVE) and big(out) and big(in0))
        return P.op(eng, lambda e: e.tensor_scalar(out, in0, s1, s2, op0=op0, op1=op1), reads, writes, stream=(eng == DVE) and big(out) and big(in0))

    def STT(out, in0, scalar, in1, op0, op1, reads, writes, eng=DVE):
        return P.op(eng, lambda e: e.scalar_tensor_tensor(out=out, in0=in0, scalar=scalar, in1=in1, op0=op0, op1=op1), reads, writes,
                    stream=(eng == DVE) and big(out) and big(in0) and big(in1))

    def CP(out, in_, reads, writes, eng=DVE):
        return P.op(eng, lambda e: e.tensor_copy(out, in_), reads, writes, stream=(eng == DVE) and big(out) and big(in_))

    def RCP(out, in_, reads, writes):
        return P.op(DVE, lambda e: e.reciprocal(out=out, in_=in_), reads, writes, stream=big(out) and big(in_))

    def MSET(ap, val, writes, eng=POOL):
        return P.op(eng, lambda e: e.memset(ap, val), (), writes)

    def LD(eng, out, in_, reads, writes):
        return P.dma(eng, lambda e: e.dma_start(out=out, in_=in_), reads, writes)

    LD(POOL, CM[:, :], cmats[:, 0:512], [], [CMb])
    LD(SP, IDF[:, :], cmats[:, 0:128], [], [IDFb])
    LD(SP, ONEF[:, :], cmats[0:16, 128:144], [], [ONEFb])
    LD(SP, VEC[:, :], vecs[:, :], [], [VECb])
    LD(SP, PFIX[:, :], pool_fix[:, :], [], [PFIXb])
    def zero_mo(reads):
        zsrc = zeros_d[0:NT // 4, :].rearrange("(a b) d -> a (b d)", a=16)
        for q in range(4):
            LD(SP, MOQ[q][:, :].rearrange("(a b) d -> a (b d)", a=16), zsrc, [MOb[q]] if reads else [], [MOb[q]])
    zero_mo(False)
    for i in range(NCT):
        MSET(ct(i, 0, CTW), 0.0, [b for j in range(9) for b in cb(i, j)])
    for k in range(12):
        MSET(wt(k), 0.0, [WTb[k]])

    A(WT[:, 0:16], VEC[:, V_CC:V_CC + 16], AF.Silu, [VECb, WTb[0]], [WTb[0]])
    CP(SCC[:, :], WT[:, 0:16], [WTb[0]], [SCCb])
    scc3 = SCC[:, :].rearrange("p (c v) -> p c v", v=2)
    pcount = [0]
    for i in range(n_layers):
        for n in range(12):
            s = pcount[0] % NRING
            pcount[0] += 1
            LD(POOL, ring3(s, 8), w_mod[i].rearrange("(c p) n -> p c n", p=128)[:, :, n * 512:(n + 1) * 512], [], [RINGb[s]])
            r3 = ring3(s, 8)
            for oc in range(4):
                k = n * 4 + oc
                MM(PS[0][:, k * 2:k * 2 + 2], [(r3[:, c, oc * 128:(oc + 1) * 128], scc3[:, c, :]) for c in range(8)],
                   [RINGb[s], SCCb], [PSb[0]])
        m3 = MODS[:, i * 96:(i + 1) * 96].rearrange("p (k v) -> p k v", v=2)
        p3 = PS[0][:, 0:96].rearrange("p (k v) -> p k v", v=2)
        for v in range(2):
            TT(m3[:, :, v], p3[:, :, v], VEC[:, V_BMOD + i * 48: V_BMOD + (i + 1) * 48], ALU.add, [PSb[0], VECb], [MODSb])

    def mod(i, grp, c, v):
        k = grp * 8 + c
        return MODS[:, i * 96 + k * 2 + v: i * 96 + k * 2 + v + 1]

    def der_setup(i):
        d3 = DER[:, 0:32].rearrange("p (g v c) -> p g v c", g=2, v=2)
        m4 = MODS[:, i * 96:(i + 1) * 96].rearrange("p (g c v) -> p g c v", g=6, v=2)
        for v in range(2):
            STT(d3[:, 0, v, :], m4[:, 1, :, v], 1.0, VEC[:, V_NMG + i * 8: V_NMG + i * 8 + 8], ALU.add, ALU.mult, [MODSb, VECb], [DERb])
            STT(d3[:, 1, v, :], m4[:, 4, :, v], 1.0, VEC[:, V_NFG + i * 8: V_NFG + i * 8 + 8], ALU.add, ALU.mult, [MODSb, VECb], [DERb])

    def G1(c, v):
        return DER[:, v * 8 + c: v * 8 + c + 1]

    def G2(c, v):
        return DER[:, 16 + v * 8 + c: 16 + v * 8 + c + 1]

    SS = 7

    sqrr = [0]

    def stat_chunk(j, c, xs_ap, xs_buf, w):
        k = 3 + (sqrr[0] % 2)
        sqrr[0] += 1
        A(wth(k, 0, w), xs_ap, AF.Square, [xs_buf], [WTb[k]])
        return k

    def stat_mm(j, c, k, w):
        P.op(PE, lambda e: e.matmul(PS[SS][:, 0:w], lhsT=cm(ONESB), rhs=wth(k, 0, w), start=(c == 0), stop=(c == 7)),
             [CMb, WTb[k]], [PSb[SS]])

    def stat_fin(j, w):
        xcol = BLKS[j][1]
        A(wt(5, w), PS[SS][:, 0:w], AF.Sqrt, [PSb[SS]], [WTb[5]], scale=1.0 / D, bias=EPS)
        RCP(RSTD[:, xcol:xcol + w], wt(5, w), [WTb[5]], [RSTDb[j]])

    def blocks_for(use_ctx):
        return list(range(9)) if use_ctx else list(range(8))

    xsrr = [0]

    def next_xs():
        k = xsrr[0] % 3
        xsrr[0] += 1
        return k

    for j in range(9):
        cs, xc, w = BLKS[j]
        pend = None
        for c in range(8):
            k = next_xs()
            src = xT[c * 128:(c + 1) * 128, xc:xc + w] if j < 8 else ctxT[c * 128:(c + 1) * 128, 0:w]
            LD(SP, wt(k, w), src, [], [WTb[k]])
            LD(SP, XA[c * 128:(c + 1) * 128, xc:xc + w], wt(k, w), [WTb[k]], [XAb[c][j]])
            kk = stat_chunk(j, c, wt(k, w), WTb[k], w)
            if pend is not None:
                stat_mm(j, *pend, w)
            pend = (c, kk)
        stat_mm(j, *pend, w)
        stat_fin(j, w)

    def chk(name):
        if stop == name:
            raise _Stop()

    HB = [WTH[:, 8 * 1024: 12 * 1024], RING[:, 6 * 4096: 7 * 4096]]
    HBb = [[WTb[8], WTb[9], WTb[10], WTb[11]], [RINGb[6]]]
    hbrr = [0]

    def build_h(i, j, which):
        cs, xc, w = BLKS[j]
        v = 1 if j == 8 else 0
        hb_i = hbrr[0] % 2 if "hb0" not in KDBG else 0
        hbrr[0] += 1
        hb3 = HB[hb_i].rearrange("p (c n) -> p c n", c=8)
        for c in range(8):
            k = next_xs()
            LD(SP, wt(k, w), XA[c * 128:(c + 1) * 128, xc:xc + w], [XAb[c][j]], [WTb[k]])
            TT(wt(k, w), wt(k, w), RSTD[:, xc:xc + w], ALU.mult, [WTb[k], RSTDb[j]], [WTb[k]])
            if which == 1:
                A(hb3[:, c, 0:w], wt(k, w), AF.Identity, [WTb[k], DERb, MODSb] + HBb[hb_i], HBb[hb_i],
                  scale=G1(c, v), bias=mod(i, 0, c, v))
            else:
                A(hb3[:, c, 0:w], wt(k, w), AF.Identity, [WTb[k], DERb, MODSb] + HBb[hb_i], HBb[hb_i],
                  scale=G2(c, v), bias=mod(i, 3, c, v))
        return hb3, HBb[hb_i]

    def load_w_slot(slot, src3, ncols=512, dst_off=0):
        LD(POOL, ring3(slot, 8)[:, :, dst_off:dst_off + ncols], src3, [], [RINGb[slot]])

    def w3(wh):
        return wh.rearrange("(c p) n -> p c n", p=128)

    def outproj_residual(i, mix_tiles, wslots, use_ctx, gate_grp):
        for j in blocks_for(use_ctx):
            cs, xc, w = BLKS[j]
            v = 1 if j == 8 else 0
            pend = None
            for c in range(8):
                slot = wslots[c // 4]
                r3 = ring3(slot, 8)
                pb = c % 2
                MM(PS[pb][:, 0:w], [(r3[:, k, (c % 4) * 128:(c % 4 + 1) * 128], ct(mix_tiles[k], cs, cs + w)) for k in range(8)],
                   [RINGb[slot]] + [b for k in range(8) for b in cb(mix_tiles[k], j)], [PSb[pb]])
                kx = next_xs()
                LD(SP, wt(kx, w), XA[c * 128:(c + 1) * 128, xc:xc + w], [XAb[c][j]], [WTb[kx]])
                STT(wt(kx, w), PS[pb][:, 0:w], mod(i, gate_grp, c, v), wt(kx, w), ALU.mult, ALU.add,
                    [PSb[pb], WTb[kx], MODSb], [WTb[kx]])
                LD(SP, XA[c * 128:(c + 1) * 128, xc:xc + w], wt(kx, w), [WTb[kx]], [XAb[c][j]])
                kk = stat_chunk(j, c, wt(kx, w), WTb[kx], w)
                if pend is not None:
                    stat_mm(j, *pend, w)
                pend = (c, kk)
            stat_mm(j, *pend, w)
            stat_fin(j, w)

    U_T = [0, 1, 2, 3]
    CO_T = [4, 5, 6, 7]
    Q_T = [0, 1, 2, 3]
    K_T = 8
    V_T = 9
    MISC_T = 11

    def vtok(kt, a, b):
        base = V_T * CTW + kt * 256
        return CT[:, base + a: base + b]

    VTb = [P.buf("vt%d" % kt) for kt in range(34)]

    COS_AP = CTF[:, (MISC_T * CTW) // 2: (MISC_T * CTW) // 2 + 512]
    SIN_AP = CTF[:, (MISC_T * CTW) // 2 + 512: (MISC_T * CTW) // 2 + 1024]

    def zero_pads(tiles):
        for t in tiles:
            MSET(ct(t, 0, PAD), 0.0, cb(t, 0))
            MSET(ct(t, L0 + NL, C0), 0.0, cb(t, 7) + cb(t, 8))
            MSET(ct(t, C0 + NCX, CTW), 0.0, cb(t, 8))

    def even_mixer(i):
        jj = i // 2
        ctx_full = i < 2
        ve = V_EV + jj * 138
        zero_pads(U_T)
        wi = w3(ev_w_in[jj])
        load_w_slot(0, wi[:, :, 0:512])
        load_w_slot(1, wi[:, :, 512:1024])
        for cq in range(4):
            for half in range(2):
                hq = half * 4 + cq
                load_w_slot(2, wi[:, :, 1024 + hq * 64: 1024 + (hq + 1) * 64], 64, cq * 128 + half * 64)
        load_w_slot(3, wi[:, :, 1536:1792], 256, 0)
        for j in blocks_for(ctx_full):
            cs, xc, w = BLKS[j]
            hb3, hbb = build_h(i, j, 1)
            for c in range(4):
                MM(PS[0][:, 0:w], [(ring3(0, 8)[:, k, c * 128:(c + 1) * 128], hb3[:, k, 0:w]) for k in range(8)],
                   [RINGb[0]] + hbb, [PSb[0]])
                MM(PS[1][:, 0:w], [(ring3(1, 8)[:, k, c * 128:(c + 1) * 128], hb3[:, k, 0:w]) for k in range(8)],
                   [RINGb[1]] + hbb, [PSb[1]])
                A(wt(5, w), PS[1][:, 0:w], AF.Sigmoid, [PSb[1]], [WTb[5]])
                TT(ct(U_T[c], cs, cs + w), PS[0][:, 0:w], wt(5, w), ALU.mult, [PSb[0], WTb[5]], cb(U_T[c], j))
        chk("passA")
        DS = [4, 5, 6, 0]
        for c in range(4 if "nodiag" not in KDBG else 0):
            for t in range(31):
                TS(ring(DS[c], t * 128, (t + 1) * 128), cm(IDB), VEC[:, ve + 14 + c * 31 + t: ve + 14 + c * 31 + t + 1], None,
                   ALU.mult, None, [CMb, VECb], [RINGb[DS[c]]], eng=POOL if (t % 2) else DVE)
        wo = w3(ev_w_out[jj])
        def load_wout(slot, col0):
            LD(POOL, ring3(slot, 8)[:, 0:4, :], wo[:, 0:4, col0:col0 + 512], [], [RINGb[slot]])
            woh = ev_w_out[jj][512:1024, col0:col0 + 512].rearrange("(h p) n -> p h n", p=64)
            for half in range(2):
                LD(POOL, RING[half * 64:(half + 1) * 64, slot * 4096:(slot + 1) * 4096].rearrange("p (c n) -> p c n", c=8)[:, 4:8, :],
                   woh[:, half * 4: half * 4 + 4, :], [], [RINGb[slot]])
        load_wout(1, 0)
        for j in (blocks_for(ctx_full) if "noconv" not in KDBG else []):
            cs, xc, w = BLKS[j]
            for c in range(4):
                MM(PS[c % 2][:, 0:w], [(ring(DS[c], t * 128, (t + 1) * 128), ct(U_T[c], cs + t - 15, cs + t - 15 + w)) for t in range(31)],
                   [RINGb[DS[c]]] + [b for jn in ((8,) if j == 8 else (max(j - 1, 0), j, min(j + 1, 7))) for b in cb(U_T[c], jn)], [PSb[c % 2]])
                A(wt(5 + c, w), PS[c % 2][:, 0:w], AF.Identity, [PSb[c % 2], VECb], [WTb[5 + c]], bias=VEC[:, ve + c: ve + c + 1])
                A(wth(3, 0, w), wt(5 + c, w), AF.Square, [WTb[5 + c]], [WTb[3]])
                CP(wth(4, 0, w), wt(5 + c, w), [WTb[5 + c]], [WTb[4]])
                P.op(PE, lambda e, c=c, w=w: e.matmul(PS[2][:, 0:w], lhsT=cm(ONESB), rhs=wth(4, 0, w), start=(c == 0), stop=(c == 3)),
                     [CMb, WTb[4]], [PSb[2]])
                P.op(PE, lambda e, c=c, w=w: e.matmul(PS[3][:, 0:w], lhsT=cm(ONESB), rhs=wth(3, 0, w), start=(c == 0), stop=(c == 3)),
                     [CMb, WTb[3]], [PSb[3]])
            TS(wt(9, w), PS[2][:, 0:w], 1.0 / 512, None, ALU.mult, None, [PSb[2]], [WTb[9]])
            TT(wt(10, w), wt(9, w), wt(9, w), ALU.mult, [WTb[9]], [WTb[10]])
            STT(wt(10, w), PS[3][:, 0:w], 1.0 / 512, wt(10, w), ALU.mult, ALU.subtract, [PSb[3], WTb[10]], [WTb[10]])
            A(wt(10, w), wt(10, w), AF.Sqrt, [WTb[10]], [WTb[10]], bias=EPS)
            RCP(wt(10, w), wt(10, w), [WTb[10]], [WTb[10]])
            for c in range(4):
                TT(wt(5 + c, w), wt(5 + c, w), wt(9, w), ALU.subtract, [WTb[5 + c], WTb[9]], [WTb[5 + c]])
                TT(wt(5 + c, w), wt(5 + c, w), wt(10, w), ALU.mult, [WTb[5 + c], WTb[10]], [WTb[5 + c]])
                A(ct(CO_T[c], cs, cs + w), wt(5 + c, w), AF.Silu, [WTb[5 + c], VECb], cb(CO_T[c], j),
                  scale=VEC[:, ve + 4 + c: ve + 5 + c], bias=VEC[:, ve + 8 + c: ve + 9 + c])
        chk("conv")
        TS(DER[:, 40:41], VEC[:, ve + 12: ve + 13], 0.125, None, ALU.mult, None, [VECb], [DERb])
        CP(DER[:, 41:42], VEC[:, ve + 13: ve + 14], [VECb], [DERb])
        for kt in range(34 if "noones" not in KDBG else 0):
            MSET(CT[:, V_T * CTW + kt * 256: V_T * CTW + (kt + 1) * 256].rearrange("p (h n) -> p h n", h=2)[:, :, 64:128], 1.0, [VTb[kt]], eng=DVE)
        for j in range(int(os.environ.get("PBN", "9"))):
            cs, xc, w = BLKS[j]
            is_ctx = (j == 8)
            hb3, hbb = build_h(i, j, 1)
            if "nochunks" in KDBG:
                continue
            if not is_ctx and "noropeld" not in KDBG:
                LD(SP, COS_AP, rope_cs[:, xc:xc + w], [], cb(MISC_T, 0))
                LD(SP, SIN_AP, rope_cs[:, NL + xc: NL + xc + w], [], cb(MISC_T, 1))
            chunks = []
            if (not is_ctx) or ctx_full:
                chunks += [("q", cq) for cq in range(4)]
            chunks += [("k", 0)]
            for kind, cq in chunks:
                if kind == "q":
                    lw = [(ring3(2, 8)[:, k, cq * 128:(cq + 1) * 128], hb3[:, k, 0:w]) for k in range(8)]
                    dst, dstb, gcol = ct(Q_T[cq], cs, cs + w), cb(Q_T[cq], j), 40
                    rb = [RINGb[2]]
                else:
                    lw = [(ring3(3, 8)[:, k, 0:128], hb3[:, k, 0:w]) for k in range(8)]
                    dst, dstb, gcol = ct(K_T, cs, cs + w), cb(K_T, j), 41
                    rb = [RINGb[3]]
                MM(PS[0][:, 0:w], lw, rb + hbb, [PSb[0]])
                CP(wt(5, w), PS[0][:, 0:w], [PSb[0]], [WTb[5]])
                A(wth(3, 0, w), wt(5, w), AF.Square, [WTb[5]], [WTb[3]])
                MM(PS[1][:, 0:w], [(cm(BLK1), wth(3, 0, w))], [CMb, WTb[3]], [PSb[1]])
                A(wt(6, w), PS[1][:, 0:w], AF.Sqrt, [PSb[1]], [WTb[6]], scale=1.0 / 64, bias=EPS)
                RCP(wt(6, w), wt(6, w), [WTb[6]], [WTb[6]])
                if "noqk" in KDBG:
                    continue
                if is_ctx or "norope" in KDBG:
                    STT(dst, wt(5, w), DER[:, gcol:gcol + 1], wt(6, w), ALU.mult, ALU.mult, [WTb[5], WTb[6], DERb], dstb)
                else:
                    STT(wt(5, w), wt(5, w), DER[:, gcol:gcol + 1], wt(6, w), ALU.mult, ALU.mult, [WTb[5], WTb[6], DERb], [WTb[5]])
                    A(wth(4, 0, w), wt(5, w), AF.Copy, [WTb[5]], [WTb[4]])
                    MM(PS[2][:, 0:w], [(cm(ROT), wth(4, 0, w))], [CMb, WTb[4]], [PSb[2]])
                    TT(wt(5, w), wt(5, w), COS_AP, ALU.mult, [WTb[5]] + cb(MISC_T, 0), [WTb[5]])
                    TT(wt(6, w), PS[2][:, 0:w], SIN_AP, ALU.mult, [PSb[2]] + cb(MISC_T, 1), [WTb[6]])
                    TT(dst, wt(5, w), wt(6, w), ALU.add, [WTb[5], WTb[6]], dstb)
            for tt in range(w // 128 if "nov" not in KDBG else 0):
                kt = (32 + tt) if is_ctx else (j * 4 + tt)
                pb = 4 + (tt % 2)
                MM(PS[pb][:, 0:128], [(hb3[:, k, tt * 128:(tt + 1) * 128], ring3(3, 8)[:, k, 128:256]) for k in range(8)],
                   [RINGb[3]] + hbb, [PSb[pb]])
                CP(vtok(kt, 0, 256).rearrange("p (h n) -> p h n", h=2)[:, :, 0:64],
                   PS[pb][:, 0:128].rearrange("p (h n) -> p h n", h=2), [PSb[pb]], [VTb[kt]])
        chk("passB")
        load_wout(2, 512)
        items = []
        for j in blocks_for(ctx_full):
            is_ctx = (j == 8)
            kts = [32, 33] if is_ctx else [32, 33] + list(range(32))
            prs = [(kts[n], kts[n + 1]) for n in range(0, len(kts), 2)]
            for cq in range(4):
                for half in range(2):
                    for n, pr in enumerate(prs):
                        items.append((j, cq, half, pr, n == 0, n == len(prs) - 1))
        LA = 3
        nit = len(items)
        for step in range(nit + LA):
            if step < nit:
                j, cq, half, pr, first, last = items[step]
                cs, xc, w = BLKS[j]
                sd = step % 3
                sbufs = [PSb[2 * sd], PSb[2 * sd + 1]]
                pairs_mm = []
                rd = [CTb[Q_T[cq]][j][half]]
                for n, kt in enumerate(pr):
                    kcol = (C0 + (kt - 32) * 128) if kt >= 32 else (L0 + kt * 128)
                    kj = 8 if kt >= 32 else kt // 4
                    pairs_mm.append((PSD[sd][:, n * w:(n + 1) * w], ctp(K_T, kcol, kcol + 128, half * 64, half * 64 + 64)))
                    rd.append(CTb[K_T][kj][half])
                qap = ctp(Q_T[cq], cs, cs + w, half * 64, half * 64 + 64)

                def smm(e, pairs_mm=pairs_mm, qap=qap):
                    last_i = None
                    for (o, kap) in pairs_mm:
                        last_i = e.matmul(o, lhsT=kap, rhs=qap, start=True, stop=True)
                    return last_i
                P.op(PE, smm, rd, sbufs)
                pk = step % 4
                A(wth(pk, 0, 2 * w), PSD[sd][:, 0:2 * w], AF.Exp, sbufs, [WTb[pk]])
            if step >= LA:
                j, cq, half, pr, first, last = items[step - LA]
                cs, xc, w = BLKS[j]
                pk = (step - LA) % 4
                ob = 6 + half

                def pvmm(e, ob=ob, w=w, pr=pr, half=half, pk=pk, first=first, last=last):
                    last_i = None
                    for n, kt in enumerate(pr):
                        last_i = e.matmul(PS[ob][:, 0:w], lhsT=vtok(kt, half * 128, half * 128 + 128), rhs=wth(pk, n * w, (n + 1) * w),
                                          start=(first and n == 0), stop=(last and n == len(pr) - 1))
                    return last_i
                P.op(PE, pvmm, [VTb[kt] for kt in pr] + [WTb[pk]], [PSb[ob]])
                if last:
                    RCP(WT[64:128, 5 * 512: 5 * 512 + w], PS[ob][64:128, 0:w], [PSb[ob]], [WTb[5]])
                    TT(ctp(Q_T[cq], cs, cs + w, half * 64, half * 64 + 64), PS[ob][0:64, 0:w], WT[64:128, 5 * 512: 5 * 512 + w], ALU.mult,
                       [PSb[ob], WTb[5]], [CTb[Q_T[cq]][j][half]])
        chk("attn")
        outproj_residual(i, CO_T + Q_T, [1, 2], ctx_full, 2)


    def odd_mixer(i):
        jj = i // 2
        ctx_full = i < 2
        vo = V_OD + jj * 16
        wi = w3(od_w_in[jj])
        PIN_T = [0, 1, 2, 3]
        PO_T = [4, 5, 6, 7]
        SC_T = [0, 1, 2, 3]
        GB_T = [8, 9, 10, 11]
        zero_pads(PIN_T)
        load_w_slot(3, wi[:, :, 1536:2048])
        load_w_slot(0, wi[:, :, 0:512])
        load_w_slot(1, wi[:, :, 512:1024])
        load_w_slot(2, wi[:, :, 1024:1536])
        PWT = ring(4, 20 * 128, 24 * 128)
        PWTb = RINGb[4]
        LD(POOL, PWT.rearrange("p (g n) -> p g n", g=4), od_pool_w[jj].rearrange("g p n -> p g n"), [], [PWTb])
        WIN = [2, 4, 8, 16]
        tapmat = {}
        tcount = 0
        for gi, wv in enumerate(WIN):
            for val in (1.0 / wv, 1.0 / wv - 1.0):
                TS(ring(4, tcount * 128, (tcount + 1) * 128), cm(IDB), float(val), None, ALU.mult, None, [CMb], [RINGb[4]])
                tapmat[(gi, val == 1.0 / wv)] = tcount
                tcount += 1
        for c in range(4):
            for t in range(3):
                TS(ring(4, (8 + c * 3 + t) * 128, (9 + c * 3 + t) * 128), cm(IDB), VEC[:, vo + c * 3 + t: vo + c * 3 + t + 1], None,
                   ALU.mult, None, [CMb, VECb], [RINGb[4]])
        for j in blocks_for(ctx_full):
            cs, xc, w = BLKS[j]
            hb3, hbb = build_h(i, j, 1)
            for c in range(4):
                MM(PS[c % 2][:, 0:w], [(ring3(3, 8)[:, k, c * 128:(c + 1) * 128], hb3[:, k, 0:w]) for k in range(8)],
                   [RINGb[3]] + hbb, [PSb[c % 2]])
                A(ct(PIN_T[c], cs, cs + w), PS[c % 2][:, 0:w], AF.Copy, [PSb[c % 2]], cb(PIN_T[c], j))
        wo = w3(od_w_out[jj])
        load_w_slot(5, wo[:, :, 0:512])
        load_w_slot(3, wo[:, :, 512:1024])
        for j in blocks_for(ctx_full):
            cs, xc, w = BLKS[j]
            seq0, seqn = (C0, NCX) if j == 8 else (L0, NL)
            for gi, wv in enumerate(WIN):
                taps = list(range(-wv // 2, wv // 2))
                nb = [b for jn in ((8,) if j == 8 else (max(j - 1, 0), j, min(j + 1, 7))) for b in cb(PIN_T[gi], jn)]
                MM(PS[gi % 2][:, 0:w], [(ring(4, tapmat[(gi, t != 0)] * 128, (tapmat[(gi, t != 0)] + 1) * 128),
                                         ct(PIN_T[gi], cs + t, cs + t + w)) for t in taps], [RINGb[4]] + nb, [PSb[gi % 2]])
                dk = 5 + (gi % 2)
                CP(wth(dk, 0, w), PS[gi % 2][:, 0:w], [PSb[gi % 2]], [WTb[dk]])
                h2 = wv // 2
                fix = []
                if cs == seq0:
                    fix.append((0, h2, gi * 16))
                if cs + w == seq0 + seqn:
                    fix.append((w - (h2 - 1), h2 - 1, gi * 16 + 8))
                for (o, n, fc) in fix:
                    if n <= 0:
                        continue
                    TT(wt(7, 16)[:, 0:n], wth(dk, o, o + n), ct(PIN_T[gi], cs + o, cs + o + n), ALU.add, [WTb[dk]] + cb(PIN_T[gi], j), [WTb[7]])
                    TT(wt(7, 16)[:, 0:n], wt(7, 16)[:, 0:n], PFIX[:, fc:fc + n], ALU.mult, [WTb[7], PFIXb], [WTb[7]])
                    TT(wth(dk, o, o + n), wt(7, 16)[:, 0:n], ct(PIN_T[gi], cs + o, cs + o + n), ALU.subtract, [WTb[7]] + cb(PIN_T[gi], j), [WTb[dk]])
                MM(PS[2 + gi % 2][:, 0:w], [(PWT[:, gi * 128:(gi + 1) * 128], wth(dk, 0, w))], [PWTb, WTb[dk]], [PSb[2 + gi % 2]])
                A(ct(PO_T[gi], cs, cs + w), PS[2 + gi % 2][:, 0:w], AF.Identity, [PSb[2 + gi % 2], VECb], cb(PO_T[gi], j),
                  scale=VEC[:, vo + 12 + gi: vo + 13 + gi])
        for j in blocks_for(ctx_full):
            cs, xc, w = BLKS[j]
            hb3, hbb = build_h(i, j, 1)
            for c in range(4):
                MM(PS[0][:, 0:w], [(ring3(0, 8)[:, k, c * 128:(c + 1) * 128], hb3[:, k, 0:w]) for k in range(8)], [RINGb[0]] + hbb, [PSb[0]])
                MM(PS[1][:, 0:w], [(ring3(2, 8)[:, k, c * 128:(c + 1) * 128], hb3[:, k, 0:w]) for k in range(8)], [RINGb[2]] + hbb, [PSb[1]])
                MM(PS[2][:, 0:w], [(ring3(1, 8)[:, k, c * 128:(c + 1) * 128], hb3[:, k, 0:w]) for k in range(8)], [RINGb[1]] + hbb, [PSb[2]])
                A(wt(5, w), PS[0][:, 0:w], AF.Copy, [PSb[0]], [WTb[5]])
                TT(ct(SC_T[c], cs, cs + w), PS[1][:, 0:w], wt(5, w), ALU.mult, [PSb[1], WTb[5]], cb(SC_T[c], j))
                A(ct(GB_T[c], cs, cs + w), PS[2][:, 0:w], AF.Copy, [PSb[2]], cb(GB_T[c], j))
        for j in blocks_for(ctx_full):
            cs, xc, w = BLKS[j]
            for c in range(4):
                nb = [b for jn in ((8,) if j == 8 else (max(j - 1, 0), j, min(j + 1, 7))) for b in cb(SC_T[c], jn)]
                MM(PS[c % 2][:, 0:w], [(ring(4, (8 + c * 3 + t) * 128, (9 + c * 3 + t) * 128), ct(SC_T[c], cs + t - 1, cs + t - 1 + w)) for t in range(3)],
                   [RINGb[4]] + nb, [PSb[c % 2]])
                TT(ct(GB_T[c], cs, cs + w), PS[c % 2][:, 0:w], ct(GB_T[c], cs, cs + w), ALU.mult, [PSb[c % 2]] + cb(GB_T[c], j), cb(GB_T[c], j))
        outproj_residual(i, GB_T + PO_T, [5, 3], ctx_full, 2)

    AFF_T = 0
    AFW_T = 2
    XE_T = [4, 5]
    ACT_T = 6

    def ctf(i, a, b, p=16):
        base = (i * CTW) // 2
        return CTF[0:p, base + a: base + b]

    def ctfb(i):
        return [b for t in (i, i + 1) for j in range(9) for b in cb(t, j)]

    TVb, TIb, TFb = [WTb[0], WTb[1]], [WTb[2], WTb[3]], [WTb[4], WTb[5]]

    def moe(i, last_layer):
        ctx_full = i < 2
        ntile = 5 if ctx_full else 4
        LD(SP, WR[:, :].rearrange("p (c n) -> p c n", c=8), w_router[i].rearrange("(c p) n -> p c n", p=128), [], [WRb])
        wr3 = WR[:, :].rearrange("p (c n) -> p c n", c=8)
        affb, afwb = ctfb(AFF_T), ctfb(AFW_T)
        for j in blocks_for(ctx_full):
            cs, xc, w = BLKS[j]
            v = 1 if j == 8 else 0
            hb_i = hbrr[0] % 2
            hbrr[0] += 1
            hb3 = HB[hb_i].rearrange("p (c n) -> p c n", c=8)
            hbb = HBb[hb_i]
            for c in range(8):
                k = next_xs()
                LD(SP, wt(k, w), XA[c * 128:(c + 1) * 128, xc:xc + w], [XAb[c][j]], [WTb[k]])
                TT(wt(k, w), wt(k, w), RSTD[:, xc:xc + w], ALU.mult, [WTb[k], RSTDb[j]], [WTb[k]])
                A(wt(k, w), wt(k, w), AF.Identity, [WTb[k], DERb, MODSb], [WTb[k]], scale=G2(c, v), bias=mod(i, 3, c, v))
                P.op(PE, lambda e, c=c, k=k, w=w: e.matmul(PS[0][0:16, 0:w], lhsT=wr3[:, c, :], rhs=wt(k, w), start=(c == 0), stop=(c == 7)),
                     [WRb, WTb[k]], [PSb[0]])
                CP(hb3[:, c, 0:w], wt(k, w), [WTb[k]] + hbb, hbb, eng=POOL)
            A(wt(5, w)[0:16, :], PS[0][0:16, 0:w], AF.Exp, [PSb[0]], [WTb[5]])
            MM(PS[1][0:16, 0:w], [(ONEF[:, :], wt(5, w)[0:16, :])], [ONEFb, WTb[5]], [PSb[1]])
            RCP(wt(6, w)[0:16, :], PS[1][0:16, 0:w], [PSb[1]], [WTb[6]])
            TT(ctf(AFF_T, xc, xc + w), wt(5, w)[0:16, :], wt(6, w)[0:16, :], ALU.mult, [WTb[5], WTb[6]], affb)
            for tt in range(w // 128):
                pb = 2 + (tt % 2)
                TRS([(PSH[pb][:, c * 128:(c + 1) * 128], hb3[:, c, tt * 128:(tt + 1) * 128], cm(IDB)) for c in range(8)],
                    [CMb] + hbb, [PSb[pb]])
                gk = 3 + (tt % 2)
                if tt % 2 == 0:
                    CP(wth(gk), PSH[pb][:, :], [PSb[pb]], [WTb[gk]])
                else:
                    A(wth(gk), PSH[pb][:, :], AF.Copy, [PSb[pb]], [WTb[gk]])
                trow = xc + tt * 128
                LD(SP, HFTOK[trow:trow + 128, :], wth(gk), [WTb[gk]], [HFb[trow // 128]])
        chk("moe_prep")
        TV = WT[0:16, 0:NSLOT]
        TI = WTU[0:16, 1024:1024 + NSLOT]
        TF = WT[0:16, 2048:2048 + NSLOT]
        for (col0, ncol, nround, s0) in ([(0, NL, CAP_L // 8, 0)] + ([(NL, NCX, CAP_C // 8, CAP_L)] if ctx_full else [])):
            for r in range(nround):
                src = ctf(AFF_T, col0, col0 + ncol) if r == 0 else ctf(AFW_T, col0, col0 + ncol)
                srcb = affb if r == 0 else afwb
                sl = slice(s0 + 8 * r, s0 + 8 * r + 8)
                P.op(DVE, lambda e, sl=sl, src=src: e.max(out=TV[:, sl], in_=src), srcb, TVb)
                P.op(DVE, lambda e, sl=sl, src=src: e.max_index(out=TI[:, sl], in_max=TV[:, sl], in_values=src), srcb + TVb, TIb)
                if r < nround - 1:
                    P.op(DVE, lambda e, sl=sl, src=src, col0=col0, ncol=ncol: e.match_replace(out=ctf(AFW_T, col0, col0 + ncol), in_to_replace=TV[:, sl],
                                                                                              in_values=src, imm_value=-1.0), srcb + TVb, afwb)
        CP(TF, TI, TIb, TFb)
        if ctx_full:
            TS(TF[:, CAP_L:NSLOT], TF[:, CAP_L:NSLOT], float(NL), None, ALU.add, None, TFb, TFb)
        for t in range(ntile):
            n = 128 if t < 4 else CAP_C
            TR(PS[4][0:n, 0:16], TV[:, t * 128: t * 128 + n], IDF[0:16, 0:16], TVb + [IDFb], [PSb[4]])
            CP(AFFT[0:n, t * 16:(t + 1) * 16], PS[4][0:n, 0:16], [PSb[4]], [AFFTb])
            TR(PS[5][0:n, 0:16], TF[:, t * 128: t * 128 + n], IDF[0:16, 0:16], TFb + [IDFb], [PSb[5]])
            CP(IDXT[0:n, t * 16:(t + 1) * 16], PS[5][0:n, 0:16], [PSb[5]], [IDXTb])
        chk("moe_topk")
        xe3 = [CT[:, XE_T[k] * CTW: XE_T[k] * CTW + 8 * NSLOT].rearrange("p (c s) -> p c s", c=8) for k in range(2)]
        xeb = [[b for j in range(9) for b in cb(XE_T[k], j)] for k in range(2)]
        act3 = CT[:, ACT_T * CTW: ACT_T * CTW + 16 * NSLOT].rearrange("p (f s) -> p f s", f=16)
        actb = [b for t in (ACT_T, ACT_T + 1) for j in range(9) for b in cb(t, j)]
        GTK = [9, 10, 11, 0, 1]

        pieces = []
        for e in range(NEXP):
            for q in range(4):
                pieces.append(("g", e, q))
                pieces.append(("u", e, q))
            for dq in range(4):
                pieces.append(("d", e, dq))
        slot_of = {}
        loaded = [0]

        def ensure_loaded(upto):
            while loaded[0] <= min(upto, len(pieces) - 1):
                kind, e, q = pieces[loaded[0]]
                s = pcount[0] % NRING
                pcount[0] += 1
                slot_of[loaded[0]] = s
                if kind == "g":
                    LD(POOL, ring3(s, 8), w3(w_gate[mmap[i], e])[:, :, q * 512:(q + 1) * 512], [], [RINGb[s]])
                elif kind == "u":
                    LD(POOL, ring3(s, 8), w3(w_up[mmap[i], e])[:, :, q * 512:(q + 1) * 512], [], [RINGb[s]])
                else:
                    LD(POOL, ring3(s, 16), w_down[mmap[i], e].rearrange("(f p) d -> p f d", p=128)[:, :, q * 256:(q + 1) * 256], [], [RINGb[s]])
                loaded[0] += 1

        def gather(e):
            for t in range(ntile):
                n = 128 if t < 4 else CAP_C
                gk = GTK[t]
                P.dma(POOL, lambda eng, gk=gk, n=n, t=t, e=e: eng.indirect_dma_start(
                    out=WTH[0:n, gk * 1024:(gk + 1) * 1024], out_offset=None, in_=HFTOK[:, :],
                    in_offset=bass.IndirectOffsetOnAxis(ap=IDXT[0:n, t * 16 + e: t * 16 + e + 1], axis=0)),
                    [IDXTb] + HFb, [WTb[gk]])

        def xe_transposes(e):
            k = e % 2
            for t in range(ntile):
                n = 128 if t < 4 else CAP_C
                gk = GTK[t]
                TRS([(PSH[6][:, c * 128: c * 128 + n], WTH[0:n, gk * 1024 + c * 128: gk * 1024 + (c + 1) * 128], CM[0:n, 0:n]) for c in range(8)],
                    [CMb, WTb[gk]], [PSb[6]])
                src = PSH[6][:, :].rearrange("p (c s) -> p c s", c=8)[:, :, 0:n]
                if t % 2 == 0:
                    CP(xe3[k][:, :, t * 128: t * 128 + n], src, [PSb[6]], xeb[k])
                else:
                    P.op(ACT, lambda eng, k=k, t=t, n=n, src=src: eng.activation(out=xe3[k][:, :, t * 128: t * 128 + n], in_=src, func=AF.Copy),
                         [PSb[6]], xeb[k])

        gather(0)
        ensure_loaded(4)
        xe_transposes(0)
        pidx = 0
        ysr = [0]
        for e in range(NEXP):
            k = e % 2
            if e + 1 < NEXP:
                gather(e + 1)
            for q in range(4):
                ensure_loaded(pidx + 5)
                sg_, su_ = slot_of[pidx], slot_of[pidx + 1]
                pidx += 2
                for fl in range(4):
                    fc = q * 4 + fl
                    pg, pu = (fc % 2), 2 + (fc % 2)
                    MM(PS[pg][:, 0:512], [(ring3(sg_, 8)[:, c, fl * 128:(fl + 1) * 128], xe3[k][:, c, 0:512]) for c in range(8)],
                       [RINGb[sg_]] + xeb[k], [PSb[pg]])
                    MM(PS[pu][:, 0:512], [(ring3(su_, 8)[:, c, fl * 128:(fl + 1) * 128], xe3[k][:, c, 0:512]) for c in range(8)],
                       [RINGb[su_]] + xeb[k], [PSb[pu]])
                    if ctx_full:
                        co = (fc % 8) * 64
                        MM(PS[4][:, co:co + 32], [(ring3(sg_, 8)[:, c, fl * 128:(fl + 1) * 128], xe3[k][:, c, 512:544]) for c in range(8)],
                           [RINGb[sg_]] + xeb[k], [PSb[4]])
                        MM(PS[4][:, co + 32:co + 64], [(ring3(su_, 8)[:, c, fl * 128:(fl + 1) * 128], xe3[k][:, c, 512:544]) for c in range(8)],
                           [RINGb[su_]] + xeb[k], [PSb[4]])
                    sk = 5 + (fc % 2)
                    A(wth(sk, 0, 512), PS[pg][:, 0:512], AF.Silu, [PSb[pg]], [WTb[sk]])
                    TT(act3[:, fc, 0:512], wth(sk, 0, 512), PS[pu][:, 0:512], ALU.mult, [WTb[sk], PSb[pu]], actb)
                    if ctx_full and fc % 8 == 7:
                        p4 = PS[4][:, :].rearrange("p (f g s) -> p f g s", f=8, g=2)
                        s3 = wth(7, 0, 256).rearrange("p (f s) -> p f s", f=8)
                        A(s3, p4[:, :, 0, :], AF.Silu, [PSb[4]], [WTb[7]])
                        TT(act3[:, fc - 7: fc + 1, 512:544], s3, p4[:, :, 1, :], ALU.mult, [WTb[7], PSb[4]], actb)
            if e + 1 < NEXP:
                xe_transposes(e + 1)
            for dq in range(4):
                ensure_loaded(pidx + 5)
                sd_ = slot_of[pidx]
                pidx += 1
                d3 = ring3(sd_, 16)
                ys_list = []
                for t in range(ntile):
                    n = 128 if t < 4 else CAP_C
                    yb = ysr[0] % 4
                    ysr[0] += 1
                    pyb, pyo = 5 + (yb // 2) * 2, (yb % 2) * 256
                    py = PS[pyb][0:n, pyo:pyo + 256]
                    MM(py, [(act3[:, fc, t * 128: t * 128 + n], d3[:, fc, :]) for fc in range(16)], [RINGb[sd_]] + actb, [PSb[pyb]])
                    yk = (ysr[0] - 1) % 8
                    ytile = [2, 3, 4, 8][yk // 2]
                    ysl = WT[0:n, ytile * 512 + (yk % 2) * 256: ytile * 512 + (yk % 2 + 1) * 256]
                    ysb = WTb[ytile]
                    if (yb // 2) == 0:
                        A(ysl, py, AF.Identity, [PSb[pyb], AFFTb], [ysb], scale=AFFT[0:n, t * 16 + e: t * 16 + e + 1])
                    else:
                        TS(ysl, py, AFFT[0:n, t * 16 + e: t * 16 + e + 1], None, ALU.mult, None, [PSb[pyb], AFFTb], [ysb])
                    ys_list.append((ysl, ysb, n, t))
                fns = []
                for (ysl, ysb, n, t) in ys_list:
                    fns.append(lambda eng, ysl=ysl, n=n, t=t, e=e, dq=dq: eng.indirect_dma_start(
                        out=MOQ[dq][:, :], out_offset=bass.IndirectOffsetOnAxis(ap=IDXT[0:n, t * 16 + e: t * 16 + e + 1], axis=0),
                        in_=ysl, in_offset=None, compute_op=ALU.add))
                P.dma(POOL, fns, [IDXTb, MOb[dq]] + [ysb for (_, ysb, _, _) in ys_list], [MOb[dq]])
        chk("moe_exp")
        for j in blocks_for(ctx_full):
            cs, xc, w = BLKS[j]
            v = 1 if j == 8 else 0
            pend = None
            for half in range(2):
                for tt in range(w // 128):
                    gk = 9 + (tt % 2)
                    trow = xc + tt * 128
                    P.dma(SP, [lambda e_, gk=gk, trow=trow, q=q: e_.dma_start(out=wt(gk, 512)[:, (q % 2) * 256:(q % 2 + 1) * 256], in_=MOQ[q][trow:trow + 128, :])
                               for q in (2 * half, 2 * half + 1)], [MOb[2 * half], MOb[2 * half + 1]], [WTb[gk]])
                    for cl in range(4):
                        TR(PS[cl][:, tt * 128:(tt + 1) * 128], wt(gk, 512)[:, cl * 128:(cl + 1) * 128], IDF[:, :], [WTb[gk], IDFb], [PSb[cl]])
                for cl in range(4):
                    c = half * 4 + cl
                    kx = next_xs()
                    LD(SP, wt(kx, w), XA[c * 128:(c + 1) * 128, xc:xc + w], [XAb[c][j]], [WTb[kx]])
                    STT(wt(kx, w), PS[cl][:, 0:w], mod(i, 5, c, v), wt(kx, w), ALU.mult, ALU.add, [PSb[cl], WTb[kx], MODSb], [WTb[kx]])
                    if last_layer:
                        LD(SP, outT[c * 128:(c + 1) * 128, xc:xc + w], wt(kx, w), [WTb[kx]], [OUTb])
                    else:
                        LD(SP, XA[c * 128:(c + 1) * 128, xc:xc + w], wt(kx, w), [WTb[kx]], [XAb[c][j]])
                        kk = stat_chunk(j, c, wt(kx, w), WTb[kx], w)
                        if pend is not None:
                            stat_mm(j, *pend, w)
                        pend = (c, kk)
            if not last_layer:
                stat_mm(j, *pend, w)
                stat_fin(j, w)
        if not last_layer:
            zero_mo(True)

    try:
        chk("pre")
        for i in range(start_layer, n_layers):
            der_setup(i)
            if i % 2 == 0:
                even_mixer(i)
            else:
                odd_mixer(i)
            if stop == "mix%d" % i:
                break
            moe(i, i == DEPTH - 1)
    except _Stop:
        pass
    if debug:
        for c in range(8):
            LD(SP, dbg["xa"][c * 128:(c + 1) * 128, :], XA[c * 128:(c + 1) * 128, :], [b for b in XAb[c]], [OUTb])
    P.finish()
    return nc, P


def _rope_tables():
    n_freq = 16
    inv = (10000.0 ** (-np.arange(n_freq, dtype=np.float32) / n_freq)).astype(np.float32)
    t = np.arange(NL)
    row = (t // 64).astype(np.float32)
    col = (t % 64).astype(np.float32)
    ang_r = row[:, None] * inv
    ang_c = col[:, None] * inv
    cos = np.zeros((64, NL), np.float32)
    sin = np.zeros((64, NL), np.float32)
    cos[0:16] = np.cos(ang_r).T
    cos[16:32] = np.cos(ang_r).T
    cos[32:48] = np.cos(ang_c).T
    cos[48:64] = np.cos(ang_c).T
    sin[0:16] = np.sin(ang_r).T
    sin[16:32] = np.sin(ang_r).T
    sin[32:48] = np.sin(ang_c).T
    sin[48:64] = np.sin(ang_c).T
    cos = np.concatenate([cos, cos], 0)
    sin = np.concatenate([sin, sin], 0)
    return np.concatenate([cos, sin], 1).astype(np.float32)


def _const_mats():
    m = np.zeros((128, 5 * 128), np.float32)
    m[:, 0:128] = np.eye(128)
    m[:, 128:256] = 1.0
    blk = np.zeros((128, 128), np.float32)
    blk[0:64, 0:64] = 1.0
    blk[64:128, 64:128] = 1.0
    m[:, 256:384] = blk
    rot = np.zeros((128, 128), np.float32)
    for base in range(0, 128, 32):
        for d in range(16):
            rot[base + 16 + d, base + d] = -1.0
            rot[base + d, base + 16 + d] = 1.0
    m[:, 384:512] = rot
    return m


def _pool_fix():
    f = np.ones((128, 64), np.float32)
    for gi, w in enumerate((2, 4, 8, 16)):
        h = w // 2
        for t in range(h):
            f[:, gi * 16 + t] = w / float(t + h)
        for m_ in range(h - 1):
            f[:, gi * 16 + 8 + m_] = w / float((h - 1 - m_) + h)
    return f


def _pack_vecs(inp, b):
    v = np.zeros((128, NV), np.float32)

    def col(a):
        a = np.asarray(a, np.float32)
        return a.reshape(-1, 128).T

    for i in range(DEPTH):
        v[:, V_NMG + i * 8: V_NMG + (i + 1) * 8] = col(inp["norm_mix_g"][i])
        v[:, V_NFG + i * 8: V_NFG + (i + 1) * 8] = col(inp["norm_ffn_g"][i])
        v[:, V_BMOD + i * 48: V_BMOD + (i + 1) * 48] = col(inp["b_mod"][i])
    for j in range(2):
        o = V_EV + j * 138
        v[:, o: o + 4] = col(inp["ev_conv_b"][j])
        v[:, o + 4: o + 8] = col(inp["ev_ln_g"][j])
        v[:, o + 8: o + 12] = col(inp["ev_ln_b"][j])
        v[:, o + 12] = np.tile(np.asarray(inp["ev_q_norm_g"][j], np.float32), 2)
        v[:, o + 13] = np.tile(np.asarray(inp["ev_k_norm_g"][j], np.float32), 2)
        cw = np.asarray(inp["ev_conv_w"][j], np.float32)
        for c in range(4):
            v[:, o + 14 + c * 31: o + 14 + (c + 1) * 31] = cw[:, c * 128:(c + 1) * 128].T
        o2 = V_OD + j * 16
        ow = np.asarray(inp["od_conv_w"][j], np.float32)
        for c in range(4):
            v[:, o2 + c * 3: o2 + (c + 1) * 3] = ow[:, c * 128:(c + 1) * 128].T
        v[:, o2 + 12: o2 + 16] = col(inp["od_pool_scale"][j])
    cc = np.stack([col(inp["c"][b]), col(inp["c_ctx"])], axis=-1)
    v[:, V_CC: V_CC + 16] = cc.reshape(128, 16)
    return v


_CACHE = {}


def kernel(**inputs):
    inp = {k: np.asarray(v) for k, v in inputs.items()}
    n = 8
    if "nc" not in _CACHE:
        _CACHE["nc"] = build_program()[0]
    nc = _CACHE["nc"]
    rope = _rope_tables()
    cm = _const_mats()
    pf = _pool_fix()
    zeros = np.zeros((NT, D), np.float32)
    shared = {k: np.ascontiguousarray(inp[k], dtype=np.float32) for k in
              ("w_mod", "ev_w_in", "ev_w_out", "od_w_in", "od_w_out", "od_pool_w", "w_router", "w_gate", "w_up", "w_down")}
    in_maps = []
    for b in range(n):
        m = dict(shared)
        m["xT"] = np.ascontiguousarray(inp["x"][b].T, dtype=np.float32)
        m["ctxT"] = np.ascontiguousarray(inp["ctx"][b].T, dtype=np.float32)
        m["vecs"] = _pack_vecs(inp, b)
        m["cmats"] = cm
        m["rope_cs"] = rope
        m["pool_fix"] = pf
        m["zeros_d"] = zeros
        in_maps.append(m)
    res = run_bass_kernel_spmd(nc, in_maps, core_ids=list(range(n)))
    out = np.stack([np.ascontiguousarray(res.results[b]["outT"].T) for b in range(n)], axis=0)
    return out.astype(np.float32)
```

```python
import os
import numpy as np
import concourse.bass as bass
import concourse.mybir as mybir
from concourse.bass_utils import run_bass_kernel_spmd

F32 = mybir.dt.float32
BF16 = mybir.dt.bfloat16
I32 = mybir.dt.int32
U32 = mybir.dt.uint32
ALU = mybir.AluOpType
AF = mybir.ActivationFunctionType

KDBG = set(os.environ.get("KDBG", "").split(","))
PE, ACT, DVE, POOL, SP = "pe", "act", "dve", "pool", "sp"
ENGS = [PE, ACT, DVE, POOL, SP]
STRICT_SAME = {PE: False, ACT: True, DVE: True, POOL: True, SP: False}


class _Stop(Exception):
    pass


class Buf:
    __slots__ = ("name", "w", "r")

    def __init__(self, name):
        self.name = name
        self.w = []
        self.r = []


class Prog:
    def __init__(self, nc):
        self.nc = nc
        self.q = {e: [] for e in ENGS}
        self.sems = {}
        self.cnt = {}
        self.waited = {e: {} for e in ENGS}
        for e in (PE, ACT, DVE, POOL):
            self._mksem("e_" + e)
        nd = {SP: 16, ACT: 4, POOL: 16}
        self.dma_pool = {}
        self.dma_rr = {}
        for e, n in nd.items():
            self.dma_pool[e] = [self._mksem("d_%s%d" % (e, i)) for i in range(n)]
            self.dma_rr[e] = 0
        self.n_ops = 0

    def _mksem(self, key):
        self.sems[key] = self.nc.alloc_semaphore(key)
        self.cnt[key] = 0
        return key

    def buf(self, name):
        return Buf(name)

    def _deps(self, eng, reads, writes, same_ok=False, stream=False):
        need = {}

        def add(t):
            k, v, e = t[0], t[1], t[2]
            if e == eng and (same_ok or not STRICT_SAME[eng] or (stream and len(t) > 3 and t[3])):
                return
            if need.get(k, 0) < v:
                need[k] = v

        for b in reads:
            for t in b.w:
                add(t)
        for b in writes:
            for t in b.w:
                add(t)
            for t in b.r:
                add(t)
        out = []
        wd = self.waited[eng]
        for k, v in need.items():
            if wd.get(k, 0) < v:
                wd[k] = v
                out.append((k, v))
        return out

    def _commit(self, toks, reads, writes):
        for b in reads:
            b.r.extend(toks)
            if len(b.r) > 64:
                mx = {}
                for t in b.r:
                    k, v = t[0], t[1]
                    if k not in mx or mx[k][1] < v:
                        mx[k] = (k, v, t[2])
                b.r = list(mx.values())
        for b in writes:
            b.w = list(toks)
            b.r = []

    def op(self, eng, fn, reads=(), writes=(), same_ok=False, stream=False):
        stream = False
        waits = self._deps(eng, reads, writes, same_ok, stream)
        k = "e_" + eng
        self.cnt[k] += 1
        tok = (k, self.cnt[k], eng, stream)
        sems = self.sems

        def thunk(e, waits=waits, fn=fn, s=sems[k]):
            for (wk, wv) in waits:
                e.wait_ge(sems[wk], wv)
            fn(e).then_inc(s, 1)

        self.q[eng].append(thunk)
        self._commit([tok], reads, writes)
        self.n_ops += 1
        return tok

    def dma(self, eng, fns, reads=(), writes=()):
        if not isinstance(fns, (list, tuple)):
            fns = [fns]
        waits = self._deps(eng, reads, writes)
        wd = self.waited[eng]
        pool = self.dma_pool[eng]
        toks = []
        items = []
        for fn in fns:
            k = pool[self.dma_rr[eng] % len(pool)]
            self.dma_rr[eng] += 1
            prev = self.cnt[k]
            if prev > 0 and wd.get(k, 0) < prev:
                wd[k] = prev
                waits.append((k, prev))
            self.cnt[k] += 16
            toks.append((k, self.cnt[k], "dma"))
            items.append((fn, self.sems[k]))
        sems = self.sems

        def thunk(e, waits=waits, items=items):
            for (wk, wv) in waits:
                e.wait_ge(sems[wk], wv)
            for fn, s in items:
                fn(e).then_inc(s, 16)

        self.q[eng].append(thunk)
        self._commit(toks, reads, writes)
        self.n_ops += 1
        return toks

    def finish(self):
        nc = self.nc
        sems = self.sems
        cnt = dict(self.cnt)

        def fin_thunk(e):
            for k, v in cnt.items():
                if v > 0:
                    e.wait_ge(sems[k], v)

        self.q[SP].append(fin_thunk)
        q = self.q
        with nc.Block() as block:
            @block.tensor
            def _(e):
                for t in q[PE]:
                    t(e)

            @block.scalar
            def _(e):
                for t in q[ACT]:
                    t(e)

            @block.vector
            def _(e):
                for t in q[DVE]:
                    t(e)

            @block.gpsimd
            def _(e):
                for t in q[POOL]:
                    t(e)

            @block.sync
            def _(e):
                for t in q[SP]:
                    t(e)


D = 1024
NL = 4096
NCX = 256
NT = NL + NCX
PAD = 15
L0 = PAD
C0 = PAD + NL + 2 * PAD
CTW = 4416
NCT = 12
NRING = 7
EPS = 1e-6
DEPTH = 4
NEXP = 16
CAP_L = 512
CAP_C = 32
NSLOT = CAP_L + CAP_C
BLKS = [(L0 + 512 * j, 512 * j, 512) for j in range(8)] + [(C0, NL, NCX)]

V_NMG = 0
V_NFG = 32
V_BMOD = 64
V_EV = 256
V_OD = 532
V_CC = 564
NV = 580


def build_program(n_layers=DEPTH, debug=False, stop=None, small_moe=False, start_layer=0, moe_decl=None):
    nc = bass.Bass("TRN2", target_bir_lowering=False)
    P = Prog(nc)

    def din(name, shape, dt=F32):
        return nc.dram_tensor(name, list(shape), dt, kind="ExternalInput")

    xT = din("xT", [D, NL])
    ctxT = din("ctxT", [D, NCX])
    vecs = din("vecs", [128, NV])
    cmats = din("cmats", [128, 5 * 128])
    rope_cs = din("rope_cs", [128, 2 * NL])
    pool_fix = din("pool_fix", [128, 64])
    zeros_d = din("zeros_d", [NT, D])
    w_mod = din("w_mod", [DEPTH, D, 6 * D])
    ev_w_in = din("ev_w_in", [2, D, 1792])
    ev_w_out = din("ev_w_out", [2, D, D])
    od_w_in = din("od_w_in", [2, D, 2048])
    od_w_out = din("od_w_out", [2, D, D])
    od_pool_w = din("od_pool_w", [2, 4, 128, 128])
    w_router = din("w_router", [DEPTH, D, NEXP])
    mshape = [1, 1] if small_moe else [DEPTH if moe_decl is None else len(moe_decl), NEXP]
    mmap = {l: l for l in range(DEPTH)} if moe_decl is None else {l: n for n, l in enumerate(moe_decl)}
    w_gate = din("w_gate", mshape + [D, 2048])
    w_up = din("w_up", mshape + [D, 2048])
    w_down = din("w_down", mshape + [2048, D])
    outT = nc.dram_tensor("outT", [D, NL], F32, kind="ExternalOutput")
    XA = nc.dram_tensor("XA", [D, NT], F32)
    HFTOK = nc.dram_tensor("HFTOK", [NT, D], BF16)
    MOQ = [nc.dram_tensor("MO%d" % q, [NT, 256], F32) for q in range(4)]
    dbg = {}
    if debug:
        dbg["xa"] = nc.dram_tensor("dbg_xa", [D, NT], F32, kind="ExternalOutput")

    XAb = [[P.buf("xa%d_%d" % (c, j)) for j in range(9)] for c in range(8)]
    HFb = [P.buf("hftok%d" % t) for t in range(34)]
    MOb = [P.buf("mo%d" % q) for q in range(4)]
    OUTb = P.buf("out")

    CT = nc.alloc_sbuf_tensor("ct", [128, NCT * CTW], BF16)
    CTF = CT.bitcast(F32)
    CTb = [[[P.buf("ct%d_%d_%d" % (i, j, h)) for h in range(2)] for j in range(9)] for i in range(NCT)]
    RING = nc.alloc_sbuf_tensor("ring", [128, NRING * 4096], BF16)
    RINGb = [P.buf("ring%d" % s) for s in range(NRING)]
    RSTD = nc.alloc_sbuf_tensor("rstd", [128, NT], F32)
    RSTDb = [P.buf("rstd%d" % j) for j in range(9)]
    WT = nc.alloc_sbuf_tensor("wt", [128, 12 * 512], F32)
    WTH = WT.bitcast(BF16)
    WTI = WT.bitcast(I32)
    WTU = WT.bitcast(U32)
    WTb = [P.buf("wt%d" % k) for k in range(12)]
    CM = nc.alloc_sbuf_tensor("cm", [128, 4 * 128], BF16)
    CMb = P.buf("cm")
    IDF = nc.alloc_sbuf_tensor("idf", [128, 128], F32)
    IDFb = P.buf("idf")
    ONEF = nc.alloc_sbuf_tensor("onef", [16, 16], F32)
    ONEFb = P.buf("onef")
    VEC = nc.alloc_sbuf_tensor("vec", [128, NV], F32)
    VECb = P.buf("vec")
    MODS = nc.alloc_sbuf_tensor("mods", [128, DEPTH * 96], F32)
    MODSb = P.buf("mods")
    DER = nc.alloc_sbuf_tensor("der", [128, 96], F32)
    DERb = P.buf("der")
    SCC = nc.alloc_sbuf_tensor("scc", [128, 16], BF16)
    SCCb = P.buf("scc")
    WR = nc.alloc_sbuf_tensor("wr", [128, 8 * NEXP], F32)
    WRb = P.buf("wr")
    AFFT = nc.alloc_sbuf_tensor("afft", [128, 5 * NEXP], F32)
    AFFTb = P.buf("afft")
    IDXT = nc.alloc_sbuf_tensor("idxt", [128, 5 * NEXP], I32)
    IDXTb = P.buf("idxt")
    PFIX = nc.alloc_sbuf_tensor("pfix", [128, 64], F32)
    PFIXb = P.buf("pfix")
    PSD = [nc.alloc_psum_tensor("psd%d" % k, [128, 1024], F32) for k in range(4)]
    PSDH = [p.bitcast(BF16) for p in PSD]
    PS = [PSD[k // 2][:, (k % 2) * 512:(k % 2 + 1) * 512] for k in range(8)]
    PSH = [PSDH[k // 2][:, (k % 2) * 1024:(k % 2 + 1) * 1024] for k in range(8)]
    PSb = [P.buf("ps%d" % k) for k in range(8)]

    def ct(i, a, b):
        return CT[:, i * CTW + a: i * CTW + b]

    def ctp(i, a, b, p0, p1):
        return CT[p0:p1, i * CTW + a: i * CTW + b]

    def cb(i, j):
        return CTb[i][j]

    def wt(k, w=512):
        return WT[:, k * 512: k * 512 + w]

    def wth(k, a=0, b=1024):
        return WTH[:, k * 1024 + a: k * 1024 + b]

    def ring(s, a=0, b=4096):
        return RING[:, s * 4096 + a: s * 4096 + b]

    def ring3(s, c):
        return RING[:, s * 4096:(s + 1) * 4096].rearrange("p (c n) -> p c n", c=c)

    def cm(k):
        return CM[:, k * 128:(k + 1) * 128]

    IDB, ONESB, BLK1, ROT = 0, 1, 2, 3

    def vcol(k, n=1):
        return VEC[:, k:k + n]

    def MM(out, pairs, reads, writes):
        def fn(e, out=out, pairs=pairs):
            n = len(pairs)
            last = None
            for t, (l, r) in enumerate(pairs):
                last = e.matmul(out, lhsT=l, rhs=r, start=(t == 0), stop=(t == n - 1))
            return last
        return P.op(PE, fn, reads, writes)

    def TR(out, in_, ident, reads, writes):
        return P.op(PE, lambda e: e.transpose(out=out, in_=in_, identity=ident), reads, writes)

    def TRS(items, reads, writes):
        def fn(e, items=items):
            last = None
            for (o, i_, idn) in items:
                last = e.transpose(out=o, in_=i_, identity=idn)
            return last
        return P.op(PE, fn, reads, writes)

    def big(ap):
        sh = ap.shape
        return len(sh) == 2 and sh[1] >= 256

    def A(out, in_, func, reads, writes, scale=1.0, bias=0.0, accum=None):
        if accum is None:
            return P.op(ACT, lambda e: e.activation(out=out, in_=in_, func=func, bias=bias, scale=scale), reads, writes,
                        stream=big(out) and big(in_))
        return P.op(ACT, lambda e: e.activation(out=out, in_=in_, func=func, bias=bias, scale=scale, accum_out=accum), reads, writes)

    def TT(out, in0, in1, op, reads, writes, eng=DVE):
        return P.op(eng, lambda e: e.tensor_tensor(out=out, in0=in0, in1=in1, op=op), reads, writes,
                    stream=(eng == DVE) and big(out) and big(in0) and big(in1))

    def TS(out, in0, s1, s2, op0, op1, reads, writes, eng=DVE):
        if s2 is None:
            return P.op(eng, lambda e: e.tensor_scalar(out, in0, s1, None, op0=op0), reads, writes, stream=(eng == DVE) and big(out) and big(in0))
        return P.op(eng, lambda e: e.tensor_scalar(out, in0, s1, s2, op0=op0, op1=op1), reads, writes, stream=(eng == DVE) and big(out) and big(in0))

    def STT(out, in0, scalar, in1, op0, op1, reads, writes, eng=DVE):
        return P.op(eng, lambda e: e.scalar_tensor_tensor(out=out, in0=in0, scalar=scalar, in1=in1, op0=op0, op1=op1), reads, writes,
                    stream=(eng == DVE) and big(out) and big(in0) and big(in1))

    def CP(out, in_, reads, writes, eng=DVE):
        return P.op(eng, lambda e: e.tensor_copy(out, in_), reads, writes, stream=(eng == DVE) and big(out) and big(in_))

    def RCP(out, in_, reads, writes):
        return P.op(DVE, lambda e: e.reciprocal(out=out, in_=in_), reads, writes, stream=big(out) and big(in_))

    def MSET(ap, val, writes, eng=POOL):
        return P.op(eng, lambda e: e.memset(ap, val), (), writes)

    def LD(eng, out, in_, reads, writes):
        return P.dma(eng, lambda e: e.dma_start(out=out, in_=in_), reads, writes)

    LD(POOL, CM[:, :], cmats[:, 0:512], [], [CMb])
    LD(SP, IDF[:, :], cmats[:, 0:128], [], [IDFb])
    LD(SP, ONEF[:, :], cmats[0:16, 128:144], [], [ONEFb])
    LD(SP, VEC[:, :], vecs[:, :], [], [VECb])
    LD(SP, PFIX[:, :], pool_fix[:, :], [], [PFIXb])
    def zero_mo(reads):
        zsrc = zeros_d[0:NT // 4, :].rearrange("(a b) d -> a (b d)", a=16)
        for q in range(4):
            LD(SP, MOQ[q][:, :].rearrange("(a b) d -> a (b d)", a=16), zsrc, [MOb[q]] if reads else [], [MOb[q]])
    zero_mo(False)
    for i in range(NCT):
        MSET(ct(i, 0, CTW), 0.0, [b for j in range(9) for b in cb(i, j)])
    for k in range(12):
        MSET(wt(k), 0.0, [WTb[k]])

    A(WT[:, 0:16], VEC[:, V_CC:V_CC + 16], AF.Silu, [VECb, WTb[0]], [WTb[0]])
    CP(SCC[:, :], WT[:, 0:16], [WTb[0]], [SCCb])
    scc3 = SCC[:, :].rearrange("p (c v) -> p c v", v=2)
    pcount = [0]
    for i in range(n_layers):
        for n in range(12):
            s = pcount[0] % NRING
            pcount[0] += 1
            LD(POOL, ring3(s, 8), w_mod[i].rearrange("(c p) n -> p c n", p=128)[:, :, n * 512:(n + 1) * 512], [], [RINGb[s]])
            r3 = ring3(s, 8)
            for oc in range(4):
                k = n * 4 + oc
                MM(PS[0][:, k * 2:k * 2 + 2], [(r3[:, c, oc * 128:(oc + 1) * 128], scc3[:, c, :]) for c in range(8)],
                   [RINGb[s], SCCb], [PSb[0]])
        m3 = MODS[:, i * 96:(i + 1) * 96].rearrange("p (k v) -> p k v", v=2)
        p3 = PS[0][:, 0:96].rearrange("p (k v) -> p k v", v=2)
        for v in range(2):
            TT(m3[:, :, v], p3[:, :, v], VEC[:, V_BMOD + i * 48: V_BMOD + (i + 1) * 48], ALU.add, [PSb[0], VECb], [MODSb])

    def mod(i, grp, c, v):
        k = grp * 8 + c
        return MODS[:, i * 96 + k * 2 + v: i * 96 + k * 2 + v + 1]

    def der_setup(i):
        d3 = DER[:, 0:32].rearrange("p (g v c) -> p g v c", g=2, v=2)
        m4 = MODS[:, i * 96:(i + 1) * 96].rearrange("p (g c v) -> p g c v", g=6, v=2)
        for v in range(2):
            STT(d3[:, 0, v, :], m4[:, 1, :, v], 1.0, VEC[:, V_NMG + i * 8: V_NMG + i * 8 + 8], ALU.add, ALU.mult, [MODSb, VECb], [DERb])
            STT(d3[:, 1, v, :], m4[:, 4, :, v], 1.0, VEC[:, V_NFG + i * 8: V_NFG + i * 8 + 8], ALU.add, ALU.mult, [MODSb, VECb], [DERb])

    def G1(c, v):
        return DER[:, v * 8 + c: v * 8 + c + 1]

    def G2(c, v):
        return DER[:, 16 + v * 8 + c: 16 + v * 8 + c + 1]

    SS = 7

    sqrr = [0]

    def stat_chunk(j, c, xs_ap, xs_buf, w):
        k = 3 + (sqrr[0] % 2)
        sqrr[0] += 1
        A(wth(k, 0, w), xs_ap, AF.Square, [xs_buf], [WTb[k]])
        return k

    def stat_mm(j, c, k, w):
        P.op(PE, lambda e: e.matmul(PS[SS][:, 0:w], lhsT=cm(ONESB), rhs=wth(k, 0, w), start=(c == 0), stop=(c == 7)),
             [CMb, WTb[k]], [PSb[SS]])

    def stat_fin(j, w):
        xcol = BLKS[j][1]
        A(wt(5, w), PS[SS][:, 0:w], AF.Sqrt, [PSb[SS]], [WTb[5]], scale=1.0 / D, bias=EPS)
        RCP(RSTD[:, xcol:xcol + w], wt(5, w), [WTb[5]], [RSTDb[j]])

    def blocks_for(use_ctx):
        return list(range(9)) if use_ctx else list(range(8))

    xsrr = [0]

    def next_xs():
        k = xsrr[0] % 3
        xsrr[0] += 1
        return k

    for j in range(9):
        cs, xc, w = BLKS[j]
        pend = None
        for c in range(8):
            k = next_xs()
            src = xT[c * 128:(c + 1) * 128, xc:xc + w] if j < 8 else ctxT[c * 128:(c + 1) * 128, 0:w]
            LD(SP, wt(k, w), src, [], [WTb[k]])
            LD(SP, XA[c * 128:(c + 1) * 128, xc:xc + w], wt(k, w), [WTb[k]], [XAb[c][j]])
            kk = stat_chunk(j, c, wt(k, w), WTb[k], w)
            if pend is not None:
                stat_mm(j, *pend, w)
            pend = (c, kk)
        stat_mm(j, *pend, w)
        stat_fin(j, w)

    def chk(name):
        if stop == name:
            raise _Stop()

    HB = [WTH[:, 8 * 1024: 12 * 1024], RING[:, 6 * 4096: 7 * 4096]]
    HBb = [[WTb[8], WTb[9], WTb[10], WTb[11]], [RINGb[6]]]
    hbrr = [0]

    def build_h(i, j, which):
        cs, xc, w = BLKS[j]
        v = 1 if j == 8 else 0
        hb_i = hbrr[0] % 2 if "hb0" not in KDBG else 0
        hbrr[0] += 1
        hb3 = HB[hb_i].rearrange("p (c n) -> p c n", c=8)
        for c in range(8):
            k = next_xs()
            LD(SP, wt(k, w), XA[c * 128:(c + 1) * 128, xc:xc + w], [XAb[c][j]], [WTb[k]])
            TT(wt(k, w), wt(k, w), RSTD[:, xc:xc + w], ALU.mult, [WTb[k], RSTDb[j]], [WTb[k]])
            if which == 1:
                A(hb3[:, c, 0:w], wt(k, w), AF.Identity, [WTb[k], DERb, MODSb] + HBb[hb_i], HBb[hb_i],
                  scale=G1(c, v), bias=mod(i, 0, c, v))
            else:
                A(hb3[:, c, 0:w], wt(k, w), AF.Identity, [WTb[k], DERb, MODSb] + HBb[hb_i], HBb[hb_i],
                  scale=G2(c, v), bias=mod(i, 3, c, v))
        return hb3, HBb[hb_i]

    def load_w_slot(slot, src3, ncols=512, dst_off=0):
        LD(POOL, ring3(slot, 8)[:, :, dst_off:dst_off + ncols], src3, [], [RINGb[slot]])

    def w3(wh):
        return wh.rearrange("(c p) n -> p c n", p=128)

    def outproj_residual(i, mix_tiles, wslots, use_ctx, gate_grp):
        for j in blocks_for(use_ctx):
            cs, xc, w = BLKS[j]
            v = 1 if j == 8 else 0
            pend = None
            for c in range(8):
                slot = wslots[c // 4]
                r3 = ring3(slot, 8)
                pb = c % 2
                MM(PS[pb][:, 0:w], [(r3[:, k, (c % 4) * 128:(c % 4 + 1) * 128], ct(mix_tiles[k], cs, cs + w)) for k in range(8)],
                   [RINGb[slot]] + [b for k in range(8) for b in cb(mix_tiles[k], j)], [PSb[pb]])
                kx = next_xs()
                LD(SP, wt(kx, w), XA[c * 128:(c + 1) * 128, xc:xc + w], [XAb[c][j]], [WTb[kx]])
                STT(wt(kx, w), PS[pb][:, 0:w], mod(i, gate_grp, c, v), wt(kx, w), ALU.mult, ALU.add,
                    [PSb[pb], WTb[kx], MODSb], [WTb[kx]])
                LD(SP, XA[c * 128:(c + 1) * 128, xc:xc + w], wt(kx, w), [WTb[kx]], [XAb[c][j]])
                kk = stat_chunk(j, c, wt(kx, w), WTb[kx], w)
                if pend is not None:
                    stat_mm(j, *pend, w)
                pend = (c, kk)
            stat_mm(j, *pend, w)
            stat_fin(j, w)

    U_T = [0, 1, 2, 3]
    CO_T = [4, 5, 6, 7]
    Q_T = [0, 1, 2, 3]
    K_T = 8
    V_T = 9
    MISC_T = 11

    def vtok(kt, a, b):
        base = V_T * CTW + kt * 256
        return CT[:, base + a: base + b]

    VTb = [P.buf("vt%d" % kt) for kt in range(34)]

    RINGF = RING.bitcast(F32)
    COS_AP = RINGF[:, 4 * 2048: 4 * 2048 + 512]
    SIN_AP = RINGF[:, 4 * 2048 + 512: 4 * 2048 + 1024]
    KP_T = [K_T, MISC_T]

    def zero_pads(tiles):
        for t in tiles:
            MSET(ct(t, 0, PAD), 0.0, cb(t, 0))
            MSET(ct(t, L0 + NL, C0), 0.0, cb(t, 7) + cb(t, 8))
            MSET(ct(t, C0 + NCX, CTW), 0.0, cb(t, 8))

    def even_mixer(i):
        jj = i // 2
        ctx_full = i < 2
        ve = V_EV + jj * 138
        zero_pads(U_T)
        wi = w3(ev_w_in[jj])
        load_w_slot(0, wi[:, :, 0:512])
        load_w_slot(1, wi[:, :, 512:1024])
        for cq in range(4):
            for half in range(2):
                hq = half * 4 + cq
                load_w_slot(2, wi[:, :, 1024 + hq * 64: 1024 + (hq + 1) * 64], 64, cq * 128 + half * 64)
        load_w_slot(3, wi[:, :, 1536:1792], 256, 0)
        for j in blocks_for(ctx_full):
            cs, xc, w = BLKS[j]
            hb3, hbb = build_h(i, j, 1)
            for c in range(4):
                MM(PS[0][:, 0:w], [(ring3(0, 8)[:, k, c * 128:(c + 1) * 128], hb3[:, k, 0:w]) for k in range(8)],
                   [RINGb[0]] + hbb, [PSb[0]])
                MM(PS[1][:, 0:w], [(ring3(1, 8)[:, k, c * 128:(c + 1) * 128], hb3[:, k, 0:w]) for k in range(8)],
                   [RINGb[1]] + hbb, [PSb[1]])
                A(wt(5, w), PS[1][:, 0:w], AF.Sigmoid, [PSb[1]], [WTb[5]])
                TT(ct(U_T[c], cs, cs + w), PS[0][:, 0:w], wt(5, w), ALU.mult, [PSb[0], WTb[5]], cb(U_T[c], j))
        chk("passA")
        DS = [4, 5, 6, 0]
        for c in range(4 if "nodiag" not in KDBG else 0):
            for t in range(31):
                TS(ring(DS[c], t * 128, (t + 1) * 128), cm(IDB), VEC[:, ve + 14 + c * 31 + t: ve + 14 + c * 31 + t + 1], None,
                   ALU.mult, None, [CMb, VECb], [RINGb[DS[c]]], eng=POOL if (t % 2) else DVE)
        wo = w3(ev_w_out[jj])
        def load_wout(slot, col0):
            LD(POOL, ring3(slot, 8)[:, 0:4, :], wo[:, 0:4, col0:col0 + 512], [], [RINGb[slot]])
            woh = ev_w_out[jj][512:1024, col0:col0 + 512].rearrange("(h p) n -> p h n", p=64)
            for half in range(2):
                LD(POOL, RING[half * 64:(half + 1) * 64, slot * 4096:(slot + 1) * 4096].rearrange("p (c n) -> p c n", c=8)[:, 4:8, :],
                   woh[:, half * 4: half * 4 + 4, :], [], [RINGb[slot]])
        load_wout(1, 0)
        for j in (blocks_for(ctx_full) if "noconv" not in KDBG else []):
            cs, xc, w = BLKS[j]
            for c in range(4):
                MM(PS[c % 2][:, 0:w], [(ring(DS[c], t * 128, (t + 1) * 128), ct(U_T[c], cs + t - 15, cs + t - 15 + w)) for t in range(31)],
                   [RINGb[DS[c]]] + [b for jn in ((8,) if j == 8 else (max(j - 1, 0), j, min(j + 1, 7))) for b in cb(U_T[c], jn)], [PSb[c % 2]])
                A(wt(5 + c, w), PS[c % 2][:, 0:w], AF.Identity, [PSb[c % 2], VECb], [WTb[5 + c]], bias=VEC[:, ve + c: ve + c + 1])
                A(wth(3, 0, w), wt(5 + c, w), AF.Square, [WTb[5 + c]], [WTb[3]])
                CP(wth(4, 0, w), wt(5 + c, w), [WTb[5 + c]], [WTb[4]])
                P.op(PE, lambda e, c=c, w=w: e.matmul(PS[2][:, 0:w], lhsT=cm(ONESB), rhs=wth(4, 0, w), start=(c == 0), stop=(c == 3)),
                     [CMb, WTb[4]], [PSb[2]])
                P.op(PE, lambda e, c=c, w=w: e.matmul(PS[3][:, 0:w], lhsT=cm(ONESB), rhs=wth(3, 0, w), start=(c == 0), stop=(c == 3)),
                     [CMb, WTb[3]], [PSb[3]])
            TS(wt(9, w), PS[2][:, 0:w], 1.0 / 512, None, ALU.mult, None, [PSb[2]], [WTb[9]])
            TT(wt(10, w), wt(9, w), wt(9, w), ALU.mult, [WTb[9]], [WTb[10]])
            STT(wt(10, w), PS[3][:, 0:w], 1.0 / 512, wt(10, w), ALU.mult, ALU.subtract, [PSb[3], WTb[10]], [WTb[10]])
            A(wt(10, w), wt(10, w), AF.Sqrt, [WTb[10]], [WTb[10]], bias=EPS)
            RCP(wt(10, w), wt(10, w), [WTb[10]], [WTb[10]])
            for c in range(4):
                TT(wt(5 + c, w), wt(5 + c, w), wt(9, w), ALU.subtract, [WTb[5 + c], WTb[9]], [WTb[5 + c]])
                TT(wt(5 + c, w), wt(5 + c, w), wt(10, w), ALU.mult, [WTb[5 + c], WTb[10]], [WTb[5 + c]])
                A(ct(CO_T[c], cs, cs + w), wt(5 + c, w), AF.Silu, [WTb[5 + c], VECb], cb(CO_T[c], j),
                  scale=VEC[:, ve + 4 + c: ve + 5 + c], bias=VEC[:, ve + 8 + c: ve + 9 + c])
        chk("conv")
        TS(DER[:, 40:41], VEC[:, ve + 12: ve + 13], 0.125, None, ALU.mult, None, [VECb], [DERb])
        CP(DER[:, 41:42], VEC[:, ve + 13: ve + 14], [VECb], [DERb])
        MSET(ct(MISC_T, 0, CTW), 0.0, [b for jx in range(9) for b in cb(MISC_T, jx)], eng=DVE)
        for kt in range(34 if "noones" not in KDBG else 0):
            MSET(CT[:, V_T * CTW + kt * 256: V_T * CTW + (kt + 1) * 256].rearrange("p (h n) -> p h n", h=2)[:, :, 64:128], 1.0, [VTb[kt]], eng=DVE)
        for j in range(int(os.environ.get("PBN", "9"))):
            cs, xc, w = BLKS[j]
            is_ctx = (j == 8)
            hb3, hbb = build_h(i, j, 1)
            if "nochunks" in KDBG:
                continue
            if not is_ctx and "noropeld" not in KDBG:
                LD(SP, COS_AP[:, 0:w], rope_cs[:, xc:xc + w], [], [RINGb[4]])
                LD(SP, SIN_AP[:, 0:w], rope_cs[:, NL + xc: NL + xc + w], [], [RINGb[4]])
            chunks = []
            if (not is_ctx) or ctx_full:
                chunks += [("q", cq) for cq in range(4)]
            chunks += [("k", 0)]
            for kind, cq in chunks:
                if kind == "q":
                    lw = [(ring3(2, 8)[:, k, cq * 128:(cq + 1) * 128], hb3[:, k, 0:w]) for k in range(8)]
                    dst, dstb, gcol = ct(Q_T[cq], cs, cs + w), cb(Q_T[cq], j), 40
                    rb = [RINGb[2]]
                else:
                    lw = [(ring3(3, 8)[:, k, 0:128], hb3[:, k, 0:w]) for k in range(8)]
                    dst, dstb, gcol = ct(K_T, cs, cs + w), cb(K_T, j), 41
                    rb = [RINGb[3]]
                MM(PS[0][:, 0:w], lw, rb + hbb, [PSb[0]])
                CP(wt(5, w), PS[0][:, 0:w], [PSb[0]], [WTb[5]])
                A(wth(3, 0, w), wt(5, w), AF.Square, [WTb[5]], [WTb[3]])
                MM(PS[1][:, 0:w], [(cm(BLK1), wth(3, 0, w))], [CMb, WTb[3]], [PSb[1]])
                A(wt(6, w), PS[1][:, 0:w], AF.Sqrt, [PSb[1]], [WTb[6]], scale=1.0 / 64, bias=EPS)
                RCP(wt(6, w), wt(6, w), [WTb[6]], [WTb[6]])
                if "noqk" in KDBG:
                    continue
                if is_ctx or "norope" in KDBG:
                    STT(dst, wt(5, w), DER[:, gcol:gcol + 1], wt(6, w), ALU.mult, ALU.mult, [WTb[5], WTb[6], DERb], dstb)
                else:
                    STT(wt(5, w), wt(5, w), DER[:, gcol:gcol + 1], wt(6, w), ALU.mult, ALU.mult, [WTb[5], WTb[6], DERb], [WTb[5]])
                    A(wth(4, 0, w), wt(5, w), AF.Copy, [WTb[5]], [WTb[4]])
                    MM(PS[2][:, 0:w], [(cm(ROT), wth(4, 0, w))], [CMb, WTb[4]], [PSb[2]])
                    TT(wt(5, w), wt(5, w), COS_AP[:, 0:w], ALU.mult, [WTb[5], RINGb[4]], [WTb[5]])
                    TT(wt(6, w), PS[2][:, 0:w], SIN_AP[:, 0:w], ALU.mult, [PSb[2], RINGb[4]], [WTb[6]])
                    TT(dst, wt(5, w), wt(6, w), ALU.add, [WTb[5], WTb[6]], dstb)
                if kind == "k":
                    CP(ctp(MISC_T, cs, cs + w, 64, 128), ctp(K_T, cs, cs + w, 64, 128), [CTb[K_T][j][1]], [CTb[MISC_T][j][1]])
                    MSET(ctp(K_T, cs, cs + w, 64, 128), 0.0, [CTb[K_T][j][1]], eng=DVE)
            for tt in range(w // 128 if "nov" not in KDBG else 0):
                kt = (32 + tt) if is_ctx else (j * 4 + tt)
                pb = 4 + (tt % 2)
                MM(PS[pb][:, 0:128], [(hb3[:, k, tt * 128:(tt + 1) * 128], ring3(3, 8)[:, k, 128:256]) for k in range(8)],
                   [RINGb[3]] + hbb, [PSb[pb]])
                CP(vtok(kt, 0, 256).rearrange("p (h n) -> p h n", h=2)[:, :, 0:64],
                   PS[pb][:, 0:128].rearrange("p (h n) -> p h n", h=2), [PSb[pb]], [VTb[kt]])
        chk("passB")
        load_wout(2, 512)
        items = []
        for j in blocks_for(ctx_full):
            is_ctx = (j == 8)
            kts = [32, 33] if is_ctx else [32, 33] + list(range(32))
            prs = [(kts[n], kts[n + 1]) for n in range(0, len(kts), 2)]
            for cq in range(4):
                for half in range(2):
                    for n, pr in enumerate(prs):
                        items.append((j, cq, half, pr, n == 0, n == len(prs) - 1))
        LA = 3
        nit = len(items)
        for step in range(nit + LA):
            if step < nit:
                j, cq, half, pr, first, last = items[step]
                cs, xc, w = BLKS[j]
                sd = step % 3
                sbufs = [PSb[2 * sd], PSb[2 * sd + 1]]
                pairs_mm = []
                rd = list(cb(Q_T[cq], j))
                for n, kt in enumerate(pr):
                    kcol = (C0 + (kt - 32) * 128) if kt >= 32 else (L0 + kt * 128)
                    kj = 8 if kt >= 32 else kt // 4
                    pairs_mm.append((PSD[sd][:, n * w:(n + 1) * w], ct(KP_T[half], kcol, kcol + 128)))
                    rd += cb(KP_T[half], kj)
                qap = ct(Q_T[cq], cs, cs + w)

                def smm(e, pairs_mm=pairs_mm, qap=qap):
                    last_i = None
                    for (o, kap) in pairs_mm:
                        last_i = e.matmul(o, lhsT=kap, rhs=qap, start=True, stop=True)
                    return last_i
                P.op(PE, smm, rd, sbufs)
                pk = step % 4
                A(wth(pk, 0, 2 * w), PSD[sd][:, 0:2 * w], AF.Exp, sbufs, [WTb[pk]])
            if step >= LA:
                j, cq, half, pr, first, last = items[step - LA]
                cs, xc, w = BLKS[j]
                pk = (step - LA) % 4
                ob = 6 + half

                def pvmm(e, ob=ob, w=w, pr=pr, half=half, pk=pk, first=first, last=last):
                    last_i = None
                    for n, kt in enumerate(pr):
                        last_i = e.matmul(PS[ob][:, 0:w], lhsT=vtok(kt, half * 128, half * 128 + 128), rhs=wth(pk, n * w, (n + 1) * w),
                                          start=(first and n == 0), stop=(last and n == len(pr) - 1))
                    return last_i
                P.op(PE, pvmm, [VTb[kt] for kt in pr] + [WTb[pk]], [PSb[ob]])
                if last:
                    RCP(WT[64:128, 5 * 512: 5 * 512 + w], PS[ob][64:128, 0:w], [PSb[ob]], [WTb[5]])
                    TT(ctp(Q_T[cq], cs, cs + w, half * 64, half * 64 + 64), PS[ob][0:64, 0:w], WT[64:128, 5 * 512: 5 * 512 + w], ALU.mult,
                       [PSb[ob], WTb[5]], [CTb[Q_T[cq]][j][half]])
        chk("attn")
        outproj_residual(i, CO_T + Q_T, [1, 2], ctx_full, 2)


    def odd_mixer(i):
        jj = i // 2
        ctx_full = i < 2
        vo = V_OD + jj * 16
        wi = w3(od_w_in[jj])
        PIN_T = [0, 1, 2, 3]
        PO_T = [4, 5, 6, 7]
        SC_T = [0, 1, 2, 3]
        GB_T = [8, 9, 10, 11]
        zero_pads(PIN_T)
        load_w_slot(3, wi[:, :, 1536:2048])
        load_w_slot(0, wi[:, :, 0:512])
        load_w_slot(1, wi[:, :, 512:1024])
        load_w_slot(2, wi[:, :, 1024:1536])
        PWT = ring(4, 20 * 128, 24 * 128)
        PWTb = RINGb[4]
        LD(POOL, PWT.rearrange("p (g n) -> p g n", g=4), od_pool_w[jj].rearrange("g p n -> p g n"), [], [PWTb])
        WIN = [2, 4, 8, 16]
        tapmat = {}
        tcount = 0
        for gi, wv in enumerate(WIN):
            for val in (1.0 / wv, 1.0 / wv - 1.0):
                TS(ring(4, tcount * 128, (tcount + 1) * 128), cm(IDB), float(val), None, ALU.mult, None, [CMb], [RINGb[4]])
                tapmat[(gi, val == 1.0 / wv)] = tcount
                tcount += 1
        for c in range(4):
            for t in range(3):
                TS(ring(4, (8 + c * 3 + t) * 128, (9 + c * 3 + t) * 128), cm(IDB), VEC[:, vo + c * 3 + t: vo + c * 3 + t + 1], None,
                   ALU.mult, None, [CMb, VECb], [RINGb[4]])
        for j in blocks_for(ctx_full):
            cs, xc, w = BLKS[j]
            hb3, hbb = build_h(i, j, 1)
            for c in range(4):
                MM(PS[c % 2][:, 0:w], [(ring3(3, 8)[:, k, c * 128:(c + 1) * 128], hb3[:, k, 0:w]) for k in range(8)],
                   [RINGb[3]] + hbb, [PSb[c % 2]])
                A(ct(PIN_T[c], cs, cs + w), PS[c % 2][:, 0:w], AF.Copy, [PSb[c % 2]], cb(PIN_T[c], j))
        wo = w3(od_w_out[jj])
        load_w_slot(5, wo[:, :, 0:512])
        load_w_slot(3, wo[:, :, 512:1024])
        for j in blocks_for(ctx_full):
            cs, xc, w = BLKS[j]
            seq0, seqn = (C0, NCX) if j == 8 else (L0, NL)
            for gi, wv in enumerate(WIN):
                taps = list(range(-wv // 2, wv // 2))
                nb = [b for jn in ((8,) if j == 8 else (max(j - 1, 0), j, min(j + 1, 7))) for b in cb(PIN_T[gi], jn)]
                MM(PS[gi % 2][:, 0:w], [(ring(4, tapmat[(gi, t != 0)] * 128, (tapmat[(gi, t != 0)] + 1) * 128),
                                         ct(PIN_T[gi], cs + t, cs + t + w)) for t in taps], [RINGb[4]] + nb, [PSb[gi % 2]])
                dk = 5 + (gi % 2)
                CP(wth(dk, 0, w), PS[gi % 2][:, 0:w], [PSb[gi % 2]], [WTb[dk]])
                h2 = wv // 2
                fix = []
                if cs == seq0:
                    fix.append((0, h2, gi * 16))
                if cs + w == seq0 + seqn:
                    fix.append((w - (h2 - 1), h2 - 1, gi * 16 + 8))
                for (o, n, fc) in fix:
                    if n <= 0:
                        continue
                    TT(wt(7, 16)[:, 0:n], wth(dk, o, o + n), ct(PIN_T[gi], cs + o, cs + o + n), ALU.add, [WTb[dk]] + cb(PIN_T[gi], j), [WTb[7]])
                    TT(wt(7, 16)[:, 0:n], wt(7, 16)[:, 0:n], PFIX[:, fc:fc + n], ALU.mult, [WTb[7], PFIXb], [WTb[7]])
                    TT(wth(dk, o, o + n), wt(7, 16)[:, 0:n], ct(PIN_T[gi], cs + o, cs + o + n), ALU.subtract, [WTb[7]] + cb(PIN_T[gi], j), [WTb[dk]])
                MM(PS[2 + gi % 2][:, 0:w], [(PWT[:, gi * 128:(gi + 1) * 128], wth(dk, 0, w))], [PWTb, WTb[dk]], [PSb[2 + gi % 2]])
                A(ct(PO_T[gi], cs, cs + w), PS[2 + gi % 2][:, 0:w], AF.Identity, [PSb[2 + gi % 2], VECb], cb(PO_T[gi], j),
                  scale=VEC[:, vo + 12 + gi: vo + 13 + gi])
        for j in blocks_for(ctx_full):
            cs, xc, w = BLKS[j]
            hb3, hbb = build_h(i, j, 1)
            for c in range(4):
                MM(PS[0][:, 0:w], [(ring3(0, 8)[:, k, c * 128:(c + 1) * 128], hb3[:, k, 0:w]) for k in range(8)], [RINGb[0]] + hbb, [PSb[0]])
                MM(PS[1][:, 0:w], [(ring3(2, 8)[:, k, c * 128:(c + 1) * 128], hb3[:, k, 0:w]) for k in range(8)], [RINGb[2]] + hbb, [PSb[1]])
                MM(PS[2][:, 0:w], [(ring3(1, 8)[:, k, c * 128:(c + 1) * 128], hb3[:, k, 0:w]) for k in range(8)], [RINGb[1]] + hbb, [PSb[2]])
                A(wt(5, w), PS[0][:, 0:w], AF.Copy, [PSb[0]], [WTb[5]])
                TT(ct(SC_T[c], cs, cs + w), PS[1][:, 0:w], wt(5, w), ALU.mult, [PSb[1], WTb[5]], cb(SC_T[c], j))
                A(ct(GB_T[c], cs, cs + w), PS[2][:, 0:w], AF.Copy, [PSb[2]], cb(GB_T[c], j))
        for j in blocks_for(ctx_full):
            cs, xc, w = BLKS[j]
            for c in range(4):
                nb = [b for jn in ((8,) if j == 8 else (max(j - 1, 0), j, min(j + 1, 7))) for b in cb(SC_T[c], jn)]
                MM(PS[c % 2][:, 0:w], [(ring(4, (8 + c * 3 + t) * 128, (9 + c * 3 + t) * 128), ct(SC_T[c], cs + t - 1, cs + t - 1 + w)) for t in range(3)],
                   [RINGb[4]] + nb, [PSb[c % 2]])
                TT(ct(GB_T[c], cs, cs + w), PS[c % 2][:, 0:w], ct(GB_T[c], cs, cs + w), ALU.mult, [PSb[c % 2]] + cb(GB_T[c], j), cb(GB_T[c], j))
        outproj_residual(i, GB_T + PO_T, [5, 3], ctx_full, 2)

    AFF_T = 0
    AFW_T = 2
    XE_T = [4, 5]
    ACT_T = 6

    def ctf(i, a, b, p=16):
        base = (i * CTW) // 2
        return CTF[0:p, base + a: base + b]

    def ctfb(i):
        return [b for t in (i, i + 1) for j in range(9) for b in cb(t, j)]

    TVb, TIb, TFb = [WTb[0], WTb[1]], [WTb[2], WTb[3]], [WTb[4], WTb[5]]

    def moe(i, last_layer):
        ctx_full = i < 2
        ntile = 5 if ctx_full else 4
        LD(SP, WR[:, :].rearrange("p (c n) -> p c n", c=8), w_router[i].rearrange("(c p) n -> p c n", p=128), [], [WRb])
        wr3 = WR[:, :].rearrange("p (c n) -> p c n", c=8)
        affb, afwb = ctfb(AFF_T), ctfb(AFW_T)
        for j in blocks_for(ctx_full):
            cs, xc, w = BLKS[j]
            v = 1 if j == 8 else 0
            hb_i = hbrr[0] % 2
            hbrr[0] += 1
            hb3 = HB[hb_i].rearrange("p (c n) -> p c n", c=8)
            hbb = HBb[hb_i]
            for c in range(8):
                k = next_xs()
                LD(SP, wt(k, w), XA[c * 128:(c + 1) * 128, xc:xc + w], [XAb[c][j]], [WTb[k]])
                TT(wt(k, w), wt(k, w), RSTD[:, xc:xc + w], ALU.mult, [WTb[k], RSTDb[j]], [WTb[k]])
                A(wt(k, w), wt(k, w), AF.Identity, [WTb[k], DERb, MODSb], [WTb[k]], scale=G2(c, v), bias=mod(i, 3, c, v))
                P.op(PE, lambda e, c=c, k=k, w=w: e.matmul(PS[0][0:16, 0:w], lhsT=wr3[:, c, :], rhs=wt(k, w), start=(c == 0), stop=(c == 7)),
                     [WRb, WTb[k]], [PSb[0]])
                CP(hb3[:, c, 0:w], wt(k, w), [WTb[k]] + hbb, hbb, eng=POOL)
            A(wt(5, w)[0:16, :], PS[0][0:16, 0:w], AF.Exp, [PSb[0]], [WTb[5]])
            MM(PS[1][0:16, 0:w], [(ONEF[:, :], wt(5, w)[0:16, :])], [ONEFb, WTb[5]], [PSb[1]])
            RCP(wt(6, w)[0:16, :], PS[1][0:16, 0:w], [PSb[1]], [WTb[6]])
            TT(ctf(AFF_T, xc, xc + w), wt(5, w)[0:16, :], wt(6, w)[0:16, :], ALU.mult, [WTb[5], WTb[6]], affb)
            for tt in range(w // 128):
                pb = 2 + (tt % 2)
                TRS([(PSH[pb][:, c * 128:(c + 1) * 128], hb3[:, c, tt * 128:(tt + 1) * 128], cm(IDB)) for c in range(8)],
                    [CMb] + hbb, [PSb[pb]])
                gk = 3 + (tt % 2)
                if tt % 2 == 0:
                    CP(wth(gk), PSH[pb][:, :], [PSb[pb]], [WTb[gk]])
                else:
                    A(wth(gk), PSH[pb][:, :], AF.Copy, [PSb[pb]], [WTb[gk]])
                trow = xc + tt * 128
                LD(SP, HFTOK[trow:trow + 128, :], wth(gk), [WTb[gk]], [HFb[trow // 128]])
        chk("moe_prep")
        TV = WT[0:16, 0:NSLOT]
        TI = WTU[0:16, 1024:1024 + NSLOT]
        TF = WT[0:16, 2048:2048 + NSLOT]
        for (col0, ncol, nround, s0) in ([(0, NL, CAP_L // 8, 0)] + ([(NL, NCX, CAP_C // 8, CAP_L)] if ctx_full else [])):
            for r in range(nround):
                src = ctf(AFF_T, col0, col0 + ncol) if r == 0 else ctf(AFW_T, col0, col0 + ncol)
                srcb = affb if r == 0 else afwb
                sl = slice(s0 + 8 * r, s0 + 8 * r + 8)
                P.op(DVE, lambda e, sl=sl, src=src: e.max(out=TV[:, sl], in_=src), srcb, TVb)
                P.op(DVE, lambda e, sl=sl, src=src: e.max_index(out=TI[:, sl], in_max=TV[:, sl], in_values=src), srcb + TVb, TIb)
                if r < nround - 1:
                    P.op(DVE, lambda e, sl=sl, src=src, col0=col0, ncol=ncol: e.match_replace(out=ctf(AFW_T, col0, col0 + ncol), in_to_replace=TV[:, sl],
                                                                                              in_values=src, imm_value=-1.0), srcb + TVb, afwb)
        CP(TF, TI, TIb, TFb)
        if ctx_full:
            TS(TF[:, CAP_L:NSLOT], TF[:, CAP_L:NSLOT], float(NL), None, ALU.add, None, TFb, TFb)
        for t in range(ntile):
            n = 128 if t < 4 else CAP_C
            TR(PS[4][0:n, 0:16], TV[:, t * 128: t * 128 + n], IDF[0:16, 0:16], TVb + [IDFb], [PSb[4]])
            CP(AFFT[0:n, t * 16:(t + 1) * 16], PS[4][0:n, 0:16], [PSb[4]], [AFFTb])
            TR(PS[5][0:n, 0:16], TF[:, t * 128: t * 128 + n], IDF[0:16, 0:16], TFb + [IDFb], [PSb[5]])
            CP(IDXT[0:n, t * 16:(t + 1) * 16], PS[5][0:n, 0:16], [PSb[5]], [IDXTb])
        chk("moe_topk")
        xe3 = [CT[:, XE_T[k] * CTW: XE_T[k] * CTW + 8 * NSLOT].rearrange("p (c s) -> p c s", c=8) for k in range(2)]
        xeb = [[b for j in range(9) for b in cb(XE_T[k], j)] for k in range(2)]
        act3 = CT[:, ACT_T * CTW: ACT_T * CTW + 16 * NSLOT].rearrange("p (f s) -> p f s", f=16)
        actb = [b for t in (ACT_T, ACT_T + 1) for j in range(9) for b in cb(t, j)]
        GTK = [9, 10, 11, 0, 1]

        pieces = []
        for e in range(NEXP):
            for q in range(4):
                pieces.append(("g", e, q))
                pieces.append(("u", e, q))
            for dq in range(4):
                pieces.append(("d", e, dq))
        slot_of = {}
        loaded = [0]

        def ensure_loaded(upto):
            while loaded[0] <= min(upto, len(pieces) - 1):
                kind, e, q = pieces[loaded[0]]
                s = pcount[0] % NRING
                pcount[0] += 1
                slot_of[loaded[0]] = s
                if kind == "g":
                    LD(POOL, ring3(s, 8), w3(w_gate[mmap[i], e])[:, :, q * 512:(q + 1) * 512], [], [RINGb[s]])
                elif kind == "u":
                    LD(POOL, ring3(s, 8), w3(w_up[mmap[i], e])[:, :, q * 512:(q + 1) * 512], [], [RINGb[s]])
                else:
                    LD(POOL, ring3(s, 16), w_down[mmap[i], e].rearrange("(f p) d -> p f d", p=128)[:, :, q * 256:(q + 1) * 256], [], [RINGb[s]])
                loaded[0] += 1

        def gather(e):
            for t in range(ntile):
                n = 128 if t < 4 else CAP_C
                gk = GTK[t]
                P.dma(POOL, lambda eng, gk=gk, n=n, t=t, e=e: eng.indirect_dma_start(
                    out=WTH[0:n, gk * 1024:(gk + 1) * 1024], out_offset=None, in_=HFTOK[:, :],
                    in_offset=bass.IndirectOffsetOnAxis(ap=IDXT[0:n, t * 16 + e: t * 16 + e + 1], axis=0)),
                    [IDXTb] + HFb, [WTb[gk]])

        def xe_transposes(e):
            k = e % 2
            for t in range(ntile):
                n = 128 if t < 4 else CAP_C
                gk = GTK[t]
                TRS([(PSH[6][:, c * 128: c * 128 + n], WTH[0:n, gk * 1024 + c * 128: gk * 1024 + (c + 1) * 128], CM[0:n, 0:n]) for c in range(8)],
                    [CMb, WTb[gk]], [PSb[6]])
                src = PSH[6][:, :].rearrange("p (c s) -> p c s", c=8)[:, :, 0:n]
                if t % 2 == 0:
                    CP(xe3[k][:, :, t * 128: t * 128 + n], src, [PSb[6]], xeb[k])
                else:
                    P.op(ACT, lambda eng, k=k, t=t, n=n, src=src: eng.activation(out=xe3[k][:, :, t * 128: t * 128 + n], in_=src, func=AF.Copy),
                         [PSb[6]], xeb[k])

        PYb = [PSb[5], PSb[7]]
        YSb = [P.buf("ys%d_%d" % (i, k)) for k in range(16)]
        gather(0)
        ensure_loaded(4)
        xe_transposes(0)
        pidx = 0
        ysr = [0]
        for e in range(NEXP):
            k = e % 2
            if e + 1 < NEXP:
                gather(e + 1)
            for q in range(4):
                ensure_loaded(pidx + 5)
                sg_, su_ = slot_of[pidx], slot_of[pidx + 1]
                pidx += 2
                for fl in range(4):
                    fc = q * 4 + fl
                    pg, pu = (fc % 2), 2 + (fc % 2)
                    MM(PS[pg][:, 0:512], [(ring3(sg_, 8)[:, c, fl * 128:(fl + 1) * 128], xe3[k][:, c, 0:512]) for c in range(8)],
                       [RINGb[sg_]] + xeb[k], [PSb[pg]])
                    MM(PS[pu][:, 0:512], [(ring3(su_, 8)[:, c, fl * 128:(fl + 1) * 128], xe3[k][:, c, 0:512]) for c in range(8)],
                       [RINGb[su_]] + xeb[k], [PSb[pu]])
                    if ctx_full:
                        co = (fc % 8) * 64
                        MM(PS[4][:, co:co + 32], [(ring3(sg_, 8)[:, c, fl * 128:(fl + 1) * 128], xe3[k][:, c, 512:544]) for c in range(8)],
                           [RINGb[sg_]] + xeb[k], [PSb[4]])
                        MM(PS[4][:, co + 32:co + 64], [(ring3(su_, 8)[:, c, fl * 128:(fl + 1) * 128], xe3[k][:, c, 512:544]) for c in range(8)],
                           [RINGb[su_]] + xeb[k], [PSb[4]])
                    sk = 5 + (fc % 2)
                    A(wth(sk, 0, 512), PS[pg][:, 0:512], AF.Silu, [PSb[pg]], [WTb[sk]])
                    TT(act3[:, fc, 0:512], wth(sk, 0, 512), PS[pu][:, 0:512], ALU.mult, [WTb[sk], PSb[pu]], actb)
                    if ctx_full and fc % 8 == 7:
                        p4 = PS[4][:, :].rearrange("p (f g s) -> p f g s", f=8, g=2)
                        s3 = wth(7, 0, 256).rearrange("p (f s) -> p f s", f=8)
                        A(s3, p4[:, :, 0, :], AF.Silu, [PSb[4]], [WTb[7]])
                        TT(act3[:, fc - 7: fc + 1, 512:544], s3, p4[:, :, 1, :], ALU.mult, [WTb[7], PSb[4]], actb)
            if e + 1 < NEXP:
                xe_transposes(e + 1)
            for dq in range(4):
                ensure_loaded(pidx + 5)
                sd_ = slot_of[pidx]
                pidx += 1
                d3 = ring3(sd_, 16)
                ys_list = []
                for t in range(ntile):
                    n = 128 if t < 4 else CAP_C
                    yb = ysr[0] % 2
                    ysr[0] += 1
                    pyb, pyo = 5 + yb * 2, 0
                    py = PS[pyb][0:n, pyo:pyo + 256]
                    MM(py, [(act3[:, fc, t * 128: t * 128 + n], d3[:, fc, :]) for fc in range(16)], [RINGb[sd_]] + actb, [PYb[yb]])
                    yk = (ysr[0] - 1) % 16
                    ybase = ((8 + yk // 8) * CTW) // 2 + (yk % 8) * 256
                    ysl = CTF[0:n, ybase: ybase + 256]
                    ysb = YSb[yk]
                    if yb == 0:
                        A(ysl, py, AF.Identity, [PYb[yb], AFFTb], [ysb], scale=AFFT[0:n, t * 16 + e: t * 16 + e + 1])
                    else:
                        TS(ysl, py, AFFT[0:n, t * 16 + e: t * 16 + e + 1], None, ALU.mult, None, [PYb[yb], AFFTb], [ysb])
                    ys_list.append((ysl, ysb, n, t))
                fns = []
                for (ysl, ysb, n, t) in ys_list:
                    fns.append(lambda eng, ysl=ysl, n=n, t=t, e=e, dq=dq: eng.indirect_dma_start(
                        out=MOQ[dq][:, :], out_offset=bass.IndirectOffsetOnAxis(ap=IDXT[0:n, t * 16 + e: t * 16 + e + 1], axis=0),
                        in_=ysl, in_offset=None, compute_op=ALU.add))
                P.dma(POOL, fns, [IDXTb, MOb[dq]] + [ysb for (_, ysb, _, _) in ys_list], [MOb[dq]])
        chk("moe_exp")
        for j in blocks_for(ctx_full):
            cs, xc, w = BLKS[j]
            v = 1 if j == 8 else 0
            pend = None
            for half in range(2):
                for tt in range(w // 128):
                    gk = 9 + (tt % 2)
                    trow = xc + tt * 128
                    P.dma(SP, [lambda e_, gk=gk, trow=trow, q=q: e_.dma_start(out=wt(gk, 512)[:, (q % 2) * 256:(q % 2 + 1) * 256], in_=MOQ[q][trow:trow + 128, :])
                               for q in (2 * half, 2 * half + 1)], [MOb[2 * half], MOb[2 * half + 1]], [WTb[gk]])
                    for cl in range(4):
                        TR(PS[cl][:, tt * 128:(tt + 1) * 128], wt(gk, 512)[:, cl * 128:(cl + 1) * 128], IDF[:, :], [WTb[gk], IDFb], [PSb[cl]])
                for cl in range(4):
                    c = half * 4 + cl
                    kx = next_xs()
                    LD(SP, wt(kx, w), XA[c * 128:(c + 1) * 128, xc:xc + w], [XAb[c][j]], [WTb[kx]])
                    STT(wt(kx, w), PS[cl][:, 0:w], mod(i, 5, c, v), wt(kx, w), ALU.mult, ALU.add, [PSb[cl], WTb[kx], MODSb], [WTb[kx]])
                    if last_layer:
                        LD(SP, outT[c * 128:(c + 1) * 128, xc:xc + w], wt(kx, w), [WTb[kx]], [OUTb])
                    else:
                        LD(SP, XA[c * 128:(c + 1) * 128, xc:xc + w], wt(kx, w), [WTb[kx]], [XAb[c][j]])
                        kk = stat_chunk(j, c, wt(kx, w), WTb[kx], w)
                        if pend is not None:
                            stat_mm(j, *pend, w)
                        pend = (c, kk)
            if not last_layer:
                stat_mm(j, *pend, w)
                stat_fin(j, w)
        if not last_layer:
            zero_mo(True)

    try:
        chk("pre")
        for i in range(start_layer, n_layers):
            der_setup(i)
            if i % 2 == 0:
                even_mixer(i)
            else:
                odd_mixer(i)
            if stop == "mix%d" % i:
                break
            moe(i, i == DEPTH - 1)
    except _Stop:
        pass
    if debug:
        for c in range(8):
            LD(SP, dbg["xa"][c * 128:(c + 1) * 128, :], XA[c * 128:(c + 1) * 128, :], [b for b in XAb[c]], [OUTb])
    P.finish()
    return nc, P


def _rope_tables():
    n_freq = 16
    inv = (10000.0 ** (-np.arange(n_freq, dtype=np.float32) / n_freq)).astype(np.float32)
    t = np.arange(NL)
    row = (t // 64).astype(np.float32)
    col = (t % 64).astype(np.float32)
    ang_r = row[:, None] * inv
    ang_c = col[:, None] * inv
    cos = np.zeros((64, NL), np.float32)
    sin = np.zeros((64, NL), np.float32)
    cos[0:16] = np.cos(ang_r).T
    cos[16:32] = np.cos(ang_r).T
    cos[32:48] = np.cos(ang_c).T
    cos[48:64] = np.cos(ang_c).T
    sin[0:16] = np.sin(ang_r).T
    sin[16:32] = np.sin(ang_r).T
    sin[32:48] = np.sin(ang_c).T
    sin[48:64] = np.sin(ang_c).T
    cos = np.concatenate([cos, cos], 0)
    sin = np.concatenate([sin, sin], 0)
    return np.concatenate([cos, sin], 1).astype(np.float32)


def _const_mats():
    m = np.zeros((128, 5 * 128), np.float32)
    m[:, 0:128] = np.eye(128)
    m[:, 128:256] = 1.0
    blk = np.zeros((128, 128), np.float32)
    blk[0:64, 0:64] = 1.0
    blk[64:128, 64:128] = 1.0
    m[:, 256:384] = blk
    rot = np.zeros((128, 128), np.float32)
    for base in range(0, 128, 32):
        for d in range(16):
            rot[base + 16 + d, base + d] = -1.0
            rot[base + d, base + 16 + d] = 1.0
    m[:, 384:512] = rot
    return m


def _pool_fix():
    f = np.ones((128, 64), np.float32)
    for gi, w in enumerate((2, 4, 8, 16)):
        h = w // 2
        for t in range(h):
            f[:, gi * 16 + t] = w / float(t + h)
        for m_ in range(h - 1):
            f[:, gi * 16 + 8 + m_] = w / float((h - 1 - m_) + h)
    return f


def _pack_vecs(inp, b):
    v = np.zeros((128, NV), np.float32)

    def col(a):
        a = np.asarray(a, np.float32)
        return a.reshape(-1, 128).T

    for i in range(DEPTH):
        v[:, V_NMG + i * 8: V_NMG + (i + 1) * 8] = col(inp["norm_mix_g"][i])
        v[:, V_NFG + i * 8: V_NFG + (i + 1) * 8] = col(inp["norm_ffn_g"][i])
        v[:, V_BMOD + i * 48: V_BMOD + (i + 1) * 48] = col(inp["b_mod"][i])
    for j in range(2):
        o = V_EV + j * 138
        v[:, o: o + 4] = col(inp["ev_conv_b"][j])
        v[:, o + 4: o + 8] = col(inp["ev_ln_g"][j])
        v[:, o + 8: o + 12] = col(inp["ev_ln_b"][j])
        v[:, o + 12] = np.tile(np.asarray(inp["ev_q_norm_g"][j], np.float32), 2)
        v[:, o + 13] = np.tile(np.asarray(inp["ev_k_norm_g"][j], np.float32), 2)
        cw = np.asarray(inp["ev_conv_w"][j], np.float32)
        for c in range(4):
            v[:, o + 14 + c * 31: o + 14 + (c + 1) * 31] = cw[:, c * 128:(c + 1) * 128].T
        o2 = V_OD + j * 16
        ow = np.asarray(inp["od_conv_w"][j], np.float32)
        for c in range(4):
            v[:, o2 + c * 3: o2 + (c + 1) * 3] = ow[:, c * 128:(c + 1) * 128].T
        v[:, o2 + 12: o2 + 16] = col(inp["od_pool_scale"][j])
    cc = np.stack([col(inp["c"][b]), col(inp["c_ctx"])], axis=-1)
    v[:, V_CC: V_CC + 16] = cc.reshape(128, 16)
    return v


_CACHE = {}


def kernel(**inputs):
    inp = {k: np.asarray(v) for k, v in inputs.items()}
    n = 8
    if "nc" not in _CACHE:
        _CACHE["nc"] = build_program()[0]
    nc = _CACHE["nc"]
    rope = _rope_tables()
    cm = _const_mats()
    pf = _pool_fix()
    zeros = np.zeros((NT, D), np.float32)
    shared = {k: np.ascontiguousarray(inp[k], dtype=np.float32) for k in
              ("w_mod", "ev_w_in", "ev_w_out", "od_w_in", "od_w_out", "od_pool_w", "w_router", "w_gate", "w_up", "w_down")}
    in_maps = []
    for b in range(n):
        m = dict(shared)
        m["xT"] = np.ascontiguousarray(inp["x"][b].T, dtype=np.float32)
        m["ctxT"] = np.ascontiguousarray(inp["ctx"][b].T, dtype=np.float32)
        m["vecs"] = _pack_vecs(inp, b)
        m["cmats"] = cm
        m["rope_cs"] = rope
        m["pool_fix"] = pf
        m["zeros_d"] = zeros
        in_maps.append(m)
    res = run_bass_kernel_spmd(nc, in_maps, core_ids=list(range(n)))
    out = np.stack([np.ascontiguousarray(res.results[b]["outT"].T) for b in range(n)], axis=0)
    return out.astype(np.float32)
```

```python
import os
import numpy as np
import concourse.bass as bass
import concourse.mybir as mybir
from concourse.bass_utils import run_bass_kernel_spmd

F32 = mybir.dt.float32
BF16 = mybir.dt.bfloat16
I32 = mybir.dt.int32
U32 = mybir.dt.uint32
ALU = mybir.AluOpType
AF = mybir.ActivationFunctionType

KDBG = set(os.environ.get("KDBG", "").split(","))
PE, ACT, DVE, POOL, SP = "pe", "act", "dve", "pool", "sp"
ENGS = [PE, ACT, DVE, POOL, SP]
STRICT_SAME = {PE: False, ACT: True, DVE: True, POOL: True, SP: False}


class _Stop(Exception):
    pass


class Buf:
    __slots__ = ("name", "w", "r")

    def __init__(self, name):
        self.name = name
        self.w = []
        self.r = []


class Prog:
    def __init__(self, nc):
        self.nc = nc
        self.q = {e: [] for e in ENGS}
        self.sems = {}
        self.cnt = {}
        self.waited = {e: {} for e in ENGS}
        for e in (PE, ACT, DVE, POOL):
            self._mksem("e_" + e)
        nd = {SP: 16, ACT: 4, POOL: 16}
        self.dma_pool = {}
        self.dma_rr = {}
        for e, n in nd.items():
            self.dma_pool[e] = [self._mksem("d_%s%d" % (e, i)) for i in range(n)]
            self.dma_rr[e] = 0
        self.n_ops = 0

    def _mksem(self, key):
        self.sems[key] = self.nc.alloc_semaphore(key)
        self.cnt[key] = 0
        return key

    def buf(self, name):
        return Buf(name)

    def _deps(self, eng, reads, writes, same_ok=False, stream=False):
        need = {}

        def add(t):
            k, v, e = t[0], t[1], t[2]
            if e == eng and (same_ok or not STRICT_SAME[eng] or (stream and len(t) > 3 and t[3])):
                return
            if need.get(k, 0) < v:
                need[k] = v

        for b in reads:
            for t in b.w:
                add(t)
        for b in writes:
            for t in b.w:
                add(t)
            for t in b.r:
                add(t)
        out = []
        wd = self.waited[eng]
        for k, v in need.items():
            if wd.get(k, 0) < v:
                wd[k] = v
                out.append((k, v))
        return out

    def _commit(self, toks, reads, writes):
        for b in reads:
            b.r.extend(toks)
            if len(b.r) > 64:
                mx = {}
                for t in b.r:
                    k, v = t[0], t[1]
                    if k not in mx or mx[k][1] < v:
                        mx[k] = (k, v, t[2])
                b.r = list(mx.values())
        for b in writes:
            b.w = list(toks)
            b.r = []

    def op(self, eng, fn, reads=(), writes=(), same_ok=False, stream=False):
        stream = False
        waits = self._deps(eng, reads, writes, same_ok, stream)
        k = "e_" + eng
        self.cnt[k] += 1
        tok = (k, self.cnt[k], eng, stream)
        sems = self.sems

        def thunk(e, waits=waits, fn=fn, s=sems[k]):
            for (wk, wv) in waits:
                e.wait_ge(sems[wk], wv)
            fn(e).then_inc(s, 1)

        self.q[eng].append(thunk)
        self._commit([tok], reads, writes)
        self.n_ops += 1
        return tok

    def dma(self, eng, fns, reads=(), writes=()):
        if not isinstance(fns, (list, tuple)):
            fns = [fns]
        waits = self._deps(eng, reads, writes)
        wd = self.waited[eng]
        pool = self.dma_pool[eng]
        toks = []
        items = []
        for fn in fns:
            k = pool[self.dma_rr[eng] % len(pool)]
            self.dma_rr[eng] += 1
            prev = self.cnt[k]
            if prev > 0 and wd.get(k, 0) < prev:
                wd[k] = prev
                waits.append((k, prev))
            self.cnt[k] += 16
            toks.append((k, self.cnt[k], "dma"))
            items.append((fn, self.sems[k]))
        sems = self.sems

        def thunk(e, waits=waits, items=items):
            for (wk, wv) in waits:
                e.wait_ge(sems[wk], wv)
            for fn, s in items:
                fn(e).then_inc(s, 16)

        self.q[eng].append(thunk)
        self._commit(toks, reads, writes)
        self.n_ops += 1
        return toks

    def finish(self):
        nc = self.nc
        sems = self.sems
        cnt = dict(self.cnt)

        def fin_thunk(e):
            for k, v in cnt.items():
                if v > 0:
                    e.wait_ge(sems[k], v)

        self.q[SP].append(fin_thunk)
        q = self.q
        with nc.Block() as block:
            @block.tensor
            def _(e):
                for t in q[PE]:
                    t(e)

            @block.scalar
            def _(e):
                for t in q[ACT]:
                    t(e)

            @block.vector
            def _(e):
                for t in q[DVE]:
                    t(e)

            @block.gpsimd
            def _(e):
                for t in q[POOL]:
                    t(e)

            @block.sync
            def _(e):
                for t in q[SP]:
                    t(e)


D = 1024
NL = 4096
NCX = 256
NT = NL + NCX
PAD = 15
L0 = PAD
C0 = PAD + NL + 2 * PAD
CTW = 4416
NCT = 12
NRING = 7
EPS = 1e-6
DEPTH = 4
NEXP = 16
CAP_L = 512
CAP_C = 32
NSLOT = CAP_L + CAP_C
BLKS = [(L0 + 512 * j, 512 * j, 512) for j in range(8)] + [(C0, NL, NCX)]

V_NMG = 0
V_NFG = 32
V_BMOD = 64
V_EV = 256
V_OD = 532
V_CC = 564
NV = 580


def build_program(n_layers=DEPTH, debug=False, stop=None, small_moe=False, start_layer=0, moe_decl=None):
    nc = bass.Bass("TRN2", target_bir_lowering=False)
    P = Prog(nc)

    def din(name, shape, dt=F32):
        return nc.dram_tensor(name, list(shape), dt, kind="ExternalInput")

    xT = din("xT", [D, NL])
    ctxT = din("ctxT", [D, NCX])
    vecs = din("vecs", [128, NV])
    cmats = din("cmats", [128, 5 * 128])
    rope_cs = din("rope_cs", [128, 2 * NL])
    pool_fix = din("pool_fix", [128, 64])
    zeros_d = din("zeros_d", [NT, D])
    w_mod = din("w_mod", [DEPTH, D, 6 * D])
    ev_w_in = din("ev_w_in", [2, D, 1792])
    ev_w_out = din("ev_w_out", [2, D, D])
    od_w_in = din("od_w_in", [2, D, 2048])
    od_w_out = din("od_w_out", [2, D, D])
    od_pool_w = din("od_pool_w", [2, 4, 128, 128])
    w_router = din("w_router", [DEPTH, D, NEXP])
    mshape = [1, 1] if small_moe else [DEPTH if moe_decl is None else len(moe_decl), NEXP]
    mmap = {l: l for l in range(DEPTH)} if moe_decl is None else {l: n for n, l in enumerate(moe_decl)}
    w_gate = din("w_gate", mshape + [D, 2048])
    w_up = din("w_up", mshape + [D, 2048])
    w_down = din("w_down", mshape + [2048, D])
    outT = nc.dram_tensor("outT", [D, NL], F32, kind="ExternalOutput")
    XA = nc.dram_tensor("XA", [D, NT], F32)
    HFTOK = nc.dram_tensor("HFTOK", [NT, D], BF16)
    MOQ = [nc.dram_tensor("MO%d" % q, [NT, 256], F32) for q in range(4)]
    dbg = {}
    if debug:
        dbg["xa"] = nc.dram_tensor("dbg_xa", [D, NT], F32, kind="ExternalOutput")

    XAb = [[P.buf("xa%d_%d" % (c, j)) for j in range(9)] for c in range(8)]
    HFb = [P.buf("hftok%d" % t) for t in range(34)]
    MOb = [P.buf("mo%d" % q) for q in range(4)]
    OUTb = P.buf("out")

    CT = nc.alloc_sbuf_tensor("ct", [128, NCT * CTW], BF16)
    CTF = CT.bitcast(F32)
    CTb = [[[P.buf("ct%d_%d_%d" % (i, j, h)) for h in range(2)] for j in range(9)] for i in range(NCT)]
    RING = nc.alloc_sbuf_tensor("ring", [128, NRING * 4096], BF16)
    RINGb = [P.buf("ring%d" % s) for s in range(NRING)]
    RSTD = nc.alloc_sbuf_tensor("rstd", [128, NT], F32)
    RSTDb = [P.buf("rstd%d" % j) for j in range(9)]
    WT = nc.alloc_sbuf_tensor("wt", [128, 12 * 512], F32)
    WTH = WT.bitcast(BF16)
    WTI = WT.bitcast(I32)
    WTU = WT.bitcast(U32)
    WTb = [P.buf("wt%d" % k) for k in range(12)]
    CM = nc.alloc_sbuf_tensor("cm", [128, 4 * 128], BF16)
    CMb = P.buf("cm")
    IDF = nc.alloc_sbuf_tensor("idf", [128, 128], F32)
    IDFb = P.buf("idf")
    ONEF = nc.alloc_sbuf_tensor("onef", [16, 16], F32)
    ONEFb = P.buf("onef")
    VEC = nc.alloc_sbuf_tensor("vec", [128, NV], F32)
    VECb = P.buf("vec")
    MODS = nc.alloc_sbuf_tensor("mods", [128, DEPTH * 96], F32)
    MODSb = P.buf("mods")
    DER = nc.alloc_sbuf_tensor("der", [128, 96], F32)
    DERb = P.buf("der")
    SCC = nc.alloc_sbuf_tensor("scc", [128, 16], BF16)
    SCCb = P.buf("scc")
    WR = nc.alloc_sbuf_tensor("wr", [128, 8 * NEXP], F32)
    WRb = P.buf("wr")
    AFFT = nc.alloc_sbuf_tensor("afft", [128, 5 * NEXP], F32)
    AFFTb = P.buf("afft")
    IDXT = nc.alloc_sbuf_tensor("idxt", [128, 5 * NEXP], I32)
    IDXTb = P.buf("idxt")
    PFIX = nc.alloc_sbuf_tensor("pfix", [128, 64], F32)
    PFIXb = P.buf("pfix")
    PSD = [nc.alloc_psum_tensor("psd%d" % k, [128, 1024], F32) for k in range(4)]
    PSDH = [p.bitcast(BF16) for p in PSD]
    PS = [PSD[k // 2][:, (k % 2) * 512:(k % 2 + 1) * 512] for k in range(8)]
    PSH = [PSDH[k // 2][:, (k % 2) * 1024:(k % 2 + 1) * 1024] for k in range(8)]
    PSb = [P.buf("ps%d" % k) for k in range(8)]

    def ct(i, a, b):
        return CT[:, i * CTW + a: i * CTW + b]

    def ctp(i, a, b, p0, p1):
        return CT[p0:p1, i * CTW + a: i * CTW + b]

    def cb(i, j):
        return CTb[i][j]

    def wt(k, w=512):
        return WT[:, k * 512: k * 512 + w]

    def wth(k, a=0, b=1024):
        return WTH[:, k * 1024 + a: k * 1024 + b]

    def ring(s, a=0, b=4096):
        return RING[:, s * 4096 + a: s * 4096 + b]

    def ring3(s, c):
        return RING[:, s * 4096:(s + 1) * 4096].rearrange("p (c n) -> p c n", c=c)

    def cm(k):
        return CM[:, k * 128:(k + 1) * 128]

    IDB, ONESB, BLK1, ROT = 0, 1, 2, 3

    def vcol(k, n=1):
        return VEC[:, k:k + n]

    def MM(out, pairs, reads, writes):
        def fn(e, out=out, pairs=pairs):
            n = len(pairs)
            last = None
            for t, (l, r) in enumerate(pairs):
                last = e.matmul(out, lhsT=l, rhs=r, start=(t == 0), stop=(t == n - 1))
            return last
        return P.op(PE, fn, reads, writes)

    def TR(out, in_, ident, reads, writes):
        return P.op(PE, lambda e: e.transpose(out=out, in_=in_, identity=ident), reads, writes)

    def TRS(items, reads, writes):
        def fn(e, items=items):
            last = None
            for (o, i_, idn) in items:
                last = e.transpose(out=o, in_=i_, identity=idn)
            return last
        return P.op(PE, fn, reads, writes)

    def big(ap):
        sh = ap.shape
        return len(sh) == 2 and sh[1] >= 256

    def A(out, in_, func, reads, writes, scale=1.0, bias=0.0, accum=None):
        if accum is None:
            return P.op(ACT, lambda e: e.activation(out=out, in_=in_, func=func, bias=bias, scale=scale), reads, writes,
                        stream=big(out) and big(in_))
        return P.op(ACT, lambda e: e.activation(out=out, in_=in_, func=func, bias=bias, scale=scale, accum_out=accum), reads, writes)

    def TT(out, in0, in1, op, reads, writes, eng=DVE):
        return P.op(eng, lambda e: e.tensor_tensor(out=out, in0=in0, in1=in1, op=op), reads, writes,
                    stream=(eng == DVE) and big(out) and big(in0) and big(in1))

    def TS(out, in0, s1, s2, op0, op1, reads, writes, eng=DVE):
        if s2 is None:
            return P.op(eng, lambda e: e.tensor_scalar(out, in0, s1, None, op0=op0), reads, writes, stream=(eng == DVE) and big(out) and big(in0))
        return P.op(eng, lambda e: e.tensor_scalar(out, in0, s1, s2, op0=op0, op1=op1), reads, writes, stream=(eng == DVE) and big(out) and big(in0))

    def STT(out, in0, scalar, in1, op0, op1, reads, writes, eng=DVE):
        return P.op(eng, lambda e: e.scalar_tensor_tensor(out=out, in0=in0, scalar=scalar, in1=in1, op0=op0, op1=op1), reads, writes,
                    stream=(eng == DVE) and big(out) and big(in0) and big(in1))

    def CP(out, in_, reads, writes, eng=DVE):
        return P.op(eng, lambda e: e.tensor_copy(out, in_), reads, writes, stream=(eng == DVE) and big(out) and big(in_))

    def RCP(out, in_, reads, writes):
        return P.op(DVE, lambda e: e.reciprocal(out=out, in_=in_), reads, writes, stream=big(out) and big(in_))

    def MSET(ap, val, writes, eng=POOL):
        return P.op(eng, lambda e: e.memset(ap, val), (), writes)

    def LD(eng, out, in_, reads, writes):
        return P.dma(eng, lambda e: e.dma_start(out=out, in_=in_), reads, writes)

    LD(POOL, CM[:, :], cmats[:, 0:512], [], [CMb])
    LD(SP, IDF[:, :], cmats[:, 0:128], [], [IDFb])
    LD(SP, ONEF[:, :], cmats[0:16, 128:144], [], [ONEFb])
    LD(SP, VEC[:, :], vecs[:, :], [], [VECb])
    LD(SP, PFIX[:, :], pool_fix[:, :], [], [PFIXb])
    def zero_mo(reads):
        zsrc = zeros_d[0:NT // 4, :].rearrange("(a b) d -> a (b d)", a=16)
        for q in range(4):
            LD(SP, MOQ[q][:, :].rearrange("(a b) d -> a (b d)", a=16), zsrc, [MOb[q]] if reads else [], [MOb[q]])
    zero_mo(False)
    for i in range(NCT):
        MSET(ct(i, 0, CTW), 0.0, [b for j in range(9) for b in cb(i, j)])
    for k in range(12):
        MSET(wt(k), 0.0, [WTb[k]])

    A(WT[:, 0:16], VEC[:, V_CC:V_CC + 16], AF.Silu, [VECb, WTb[0]], [WTb[0]])
    CP(SCC[:, :], WT[:, 0:16], [WTb[0]], [SCCb])
    scc3 = SCC[:, :].rearrange("p (c v) -> p c v", v=2)
    pcount = [0]

    def mods_mm(i):
        for n in range(12):
            s_ = pcount[0] % NRING
            pcount[0] += 1
            LD(POOL, ring3(s_, 8), w_mod[i].rearrange("(c p) n -> p c n", p=128)[:, :, n * 512:(n + 1) * 512], [], [RINGb[s_]])
            r3 = ring3(s_, 8)
            for oc in range(4):
                k = n * 4 + oc
                MM(PS[0][:, k * 2:k * 2 + 2], [(r3[:, c, oc * 128:(oc + 1) * 128], scc3[:, c, :]) for c in range(8)],
                   [RINGb[s_], SCCb], [PSb[0]])

    def mods_fin(i):
        m3 = MODS[:, i * 96:(i + 1) * 96].rearrange("p (k v) -> p k v", v=2)
        p3 = PS[0][:, 0:96].rearrange("p (k v) -> p k v", v=2)
        for v in range(2):
            TT(m3[:, :, v], p3[:, :, v], VEC[:, V_BMOD + i * 48: V_BMOD + (i + 1) * 48], ALU.add, [PSb[0], VECb], [MODSb])

    mods_mm(start_layer)
    mods_fin(start_layer)

    def mod(i, grp, c, v):
        k = grp * 8 + c
        return MODS[:, i * 96 + k * 2 + v: i * 96 + k * 2 + v + 1]

    def der_setup(i):
        d3 = DER[:, 0:32].rearrange("p (g v c) -> p g v c", g=2, v=2)
        m4 = MODS[:, i * 96:(i + 1) * 96].rearrange("p (g c v) -> p g c v", g=6, v=2)
        for v in range(2):
            STT(d3[:, 0, v, :], m4[:, 1, :, v], 1.0, VEC[:, V_NMG + i * 8: V_NMG + i * 8 + 8], ALU.add, ALU.mult, [MODSb, VECb], [DERb])
            STT(d3[:, 1, v, :], m4[:, 4, :, v], 1.0, VEC[:, V_NFG + i * 8: V_NFG + i * 8 + 8], ALU.add, ALU.mult, [MODSb, VECb], [DERb])

    def G1(c, v):
        return DER[:, v * 8 + c: v * 8 + c + 1]

    def G2(c, v):
        return DER[:, 16 + v * 8 + c: 16 + v * 8 + c + 1]

    SS = 7

    sqrr = [0]

    def stat_chunk(j, c, xs_ap, xs_buf, w):
        k = 3 + (sqrr[0] % 2)
        sqrr[0] += 1
        A(wth(k, 0, w), xs_ap, AF.Square, [xs_buf], [WTb[k]])
        return k

    def stat_mm(j, c, k, w):
        P.op(PE, lambda e: e.matmul(PS[SS][:, 0:w], lhsT=cm(ONESB), rhs=wth(k, 0, w), start=(c == 0), stop=(c == 7)),
             [CMb, WTb[k]], [PSb[SS]])

    def stat_fin(j, w):
        xcol = BLKS[j][1]
        A(wt(5, w), PS[SS][:, 0:w], AF.Sqrt, [PSb[SS]], [WTb[5]], scale=1.0 / D, bias=EPS)
        RCP(RSTD[:, xcol:xcol + w], wt(5, w), [WTb[5]], [RSTDb[j]])

    def blocks_for(use_ctx):
        return list(range(9)) if use_ctx else list(range(8))

    xsrr = [0]

    def next_xs():
        k = xsrr[0] % 3
        xsrr[0] += 1
        return k

    for j in range(9):
        cs, xc, w = BLKS[j]
        pend = None
        for c in range(8):
            k = next_xs()
            src = xT[c * 128:(c + 1) * 128, xc:xc + w] if j < 8 else ctxT[c * 128:(c + 1) * 128, 0:w]
            LD(SP, wt(k, w), src, [], [WTb[k]])
            LD(SP, XA[c * 128:(c + 1) * 128, xc:xc + w], wt(k, w), [WTb[k]], [XAb[c][j]])
            kk = stat_chunk(j, c, wt(k, w), WTb[k], w)
            if pend is not None:
                stat_mm(j, *pend, w)
            pend = (c, kk)
        stat_mm(j, *pend, w)
        stat_fin(j, w)

    def chk(name):
        if stop == name:
            raise _Stop()

    HB = [WTH[:, 8 * 1024: 12 * 1024], RING[:, 6 * 4096: 7 * 4096]]
    HBb = [[WTb[8], WTb[9], WTb[10], WTb[11]], [RINGb[6]]]
    hbrr = [0]

    def build_h(i, j, which):
        cs, xc, w = BLKS[j]
        v = 1 if j == 8 else 0
        hb_i = hbrr[0] % 2 if "hb0" not in KDBG else 0
        hbrr[0] += 1
        hb3 = HB[hb_i].rearrange("p (c n) -> p c n", c=8)
        for c in range(8):
            k = next_xs()
            LD(SP, wt(k, w), XA[c * 128:(c + 1) * 128, xc:xc + w], [XAb[c][j]], [WTb[k]])
            TT(wt(k, w), wt(k, w), RSTD[:, xc:xc + w], ALU.mult, [WTb[k], RSTDb[j]], [WTb[k]])
            if which == 1:
                A(hb3[:, c, 0:w], wt(k, w), AF.Identity, [WTb[k], DERb, MODSb] + HBb[hb_i], HBb[hb_i],
                  scale=G1(c, v), bias=mod(i, 0, c, v))
            else:
                A(hb3[:, c, 0:w], wt(k, w), AF.Identity, [WTb[k], DERb, MODSb] + HBb[hb_i], HBb[hb_i],
                  scale=G2(c, v), bias=mod(i, 3, c, v))
        return hb3, HBb[hb_i]

    def load_w_slot(slot, src3, ncols=512, dst_off=0):
        LD(POOL, ring3(slot, 8)[:, :, dst_off:dst_off + ncols], src3, [], [RINGb[slot]])

    def w3(wh):
        return wh.rearrange("(c p) n -> p c n", p=128)

    def outproj_residual(i, mix_tiles, wslots, use_ctx, gate_grp):
        for j in blocks_for(use_ctx):
            cs, xc, w = BLKS[j]
            v = 1 if j == 8 else 0
            pend = None
            for c in range(8):
                slot = wslots[c // 4]
                r3 = ring3(slot, 8)
                pb = c % 2
                MM(PS[pb][:, 0:w], [(r3[:, k, (c % 4) * 128:(c % 4 + 1) * 128], ct(mix_tiles[k], cs, cs + w)) for k in range(8)],
                   [RINGb[slot]] + [b for k in range(8) for b in cb(mix_tiles[k], j)], [PSb[pb]])
                kx = next_xs()
                LD(SP, wt(kx, w), XA[c * 128:(c + 1) * 128, xc:xc + w], [XAb[c][j]], [WTb[kx]])
                STT(wt(kx, w), PS[pb][:, 0:w], mod(i, gate_grp, c, v), wt(kx, w), ALU.mult, ALU.add,
                    [PSb[pb], WTb[kx], MODSb], [WTb[kx]])
                LD(SP, XA[c * 128:(c + 1) * 128, xc:xc + w], wt(kx, w), [WTb[kx]], [XAb[c][j]])
                kk = stat_chunk(j, c, wt(kx, w), WTb[kx], w)
                if pend is not None:
                    stat_mm(j, *pend, w)
                pend = (c, kk)
            stat_mm(j, *pend, w)
            stat_fin(j, w)

    U_T = [0, 1, 2, 3]
    CO_T = [4, 5, 6, 7]
    Q_T = [0, 1, 2, 3]
    K_T = 8
    V_T = 9
    MISC_T = 11

    def vtok(kt, a, b):
        base = V_T * CTW + kt * 256
        return CT[:, base + a: base + b]

    VTb = [P.buf("vt%d" % kt) for kt in range(34)]

    RINGF = RING.bitcast(F32)
    COS_AP = RINGF[:, 4 * 2048: 4 * 2048 + 512]
    SIN_AP = RINGF[:, 4 * 2048 + 512: 4 * 2048 + 1024]
    KP_T = [K_T, MISC_T]

    def zero_pads(tiles):
        for t in tiles:
            MSET(ct(t, 0, PAD), 0.0, cb(t, 0))
            MSET(ct(t, L0 + NL, C0), 0.0, cb(t, 7) + cb(t, 8))
            MSET(ct(t, C0 + NCX, CTW), 0.0, cb(t, 8))

    def even_mixer(i):
        jj = i // 2
        ctx_full = i < 2
        ve = V_EV + jj * 138
        zero_pads(U_T)
        wi = w3(ev_w_in[jj])
        load_w_slot(0, wi[:, :, 0:512])
        load_w_slot(1, wi[:, :, 512:1024])
        for cq in range(4):
            for half in range(2):
                hq = half * 4 + cq
                load_w_slot(2, wi[:, :, 1024 + hq * 64: 1024 + (hq + 1) * 64], 64, cq * 128 + half * 64)
        load_w_slot(3, wi[:, :, 1536:1792], 256, 0)
        for j in blocks_for(ctx_full):
            cs, xc, w = BLKS[j]
            hb3, hbb = build_h(i, j, 1)
            for c in range(4):
                MM(PS[0][:, 0:w], [(ring3(0, 8)[:, k, c * 128:(c + 1) * 128], hb3[:, k, 0:w]) for k in range(8)],
                   [RINGb[0]] + hbb, [PSb[0]])
                MM(PS[1][:, 0:w], [(ring3(1, 8)[:, k, c * 128:(c + 1) * 128], hb3[:, k, 0:w]) for k in range(8)],
                   [RINGb[1]] + hbb, [PSb[1]])
                A(wt(5, w), PS[1][:, 0:w], AF.Sigmoid, [PSb[1]], [WTb[5]])
                TT(ct(U_T[c], cs, cs + w), PS[0][:, 0:w], wt(5, w), ALU.mult, [PSb[0], WTb[5]], cb(U_T[c], j))
        chk("passA")
        DS = [4, 5, 6, 0]
        for c in range(4 if "nodiag" not in KDBG else 0):
            for t in range(31):
                TS(ring(DS[c], t * 128, (t + 1) * 128), cm(IDB), VEC[:, ve + 14 + c * 31 + t: ve + 14 + c * 31 + t + 1], None,
                   ALU.mult, None, [CMb, VECb], [RINGb[DS[c]]], eng=POOL if (t % 2) else DVE)
        wo = w3(ev_w_out[jj])
        def load_wout(slot, col0):
            LD(POOL, ring3(slot, 8)[:, 0:4, :], wo[:, 0:4, col0:col0 + 512], [], [RINGb[slot]])
            woh = ev_w_out[jj][512:1024, col0:col0 + 512].rearrange("(h p) n -> p h n", p=64)
            for half in range(2):
                LD(POOL, RING[half * 64:(half + 1) * 64, slot * 4096:(slot + 1) * 4096].rearrange("p (c n) -> p c n", c=8)[:, 4:8, :],
                   woh[:, half * 4: half * 4 + 4, :], [], [RINGb[slot]])
        load_wout(1, 0)
        for j in (blocks_for(ctx_full) if "noconv" not in KDBG else []):
            cs, xc, w = BLKS[j]
            for c in range(4):
                MM(PS[c % 2][:, 0:w], [(ring(DS[c], t * 128, (t + 1) * 128), ct(U_T[c], cs + t - 15, cs + t - 15 + w)) for t in range(31)],
                   [RINGb[DS[c]]] + [b for jn in ((8,) if j == 8 else (max(j - 1, 0), j, min(j + 1, 7))) for b in cb(U_T[c], jn)], [PSb[c % 2]])
                A(wt(5 + c, w), PS[c % 2][:, 0:w], AF.Identity, [PSb[c % 2], VECb], [WTb[5 + c]], bias=VEC[:, ve + c: ve + c + 1])
                A(wth(3, 0, w), wt(5 + c, w), AF.Square, [WTb[5 + c]], [WTb[3]])
                CP(wth(4, 0, w), wt(5 + c, w), [WTb[5 + c]], [WTb[4]])
                P.op(PE, lambda e, c=c, w=w: e.matmul(PS[2][:, 0:w], lhsT=cm(ONESB), rhs=wth(4, 0, w), start=(c == 0), stop=(c == 3)),
                     [CMb, WTb[4]], [PSb[2]])
                P.op(PE, lambda e, c=c, w=w: e.matmul(PS[3][:, 0:w], lhsT=cm(ONESB), rhs=wth(3, 0, w), start=(c == 0), stop=(c == 3)),
                     [CMb, WTb[3]], [PSb[3]])
            TS(wt(9, w), PS[2][:, 0:w], 1.0 / 512, None, ALU.mult, None, [PSb[2]], [WTb[9]])
            TT(wt(10, w), wt(9, w), wt(9, w), ALU.mult, [WTb[9]], [WTb[10]])
            STT(wt(10, w), PS[3][:, 0:w], 1.0 / 512, wt(10, w), ALU.mult, ALU.subtract, [PSb[3], WTb[10]], [WTb[10]])
            A(wt(10, w), wt(10, w), AF.Sqrt, [WTb[10]], [WTb[10]], bias=EPS)
            RCP(wt(10, w), wt(10, w), [WTb[10]], [WTb[10]])
            for c in range(4):
                TT(wt(5 + c, w), wt(5 + c, w), wt(9, w), ALU.subtract, [WTb[5 + c], WTb[9]], [WTb[5 + c]])
                TT(wt(5 + c, w), wt(5 + c, w), wt(10, w), ALU.mult, [WTb[5 + c], WTb[10]], [WTb[5 + c]])
                A(ct(CO_T[c], cs, cs + w), wt(5 + c, w), AF.Silu, [WTb[5 + c], VECb], cb(CO_T[c], j),
                  scale=VEC[:, ve + 4 + c: ve + 5 + c], bias=VEC[:, ve + 8 + c: ve + 9 + c])
        chk("conv")
        TS(DER[:, 40:41], VEC[:, ve + 12: ve + 13], 0.125, None, ALU.mult, None, [VECb], [DERb])
        CP(DER[:, 41:42], VEC[:, ve + 13: ve + 14], [VECb], [DERb])
        MSET(ct(MISC_T, 0, CTW), 0.0, [b for jx in range(9) for b in cb(MISC_T, jx)], eng=DVE)
        for kt in range(34 if "noones" not in KDBG else 0):
            MSET(CT[:, V_T * CTW + kt * 256: V_T * CTW + (kt + 1) * 256].rearrange("p (h n) -> p h n", h=2)[:, :, 64:128], 1.0, [VTb[kt]], eng=DVE)
        for j in range(int(os.environ.get("PBN", "9"))):
            cs, xc, w = BLKS[j]
            is_ctx = (j == 8)
            hb3, hbb = build_h(i, j, 1)
            if "nochunks" in KDBG:
                continue
            if not is_ctx and "noropeld" not in KDBG:
                LD(SP, COS_AP[:, 0:w], rope_cs[:, xc:xc + w], [], [RINGb[4]])
                LD(SP, SIN_AP[:, 0:w], rope_cs[:, NL + xc: NL + xc + w], [], [RINGb[4]])
            chunks = []
            if (not is_ctx) or ctx_full:
                chunks += [("q", cq) for cq in range(4)]
            chunks += [("k", 0)]
            for kind, cq in chunks:
                if kind == "q":
                    lw = [(ring3(2, 8)[:, k, cq * 128:(cq + 1) * 128], hb3[:, k, 0:w]) for k in range(8)]
                    dst, dstb, gcol = ct(Q_T[cq], cs, cs + w), cb(Q_T[cq], j), 40
                    rb = [RINGb[2]]
                else:
                    lw = [(ring3(3, 8)[:, k, 0:128], hb3[:, k, 0:w]) for k in range(8)]
                    dst, dstb, gcol = ct(K_T, cs, cs + w), cb(K_T, j), 41
                    rb = [RINGb[3]]
                MM(PS[0][:, 0:w], lw, rb + hbb, [PSb[0]])
                CP(wt(5, w), PS[0][:, 0:w], [PSb[0]], [WTb[5]])
                A(wth(3, 0, w), wt(5, w), AF.Square, [WTb[5]], [WTb[3]])
                MM(PS[1][:, 0:w], [(cm(BLK1), wth(3, 0, w))], [CMb, WTb[3]], [PSb[1]])
                A(wt(6, w), PS[1][:, 0:w], AF.Sqrt, [PSb[1]], [WTb[6]], scale=1.0 / 64, bias=EPS)
                RCP(wt(6, w), wt(6, w), [WTb[6]], [WTb[6]])
                if "noqk" in KDBG:
                    continue
                if is_ctx or "norope" in KDBG:
                    STT(dst, wt(5, w), DER[:, gcol:gcol + 1], wt(6, w), ALU.mult, ALU.mult, [WTb[5], WTb[6], DERb], dstb)
                else:
                    STT(wt(5, w), wt(5, w), DER[:, gcol:gcol + 1], wt(6, w), ALU.mult, ALU.mult, [WTb[5], WTb[6], DERb], [WTb[5]])
                    A(wth(4, 0, w), wt(5, w), AF.Copy, [WTb[5]], [WTb[4]])
                    MM(PS[2][:, 0:w], [(cm(ROT), wth(4, 0, w))], [CMb, WTb[4]], [PSb[2]])
                    TT(wt(5, w), wt(5, w), COS_AP[:, 0:w], ALU.mult, [WTb[5], RINGb[4]], [WTb[5]])
                    TT(wt(6, w), PS[2][:, 0:w], SIN_AP[:, 0:w], ALU.mult, [PSb[2], RINGb[4]], [WTb[6]])
                    TT(dst, wt(5, w), wt(6, w), ALU.add, [WTb[5], WTb[6]], dstb)
                if kind == "k":
                    CP(ctp(MISC_T, cs, cs + w, 64, 128), ctp(K_T, cs, cs + w, 64, 128), [CTb[K_T][j][1]], [CTb[MISC_T][j][1]])
                    MSET(ctp(K_T, cs, cs + w, 64, 128), 0.0, [CTb[K_T][j][1]], eng=DVE)
            for tt in range(w // 128 if "nov" not in KDBG else 0):
                kt = (32 + tt) if is_ctx else (j * 4 + tt)
                pb = 4 + (tt % 2)
                MM(PS[pb][:, 0:128], [(hb3[:, k, tt * 128:(tt + 1) * 128], ring3(3, 8)[:, k, 128:256]) for k in range(8)],
                   [RINGb[3]] + hbb, [PSb[pb]])
                CP(vtok(kt, 0, 256).rearrange("p (h n) -> p h n", h=2)[:, :, 0:64],
                   PS[pb][:, 0:128].rearrange("p (h n) -> p h n", h=2), [PSb[pb]], [VTb[kt]])
        chk("passB")
        load_wout(2, 512)
        items = []
        for j in blocks_for(ctx_full):
            is_ctx = (j == 8)
            kts = [32, 33] if is_ctx else [32, 33] + list(range(32))
            prs = [(kts[n], kts[n + 1]) for n in range(0, len(kts), 2)]
            for cq in range(4):
                for half in range(2):
                    for n, pr in enumerate(prs):
                        items.append((j, cq, half, pr, n == 0, n == len(prs) - 1))
        LA = 3
        nit = len(items)
        for step in range(nit + LA):
            if step < nit:
                j, cq, half, pr, first, last = items[step]
                cs, xc, w = BLKS[j]
                sd = step % 3
                sbufs = [PSb[2 * sd], PSb[2 * sd + 1]]
                pairs_mm = []
                rd = list(cb(Q_T[cq], j))
                for n, kt in enumerate(pr):
                    kcol = (C0 + (kt - 32) * 128) if kt >= 32 else (L0 + kt * 128)
                    kj = 8 if kt >= 32 else kt // 4
                    pairs_mm.append((PSD[sd][:, n * w:(n + 1) * w], ct(KP_T[half], kcol, kcol + 128)))
                    rd += cb(KP_T[half], kj)
                qap = ct(Q_T[cq], cs, cs + w)

                def smm(e, pairs_mm=pairs_mm, qap=qap):
                    last_i = None
                    for (o, kap) in pairs_mm:
                        last_i = e.matmul(o, lhsT=kap, rhs=qap, start=True, stop=True)
                    return last_i
                P.op(PE, smm, rd, sbufs)
                pk = step % 4
                A(wth(pk, 0, 2 * w), PSD[sd][:, 0:2 * w], AF.Exp, sbufs, [WTb[pk]])
            if step >= LA:
                j, cq, half, pr, first, last = items[step - LA]
                cs, xc, w = BLKS[j]
                pk = (step - LA) % 4
                ob = 6 + half

                def pvmm(e, ob=ob, w=w, pr=pr, half=half, pk=pk, first=first, last=last):
                    last_i = None
                    for n, kt in enumerate(pr):
                        last_i = e.matmul(PS[ob][:, 0:w], lhsT=vtok(kt, half * 128, half * 128 + 128), rhs=wth(pk, n * w, (n + 1) * w),
                                          start=(first and n == 0), stop=(last and n == len(pr) - 1))
                    return last_i
                P.op(PE, pvmm, [VTb[kt] for kt in pr] + [WTb[pk]], [PSb[ob]])
                if last:
                    RCP(WT[64:128, 5 * 512: 5 * 512 + w], PS[ob][64:128, 0:w], [PSb[ob]], [WTb[5]])
                    TT(ctp(Q_T[cq], cs, cs + w, half * 64, half * 64 + 64), PS[ob][0:64, 0:w], WT[64:128, 5 * 512: 5 * 512 + w], ALU.mult,
                       [PSb[ob], WTb[5]], [CTb[Q_T[cq]][j][half]])
        chk("attn")
        outproj_residual(i, CO_T + Q_T, [1, 2], ctx_full, 2)


    def odd_mixer(i):
        jj = i // 2
        ctx_full = i < 2
        vo = V_OD + jj * 16
        wi = w3(od_w_in[jj])
        PIN_T = [0, 1, 2, 3]
        PO_T = [4, 5, 6, 7]
        SC_T = [0, 1, 2, 3]
        GB_T = [8, 9, 10, 11]
        zero_pads(PIN_T)
        load_w_slot(3, wi[:, :, 1536:2048])
        load_w_slot(0, wi[:, :, 0:512])
        load_w_slot(1, wi[:, :, 512:1024])
        load_w_slot(2, wi[:, :, 1024:1536])
        PWT = ring(4, 20 * 128, 24 * 128)
        PWTb = RINGb[4]
        LD(POOL, PWT.rearrange("p (g n) -> p g n", g=4), od_pool_w[jj].rearrange("g p n -> p g n"), [], [PWTb])
        WIN = [2, 4, 8, 16]
        tapmat = {}
        tcount = 0
        for gi, wv in enumerate(WIN):
            for val in (1.0 / wv, 1.0 / wv - 1.0):
                TS(ring(4, tcount * 128, (tcount + 1) * 128), cm(IDB), float(val), None, ALU.mult, None, [CMb], [RINGb[4]])
                tapmat[(gi, val == 1.0 / wv)] = tcount
                tcount += 1
        for c in range(4):
            for t in range(3):
                TS(ring(4, (8 + c * 3 + t) * 128, (9 + c * 3 + t) * 128), cm(IDB), VEC[:, vo + c * 3 + t: vo + c * 3 + t + 1], None,
                   ALU.mult, None, [CMb, VECb], [RINGb[4]])
        for j in blocks_for(ctx_full):
            cs, xc, w = BLKS[j]
            hb3, hbb = build_h(i, j, 1)
            for c in range(4):
                MM(PS[c % 2][:, 0:w], [(ring3(3, 8)[:, k, c * 128:(c + 1) * 128], hb3[:, k, 0:w]) for k in range(8)],
                   [RINGb[3]] + hbb, [PSb[c % 2]])
                A(ct(PIN_T[c], cs, cs + w), PS[c % 2][:, 0:w], AF.Copy, [PSb[c % 2]], cb(PIN_T[c], j))
        wo = w3(od_w_out[jj])
        load_w_slot(5, wo[:, :, 0:512])
        load_w_slot(3, wo[:, :, 512:1024])
        for j in blocks_for(ctx_full):
            cs, xc, w = BLKS[j]
            seq0, seqn = (C0, NCX) if j == 8 else (L0, NL)
            for gi, wv in enumerate(WIN):
                taps = list(range(-wv // 2, wv // 2))
                nb = [b for jn in ((8,) if j == 8 else (max(j - 1, 0), j, min(j + 1, 7))) for b in cb(PIN_T[gi], jn)]
                MM(PS[gi % 2][:, 0:w], [(ring(4, tapmat[(gi, t != 0)] * 128, (tapmat[(gi, t != 0)] + 1) * 128),
                                         ct(PIN_T[gi], cs + t, cs + t + w)) for t in taps], [RINGb[4]] + nb, [PSb[gi % 2]])
                dk = 5 + (gi % 2)
                CP(wth(dk, 0, w), PS[gi % 2][:, 0:w], [PSb[gi % 2]], [WTb[dk]])
                h2 = wv // 2
                fix = []
                if cs == seq0:
                    fix.append((0, h2, gi * 16))
                if cs + w == seq0 + seqn:
                    fix.append((w - (h2 - 1), h2 - 1, gi * 16 + 8))
                for (o, n, fc) in fix:
                    if n <= 0:
                        continue
                    TT(wt(7, 16)[:, 0:n], wth(dk, o, o + n), ct(PIN_T[gi], cs + o, cs + o + n), ALU.add, [WTb[dk]] + cb(PIN_T[gi], j), [WTb[7]])
                    TT(wt(7, 16)[:, 0:n], wt(7, 16)[:, 0:n], PFIX[:, fc:fc + n], ALU.mult, [WTb[7], PFIXb], [WTb[7]])
                    TT(wth(dk, o, o + n), wt(7, 16)[:, 0:n], ct(PIN_T[gi], cs + o, cs + o + n), ALU.subtract, [WTb[7]] + cb(PIN_T[gi], j), [WTb[dk]])
                MM(PS[2 + gi % 2][:, 0:w], [(PWT[:, gi * 128:(gi + 1) * 128], wth(dk, 0, w))], [PWTb, WTb[dk]], [PSb[2 + gi % 2]])
                A(ct(PO_T[gi], cs, cs + w), PS[2 + gi % 2][:, 0:w], AF.Identity, [PSb[2 + gi % 2], VECb], cb(PO_T[gi], j),
                  scale=VEC[:, vo + 12 + gi: vo + 13 + gi])
        for j in blocks_for(ctx_full):
            cs, xc, w = BLKS[j]
            hb3, hbb = build_h(i, j, 1)
            for c in range(4):
                MM(PS[0][:, 0:w], [(ring3(0, 8)[:, k, c * 128:(c + 1) * 128], hb3[:, k, 0:w]) for k in range(8)], [RINGb[0]] + hbb, [PSb[0]])
                MM(PS[1][:, 0:w], [(ring3(2, 8)[:, k, c * 128:(c + 1) * 128], hb3[:, k, 0:w]) for k in range(8)], [RINGb[2]] + hbb, [PSb[1]])
                MM(PS[2][:, 0:w], [(ring3(1, 8)[:, k, c * 128:(c + 1) * 128], hb3[:, k, 0:w]) for k in range(8)], [RINGb[1]] + hbb, [PSb[2]])
                A(wt(5, w), PS[0][:, 0:w], AF.Copy, [PSb[0]], [WTb[5]])
                TT(ct(SC_T[c], cs, cs + w), PS[1][:, 0:w], wt(5, w), ALU.mult, [PSb[1], WTb[5]], cb(SC_T[c], j))
                A(ct(GB_T[c], cs, cs + w), PS[2][:, 0:w], AF.Copy, [PSb[2]], cb(GB_T[c], j))
        for j in blocks_for(ctx_full):
            cs, xc, w = BLKS[j]
            for c in range(4):
                nb = [b for jn in ((8,) if j == 8 else (max(j - 1, 0), j, min(j + 1, 7))) for b in cb(SC_T[c], jn)]
                MM(PS[c % 2][:, 0:w], [(ring(4, (8 + c * 3 + t) * 128, (9 + c * 3 + t) * 128), ct(SC_T[c], cs + t - 1, cs + t - 1 + w)) for t in range(3)],
                   [RINGb[4]] + nb, [PSb[c % 2]])
                TT(ct(GB_T[c], cs, cs + w), PS[c % 2][:, 0:w], ct(GB_T[c], cs, cs + w), ALU.mult, [PSb[c % 2]] + cb(GB_T[c], j), cb(GB_T[c], j))
        outproj_residual(i, GB_T + PO_T, [5, 3], ctx_full, 2)

    AFF_T = 0
    AFW_T = 2
    XE_T = [4, 5]
    ACT_T = 6

    def ctf(i, a, b, p=16):
        base = (i * CTW) // 2
        return CTF[0:p, base + a: base + b]

    def ctfb(i):
        return [b for t in (i, i + 1) for j in range(9) for b in cb(t, j)]

    TVb, TIb, TFb = [WTb[0], WTb[1]], [WTb[2], WTb[3]], [WTb[4], WTb[5]]

    def moe(i, last_layer):
        ctx_full = i < 2
        ntile = 5 if ctx_full else 4
        LD(SP, WR[:, :].rearrange("p (c n) -> p c n", c=8), w_router[i].rearrange("(c p) n -> p c n", p=128), [], [WRb])
        wr3 = WR[:, :].rearrange("p (c n) -> p c n", c=8)
        affb, afwb = ctfb(AFF_T), ctfb(AFW_T)
        for j in blocks_for(ctx_full):
            cs, xc, w = BLKS[j]
            v = 1 if j == 8 else 0
            hb_i = hbrr[0] % 2
            hbrr[0] += 1
            hb3 = HB[hb_i].rearrange("p (c n) -> p c n", c=8)
            hbb = HBb[hb_i]
            for c in range(8):
                k = next_xs()
                LD(SP, wt(k, w), XA[c * 128:(c + 1) * 128, xc:xc + w], [XAb[c][j]], [WTb[k]])
                TT(wt(k, w), wt(k, w), RSTD[:, xc:xc + w], ALU.mult, [WTb[k], RSTDb[j]], [WTb[k]])
                A(wt(k, w), wt(k, w), AF.Identity, [WTb[k], DERb, MODSb], [WTb[k]], scale=G2(c, v), bias=mod(i, 3, c, v))
                P.op(PE, lambda e, c=c, k=k, w=w: e.matmul(PS[0][0:16, 0:w], lhsT=wr3[:, c, :], rhs=wt(k, w), start=(c == 0), stop=(c == 7)),
                     [WRb, WTb[k]], [PSb[0]])
                CP(hb3[:, c, 0:w], wt(k, w), [WTb[k]] + hbb, hbb, eng=POOL)
            A(wt(5, w)[0:16, :], PS[0][0:16, 0:w], AF.Exp, [PSb[0]], [WTb[5]])
            MM(PS[1][0:16, 0:w], [(ONEF[:, :], wt(5, w)[0:16, :])], [ONEFb, WTb[5]], [PSb[1]])
            RCP(wt(6, w)[0:16, :], PS[1][0:16, 0:w], [PSb[1]], [WTb[6]])
            TT(ctf(AFF_T, xc, xc + w), wt(5, w)[0:16, :], wt(6, w)[0:16, :], ALU.mult, [WTb[5], WTb[6]], affb)
            for tt in range(w // 128):
                pb = 2 + (tt % 2)
                TRS([(PSH[pb][:, c * 128:(c + 1) * 128], hb3[:, c, tt * 128:(tt + 1) * 128], cm(IDB)) for c in range(8)],
                    [CMb] + hbb, [PSb[pb]])
                gk = 3 + (tt % 2)
                if tt % 2 == 0:
                    CP(wth(gk), PSH[pb][:, :], [PSb[pb]], [WTb[gk]])
                else:
                    A(wth(gk), PSH[pb][:, :], AF.Copy, [PSb[pb]], [WTb[gk]])
                trow = xc + tt * 128
                LD(SP, HFTOK[trow:trow + 128, :], wth(gk), [WTb[gk]], [HFb[trow // 128]])
        chk("moe_prep")
        if i + 1 < n_layers:
            mods_mm(i + 1)
        TV = WT[0:16, 0:NSLOT]
        TI = WTU[0:16, 1024:1024 + NSLOT]
        TF = WT[0:16, 2048:2048 + NSLOT]
        for (col0, ncol, nround, s0) in ([(0, NL, CAP_L // 8, 0)] + ([(NL, NCX, CAP_C // 8, CAP_L)] if ctx_full else [])):
            for r in range(nround):
                src = ctf(AFF_T, col0, col0 + ncol) if r == 0 else ctf(AFW_T, col0, col0 + ncol)
                srcb = affb if r == 0 else afwb
                sl = slice(s0 + 8 * r, s0 + 8 * r + 8)
                P.op(DVE, lambda e, sl=sl, src=src: e.max(out=TV[:, sl], in_=src), srcb, TVb)
                P.op(DVE, lambda e, sl=sl, src=src: e.max_index(out=TI[:, sl], in_max=TV[:, sl], in_values=src), srcb + TVb, TIb)
                if r < nround - 1:
                    P.op(DVE, lambda e, sl=sl, src=src, col0=col0, ncol=ncol: e.match_replace(out=ctf(AFW_T, col0, col0 + ncol), in_to_replace=TV[:, sl],
                                                                                              in_values=src, imm_value=-1.0), srcb + TVb, afwb)
        CP(TF, TI, TIb, TFb)
        if ctx_full:
            TS(TF[:, CAP_L:NSLOT], TF[:, CAP_L:NSLOT], float(NL), None, ALU.add, None, TFb, TFb)
        for t in range(ntile):
            n = 128 if t < 4 else CAP_C
            TR(PS[4][0:n, 0:16], TV[:, t * 128: t * 128 + n], IDF[0:16, 0:16], TVb + [IDFb], [PSb[4]])
            CP(AFFT[0:n, t * 16:(t + 1) * 16], PS[4][0:n, 0:16], [PSb[4]], [AFFTb])
            TR(PS[5][0:n, 0:16], TF[:, t * 128: t * 128 + n], IDF[0:16, 0:16], TFb + [IDFb], [PSb[5]])
            CP(IDXT[0:n, t * 16:(t + 1) * 16], PS[5][0:n, 0:16], [PSb[5]], [IDXTb])
        if i + 1 < n_layers:
            mods_fin(i + 1)
        chk("moe_topk")
        xe3 = [CT[:, XE_T[k] * CTW: XE_T[k] * CTW + 8 * NSLOT].rearrange("p (c s) -> p c s", c=8) for k in range(2)]
        xeb = [[b for j in range(9) for b in cb(XE_T[k], j)] for k in range(2)]
        act3 = CT[:, ACT_T * CTW: ACT_T * CTW + 16 * NSLOT].rearrange("p (f s) -> p f s", f=16)
        actb = [b for t in (ACT_T, ACT_T + 1) for j in range(9) for b in cb(t, j)]
        GTK = [9, 10, 11, 0, 1]

        pieces = []
        for e in range(NEXP):
            for q in range(4):
                pieces.append(("g", e, q))
                pieces.append(("u", e, q))
            for dq in range(4):
                pieces.append(("d", e, dq))
        slot_of = {}
        loaded = [0]

        def ensure_loaded(upto):
            while loaded[0] <= min(upto, len(pieces) - 1):
                kind, e, q = pieces[loaded[0]]
                s = pcount[0] % NRING
                pcount[0] += 1
                slot_of[loaded[0]] = s
                if kind == "g":
                    LD(POOL, ring3(s, 8), w3(w_gate[mmap[i], e])[:, :, q * 512:(q + 1) * 512], [], [RINGb[s]])
                elif kind == "u":
                    LD(POOL, ring3(s, 8), w3(w_up[mmap[i], e])[:, :, q * 512:(q + 1) * 512], [], [RINGb[s]])
                else:
                    LD(POOL, ring3(s, 16), w_down[mmap[i], e].rearrange("(f p) d -> p f d", p=128)[:, :, q * 256:(q + 1) * 256], [], [RINGb[s]])
                loaded[0] += 1

        def gather(e):
            for t in range(ntile):
                n = 128 if t < 4 else CAP_C
                gk = GTK[t]
                P.dma(POOL, lambda eng, gk=gk, n=n, t=t, e=e: eng.indirect_dma_start(
                    out=WTH[0:n, gk * 1024:(gk + 1) * 1024], out_offset=None, in_=HFTOK[:, :],
                    in_offset=bass.IndirectOffsetOnAxis(ap=IDXT[0:n, t * 16 + e: t * 16 + e + 1], axis=0)),
                    [IDXTb] + HFb, [WTb[gk]])

        def xe_transposes(e):
            k = e % 2
            for t in range(ntile):
                n = 128 if t < 4 else CAP_C
                gk = GTK[t]
                TRS([(PSH[6][:, c * 128: c * 128 + n], WTH[0:n, gk * 1024 + c * 128: gk * 1024 + (c + 1) * 128], CM[0:n, 0:n]) for c in range(8)],
                    [CMb, WTb[gk]], [PSb[6]])
                src = PSH[6][:, :].rearrange("p (c s) -> p c s", c=8)[:, :, 0:n]
                if t % 2 == 0:
                    CP(xe3[k][:, :, t * 128: t * 128 + n], src, [PSb[6]], xeb[k])
                else:
                    P.op(ACT, lambda eng, k=k, t=t, n=n, src=src: eng.activation(out=xe3[k][:, :, t * 128: t * 128 + n], in_=src, func=AF.Copy),
                         [PSb[6]], xeb[k])

        PYb = [PSb[5], PSb[7]]
        YSb = [P.buf("ys%d_%d" % (i, k)) for k in range(16)]
        gather(0)
        ensure_loaded(4)
        xe_transposes(0)
        pidx = 0
        ysr = [0]
        for e in range(NEXP):
            k = e % 2
            if e + 1 < NEXP:
                gather(e + 1)
            for q in range(4):
                ensure_loaded(pidx + 5)
                sg_, su_ = slot_of[pidx], slot_of[pidx + 1]
                pidx += 2
                for fl in range(4):
                    fc = q * 4 + fl
                    pg, pu = (fc % 2), 2 + (fc % 2)
                    MM(PS[pg][:, 0:512], [(ring3(sg_, 8)[:, c, fl * 128:(fl + 1) * 128], xe3[k][:, c, 0:512]) for c in range(8)],
                       [RINGb[sg_]] + xeb[k], [PSb[pg]])
                    MM(PS[pu][:, 0:512], [(ring3(su_, 8)[:, c, fl * 128:(fl + 1) * 128], xe3[k][:, c, 0:512]) for c in range(8)],
                       [RINGb[su_]] + xeb[k], [PSb[pu]])
                    if ctx_full:
                        co = (fc % 8) * 64
                        MM(PS[4][:, co:co + 32], [(ring3(sg_, 8)[:, c, fl * 128:(fl + 1) * 128], xe3[k][:, c, 512:544]) for c in range(8)],
                           [RINGb[sg_]] + xeb[k], [PSb[4]])
                        MM(PS[4][:, co + 32:co + 64], [(ring3(su_, 8)[:, c, fl * 128:(fl + 1) * 128], xe3[k][:, c, 512:544]) for c in range(8)],
                           [RINGb[su_]] + xeb[k], [PSb[4]])
                    sk = 5 + (fc % 2)
                    A(wth(sk, 0, 512), PS[pg][:, 0:512], AF.Silu, [PSb[pg]], [WTb[sk]])
                    TT(act3[:, fc, 0:512], wth(sk, 0, 512), PS[pu][:, 0:512], ALU.mult, [WTb[sk], PSb[pu]], actb)
                    if ctx_full and fc % 8 == 7:
                        p4 = PS[4][:, :].rearrange("p (f g s) -> p f g s", f=8, g=2)
                        s3 = wth(7, 0, 256).rearrange("p (f s) -> p f s", f=8)
                        A(s3, p4[:, :, 0, :], AF.Silu, [PSb[4]], [WTb[7]])
                        TT(act3[:, fc - 7: fc + 1, 512:544], s3, p4[:, :, 1, :], ALU.mult, [WTb[7], PSb[4]], actb)
            if e + 1 < NEXP:
                xe_transposes(e + 1)
            for dq in range(4):
                ensure_loaded(pidx + 5)
                sd_ = slot_of[pidx]
                pidx += 1
                d3 = ring3(sd_, 16)
                ys_list = []
                for t in range(ntile):
                    n = 128 if t < 4 else CAP_C
                    yb = ysr[0] % 2
                    ysr[0] += 1
                    pyb, pyo = 5 + yb * 2, 0
                    py = PS[pyb][0:n, pyo:pyo + 256]
                    MM(py, [(act3[:, fc, t * 128: t * 128 + n], d3[:, fc, :]) for fc in range(16)], [RINGb[sd_]] + actb, [PYb[yb]])
                    yk = (ysr[0] - 1) % 16
                    ybase = ((8 + yk // 8) * CTW) // 2 + (yk % 8) * 256
                    ysl = CTF[0:n, ybase: ybase + 256]
                    ysb = YSb[yk]
                    if yb == 0:
                        A(ysl, py, AF.Identity, [PYb[yb], AFFTb], [ysb], scale=AFFT[0:n, t * 16 + e: t * 16 + e + 1])
                    else:
                        TS(ysl, py, AFFT[0:n, t * 16 + e: t * 16 + e + 1], None, ALU.mult, None, [PYb[yb], AFFTb], [ysb])
                    ys_list.append((ysl, ysb, n, t))
                fns = []
                for (ysl, ysb, n, t) in ys_list:
                    fns.append(lambda eng, ysl=ysl, n=n, t=t, e=e, dq=dq: eng.indirect_dma_start(
                        out=MOQ[dq][:, :], out_offset=bass.IndirectOffsetOnAxis(ap=IDXT[0:n, t * 16 + e: t * 16 + e + 1], axis=0),
                        in_=ysl, in_offset=None, compute_op=ALU.add))
                P.dma(POOL, fns, [IDXTb, MOb[dq]] + [ysb for (_, ysb, _, _) in ys_list], [MOb[dq]])
        chk("moe_exp")
        for j in blocks_for(ctx_full):
            cs, xc, w = BLKS[j]
            v = 1 if j == 8 else 0
            pend = None
            for half in range(2):
                for tt in range(w // 128):
                    gk = 9 + (tt % 2)
                    trow = xc + tt * 128
                    P.dma(SP, [lambda e_, gk=gk, trow=trow, q=q: e_.dma_start(out=wt(gk, 512)[:, (q % 2) * 256:(q % 2 + 1) * 256], in_=MOQ[q][trow:trow + 128, :])
                               for q in (2 * half, 2 * half + 1)], [MOb[2 * half], MOb[2 * half + 1]], [WTb[gk]])
                    for cl in range(4):
                        TR(PS[cl][:, tt * 128:(tt + 1) * 128], wt(gk, 512)[:, cl * 128:(cl + 1) * 128], IDF[:, :], [WTb[gk], IDFb], [PSb[cl]])
                for cl in range(4):
                    c = half * 4 + cl
                    kx = next_xs()
                    LD(SP, wt(kx, w), XA[c * 128:(c + 1) * 128, xc:xc + w], [XAb[c][j]], [WTb[kx]])
                    STT(wt(kx, w), PS[cl][:, 0:w], mod(i, 5, c, v), wt(kx, w), ALU.mult, ALU.add, [PSb[cl], WTb[kx], MODSb], [WTb[kx]])
                    if last_layer:
                        LD(SP, outT[c * 128:(c + 1) * 128, xc:xc + w], wt(kx, w), [WTb[kx]], [OUTb])
                    else:
                        LD(SP, XA[c * 128:(c + 1) * 128, xc:xc + w], wt(kx, w), [WTb[kx]], [XAb[c][j]])
                        kk = stat_chunk(j, c, wt(kx, w), WTb[kx], w)
                        if pend is not None:
                            stat_mm(j, *pend, w)
                        pend = (c, kk)
            if not last_layer:
                stat_mm(j, *pend, w)
                stat_fin(j, w)
        if not last_layer:
            zero_mo(True)

    try:
        chk("pre")
        for i in range(start_layer, n_layers):
            der_setup(i)
            if i % 2 == 0:
                even_mixer(i)
            else:
                odd_mixer(i)
            if stop == "mix%d" % i:
                break
            moe(i, i == DEPTH - 1)
    except _Stop:
        pass
    if debug:
        for c in range(8):
            LD(SP, dbg["xa"][c * 128:(c + 1) * 128, :], XA[c * 128:(c + 1) * 128, :], [b for b in XAb[c]], [OUTb])
    P.finish()
    return nc, P


def _rope_tables():
    n_freq = 16
    inv = (10000.0 ** (-np.arange(n_freq, dtype=np.float32) / n_freq)).astype(np.float32)
    t = np.arange(NL)
    row = (t // 64).astype(np.float32)
    col = (t % 64).astype(np.float32)
    ang_r = row[:, None] * inv
    ang_c = col[:, None] * inv
    cos = np.zeros((64, NL), np.float32)
    sin = np.zeros((64, NL), np.float32)
    cos[0:16] = np.cos(ang_r).T
    cos[16:32] = np.cos(ang_r).T
    cos[32:48] = np.cos(ang_c).T
    cos[48:64] = np.cos(ang_c).T
    sin[0:16] = np.sin(ang_r).T
    sin[16:32] = np.sin(ang_r).T
    sin[32:48] = np.sin(ang_c).T
    sin[48:64] = np.sin(ang_c).T
    cos = np.concatenate([cos, cos], 0)
    sin = np.concatenate([sin, sin], 0)
    return np.concatenate([cos, sin], 1).astype(np.float32)


def _const_mats():
    m = np.zeros((128, 5 * 128), np.float32)
    m[:, 0:128] = np.eye(128)
    m[:, 128:256] = 1.0
    blk = np.zeros((128, 128), np.float32)
    blk[0:64, 0:64] = 1.0
    blk[64:128, 64:128] = 1.0
    m[:, 256:384] = blk
    rot = np.zeros((128, 128), np.float32)
    for base in range(0, 128, 32):
        for d in range(16):
            rot[base + 16 + d, base + d] = -1.0
            rot[base + d, base + 16 + d] = 1.0
    m[:, 384:512] = rot
    return m


def _pool_fix():
    f = np.ones((128, 64), np.float32)
    for gi, w in enumerate((2, 4, 8, 16)):
        h = w // 2
        for t in range(h):
            f[:, gi * 16 + t] = w / float(t + h)
        for m_ in range(h - 1):
            f[:, gi * 16 + 8 + m_] = w / float((h - 1 - m_) + h)
    return f


def _pack_vecs(inp, b):
    v = np.zeros((128, NV), np.float32)

    def col(a):
        a = np.asarray(a, np.float32)
        return a.reshape(-1, 128).T

    for i in range(DEPTH):
        v[:, V_NMG + i * 8: V_NMG + (i + 1) * 8] = col(inp["norm_mix_g"][i])
        v[:, V_NFG + i * 8: V_NFG + (i + 1) * 8] = col(inp["norm_ffn_g"][i])
        v[:, V_BMOD + i * 48: V_BMOD + (i + 1) * 48] = col(inp["b_mod"][i])
    for j in range(2):
        o = V_EV + j * 138
        v[:, o: o + 4] = col(inp["ev_conv_b"][j])
        v[:, o + 4: o + 8] = col(inp["ev_ln_g"][j])
        v[:, o + 8: o + 12] = col(inp["ev_ln_b"][j])
        v[:, o + 12] = np.tile(np.asarray(inp["ev_q_norm_g"][j], np.float32), 2)
        v[:, o + 13] = np.tile(np.asarray(inp["ev_k_norm_g"][j], np.float32), 2)
        cw = np.asarray(inp["ev_conv_w"][j], np.float32)
        for c in range(4):
            v[:, o + 14 + c * 31: o + 14 + (c + 1) * 31] = cw[:, c * 128:(c + 1) * 128].T
        o2 = V_OD + j * 16
        ow = np.asarray(inp["od_conv_w"][j], np.float32)
        for c in range(4):
            v[:, o2 + c * 3: o2 + (c + 1) * 3] = ow[:, c * 128:(c + 1) * 128].T
        v[:, o2 + 12: o2 + 16] = col(inp["od_pool_scale"][j])
    cc = np.stack([col(inp["c"][b]), col(inp["c_ctx"])], axis=-1)
    v[:, V_CC: V_CC + 16] = cc.reshape(128, 16)
    return v


_CACHE = {}


def kernel(**inputs):
    inp = {k: np.asarray(v) for k, v in inputs.items()}
    n = 8
    if "nc" not in _CACHE:
        _CACHE["nc"] = build_program()[0]
    nc = _CACHE["nc"]
    rope = _rope_tables()
    cm = _const_mats()
    pf = _pool_fix()
    zeros = np.zeros((NT, D), np.float32)
    shared = {k: np.ascontiguousarray(inp[k], dtype=np.float32) for k in
              ("w_mod", "ev_w_in", "ev_w_out", "od_w_in", "od_w_out", "od_pool_w", "w_router", "w_gate", "w_up", "w_down")}
    in_maps = []
    for b in range(n):
        m = dict(shared)
        m["xT"] = np.ascontiguousarray(inp["x"][b].T, dtype=np.float32)
        m["ctxT"] = np.ascontiguousarray(inp["ctx"][b].T, dtype=np.float32)
        m["vecs"] = _pack_vecs(inp, b)
        m["cmats"] = cm
        m["rope_cs"] = rope
        m["pool_fix"] = pf
        m["zeros_d"] = zeros
        in_maps.append(m)
    res = run_bass_kernel_spmd(nc, in_maps, core_ids=list(range(n)))
    out = np.stack([np.ascontiguousarray(res.results[b]["outT"].T) for b in range(n)], axis=0)
    return out.astype(np.float32)
```

```python
import os
import numpy as np
import concourse.bass as bass
import concourse.mybir as mybir
from concourse.bass_utils import run_bass_kernel_spmd

F32 = mybir.dt.float32
BF16 = mybir.dt.bfloat16
I32 = mybir.dt.int32
U32 = mybir.dt.uint32
ALU = mybir.AluOpType
AF = mybir.ActivationFunctionType

KDBG = set(os.environ.get("KDBG", "").split(","))
PE, ACT, DVE, POOL, SP = "pe", "act", "dve", "pool", "sp"
ENGS = [PE, ACT, DVE, POOL, SP]
STRICT_SAME = {PE: False, ACT: True, DVE: True, POOL: True, SP: False}


class _Stop(Exception):
    pass


class Buf:
    __slots__ = ("name", "w", "r")

    def __init__(self, name):
        self.name = name
        self.w = []
        self.r = []


class Prog:
    def __init__(self, nc):
        self.nc = nc
        self.q = {e: [] for e in ENGS}
        self.sems = {}
        self.cnt = {}
        self.waited = {e: {} for e in ENGS}
        for e in (PE, ACT, DVE, POOL):
            self._mksem("e_" + e)
        nd = {SP: 16, ACT: 4, POOL: 16}
        self.dma_pool = {}
        self.dma_rr = {}
        for e, n in nd.items():
            self.dma_pool[e] = [self._mksem("d_%s%d" % (e, i)) for i in range(n)]
            self.dma_rr[e] = 0
        self.n_ops = 0

    def _mksem(self, key):
        self.sems[key] = self.nc.alloc_semaphore(key)
        self.cnt[key] = 0
        return key

    def buf(self, name):
        return Buf(name)

    def _deps(self, eng, reads, writes, same_ok=False, stream=False):
        need = {}

        def add(t):
            k, v, e = t[0], t[1], t[2]
            if e == eng and (same_ok or not STRICT_SAME[eng] or (stream and len(t) > 3 and t[3])):
                return
            if need.get(k, 0) < v:
                need[k] = v

        for b in reads:
            for t in b.w:
                add(t)
        for b in writes:
            for t in b.w:
                add(t)
            for t in b.r:
                add(t)
        out = []
        wd = self.waited[eng]
        for k, v in need.items():
            if wd.get(k, 0) < v:
                wd[k] = v
                out.append((k, v))
        return out

    def _commit(self, toks, reads, writes):
        for b in reads:
            b.r.extend(toks)
            if len(b.r) > 64:
                mx = {}
                for t in b.r:
                    k, v = t[0], t[1]
                    if k not in mx or mx[k][1] < v:
                        mx[k] = (k, v, t[2])
                b.r = list(mx.values())
        for b in writes:
            b.w = list(toks)
            b.r = []

    def op(self, eng, fn, reads=(), writes=(), same_ok=False, stream=False):
        stream = False
        waits = self._deps(eng, reads, writes, same_ok, stream)
        k = "e_" + eng
        self.cnt[k] += 1
        tok = (k, self.cnt[k], eng, stream)
        sems = self.sems

        def thunk(e, waits=waits, fn=fn, s=sems[k]):
            for (wk, wv) in waits:
                e.wait_ge(sems[wk], wv)
            fn(e).then_inc(s, 1)

        self.q[eng].append(thunk)
        self._commit([tok], reads, writes)
        self.n_ops += 1
        return tok

    def dma(self, eng, fns, reads=(), writes=()):
        if not isinstance(fns, (list, tuple)):
            fns = [fns]
        waits = self._deps(eng, reads, writes)
        wd = self.waited[eng]
        pool = self.dma_pool[eng]
        toks = []
        items = []
        for fn in fns:
            k = pool[self.dma_rr[eng] % len(pool)]
            self.dma_rr[eng] += 1
            prev = self.cnt[k]
            if prev > 0 and wd.get(k, 0) < prev:
                wd[k] = prev
                waits.append((k, prev))
            self.cnt[k] += 16
            toks.append((k, self.cnt[k], "dma"))
            items.append((fn, self.sems[k]))
        sems = self.sems

        def thunk(e, waits=waits, items=items):
            for (wk, wv) in waits:
                e.wait_ge(sems[wk], wv)
            for fn, s in items:
                fn(e).then_inc(s, 16)

        self.q[eng].append(thunk)
        self._commit(toks, reads, writes)
        self.n_ops += 1
        return toks

    def finish(self):
        nc = self.nc
        sems = self.sems
        cnt = dict(self.cnt)

        def fin_thunk(e):
            for k, v in cnt.items():
                if v > 0:
                    e.wait_ge(sems[k], v)

        self.q[SP].append(fin_thunk)
        q = self.q
        with nc.Block() as block:
            @block.tensor
            def _(e):
                for t in q[PE]:
                    t(e)

            @block.scalar
            def _(e):
                for t in q[ACT]:
                    t(e)

            @block.vector
            def _(e):
                for t in q[DVE]:
                    t(e)

            @block.gpsimd
            def _(e):
                for t in q[POOL]:
                    t(e)

            @block.sync
            def _(e):
                for t in q[SP]:
                    t(e)


D = 1024
NL = 4096
NCX = 256
NT = NL + NCX
PAD = 15
L0 = PAD
C0 = PAD + NL + 2 * PAD
CTW = 4416
NCT = 12
NRING = 7
EPS = 1e-6
DEPTH = 4
NEXP = 16
CAP_L = 512
CAP_C = 32
NSLOT = CAP_L + CAP_C
BLKS = [(L0 + 512 * j, 512 * j, 512) for j in range(8)] + [(C0, NL, NCX)]

V_NMG = 0
V_NFG = 32
V_BMOD = 64
V_EV = 256
V_OD = 532
V_CC = 564
NV = 580


def build_program(n_layers=DEPTH, debug=False, stop=None, small_moe=False, start_layer=0, moe_decl=None):
    nc = bass.Bass("TRN2", target_bir_lowering=False)
    P = Prog(nc)

    def din(name, shape, dt=F32):
        return nc.dram_tensor(name, list(shape), dt, kind="ExternalInput")

    xT = din("xT", [D, NL])
    ctxT = din("ctxT", [D, NCX])
    vecs = din("vecs", [128, NV])
    cmats = din("cmats", [128, 5 * 128])
    rope_cs = din("rope_cs", [128, 2 * NL])
    pool_fix = din("pool_fix", [128, 64])
    zeros_d = din("zeros_d", [NT, D])
    w_mod = din("w_mod", [DEPTH, D, 6 * D])
    ev_w_in = din("ev_w_in", [2, D, 1792])
    ev_w_out = din("ev_w_out", [2, D, D])
    od_w_in = din("od_w_in", [2, D, 2048])
    od_w_out = din("od_w_out", [2, D, D])
    od_pool_w = din("od_pool_w", [2, 4, 128, 128])
    w_router = din("w_router", [DEPTH, D, NEXP])
    mshape = [1, 1] if small_moe else [DEPTH if moe_decl is None else len(moe_decl), NEXP]
    mmap = {l: l for l in range(DEPTH)} if moe_decl is None else {l: n for n, l in enumerate(moe_decl)}
    w_gate = din("w_gate", mshape + [D, 2048])
    w_up = din("w_up", mshape + [D, 2048])
    w_down = din("w_down", mshape + [2048, D])
    outT = nc.dram_tensor("outT", [D, NL], F32, kind="ExternalOutput")
    XA = nc.dram_tensor("XA", [D, NT], F32)
    HFTOK = nc.dram_tensor("HFTOK", [NT, D], BF16)
    MOQ = [nc.dram_tensor("MO%d" % q, [NT, 256], F32) for q in range(4)]
    dbg = {}
    if debug:
        dbg["xa"] = nc.dram_tensor("dbg_xa", [D, NT], F32, kind="ExternalOutput")

    XAb = [[P.buf("xa%d_%d" % (c, j)) for j in range(9)] for c in range(8)]
    HFb = [P.buf("hftok%d" % t) for t in range(34)]
    MOb = [P.buf("mo%d" % q) for q in range(4)]
    OUTb = P.buf("out")

    CT = nc.alloc_sbuf_tensor("ct", [128, NCT * CTW], BF16)
    CTF = CT.bitcast(F32)
    CTb = [[[P.buf("ct%d_%d_%d" % (i, j, h)) for h in range(2)] for j in range(9)] for i in range(NCT)]
    RING = nc.alloc_sbuf_tensor("ring", [128, NRING * 4096], BF16)
    RINGb = [P.buf("ring%d" % s) for s in range(NRING)]
    RSTD = nc.alloc_sbuf_tensor("rstd", [128, NT], F32)
    RSTDb = [P.buf("rstd%d" % j) for j in range(9)]
    WT = nc.alloc_sbuf_tensor("wt", [128, 12 * 512], F32)
    WTH = WT.bitcast(BF16)
    WTI = WT.bitcast(I32)
    WTU = WT.bitcast(U32)
    WTb = [P.buf("wt%d" % k) for k in range(12)]
    CM = nc.alloc_sbuf_tensor("cm", [128, 4 * 128], BF16)
    CMb = P.buf("cm")
    IDF = nc.alloc_sbuf_tensor("idf", [128, 128], F32)
    IDFb = P.buf("idf")
    ONEF = nc.alloc_sbuf_tensor("onef", [16, 16], F32)
    ONEFb = P.buf("onef")
    VEC = nc.alloc_sbuf_tensor("vec", [128, NV], F32)
    VECb = P.buf("vec")
    MODS = nc.alloc_sbuf_tensor("mods", [128, DEPTH * 96], F32)
    MODSb = P.buf("mods")
    DER = nc.alloc_sbuf_tensor("der", [128, 96], F32)
    DERb = P.buf("der")
    SCC = nc.alloc_sbuf_tensor("scc", [128, 16], BF16)
    SCCb = P.buf("scc")
    WR = nc.alloc_sbuf_tensor("wr", [128, 8 * NEXP], F32)
    WRb = P.buf("wr")
    AFFT = nc.alloc_sbuf_tensor("afft", [128, 5 * NEXP], F32)
    AFFTb = P.buf("afft")
    IDXT = nc.alloc_sbuf_tensor("idxt", [128, 5 * NEXP], I32)
    IDXTb = P.buf("idxt")
    PFIX = nc.alloc_sbuf_tensor("pfix", [128, 64], F32)
    PFIXb = P.buf("pfix")
    PSD = [nc.alloc_psum_tensor("psd%d" % k, [128, 1024], F32) for k in range(4)]
    PSDH = [p.bitcast(BF16) for p in PSD]
    PS = [PSD[k // 2][:, (k % 2) * 512:(k % 2 + 1) * 512] for k in range(8)]
    PSH = [PSDH[k // 2][:, (k % 2) * 1024:(k % 2 + 1) * 1024] for k in range(8)]
    PSb = [P.buf("ps%d" % k) for k in range(8)]

    def ct(i, a, b):
        return CT[:, i * CTW + a: i * CTW + b]

    def ctp(i, a, b, p0, p1):
        return CT[p0:p1, i * CTW + a: i * CTW + b]

    def cb(i, j):
        return CTb[i][j]

    def wt(k, w=512):
        return WT[:, k * 512: k * 512 + w]

    def wth(k, a=0, b=1024):
        return WTH[:, k * 1024 + a: k * 1024 + b]

    def ring(s, a=0, b=4096):
        return RING[:, s * 4096 + a: s * 4096 + b]

    def ring3(s, c):
        return RING[:, s * 4096:(s + 1) * 4096].rearrange("p (c n) -> p c n", c=c)

    def cm(k):
        return CM[:, k * 128:(k + 1) * 128]

    IDB, ONESB, BLK1, ROT = 0, 1, 2, 3

    def vcol(k, n=1):
        return VEC[:, k:k + n]

    def MM(out, pairs, reads, writes):
        def fn(e, out=out, pairs=pairs):
            n = len(pairs)
            last = None
            for t, (l, r) in enumerate(pairs):
                last = e.matmul(out, lhsT=l, rhs=r, start=(t == 0), stop=(t == n - 1))
            return last
        return P.op(PE, fn, reads, writes)

    def TR(out, in_, ident, reads, writes):
        return P.op(PE, lambda e: e.transpose(out=out, in_=in_, identity=ident), reads, writes)

    def TRS(items, reads, writes):
        def fn(e, items=items):
            last = None
            for (o, i_, idn) in items:
                last = e.transpose(out=o, in_=i_, identity=idn)
            return last
        return P.op(PE, fn, reads, writes)

    def big(ap):
        sh = ap.shape
        return len(sh) == 2 and sh[1] >= 256

    def A(out, in_, func, reads, writes, scale=1.0, bias=0.0, accum=None):
        if accum is None:
            return P.op(ACT, lambda e: e.activation(out=out, in_=in_, func=func, bias=bias, scale=scale), reads, writes,
                        stream=big(out) and big(in_))
        return P.op(ACT, lambda e: e.activation(out=out, in_=in_, func=func, bias=bias, scale=scale, accum_out=accum), reads, writes)

    def TT(out, in0, in1, op, reads, writes, eng=DVE):
        return P.op(eng, lambda e: e.tensor_tensor(out=out, in0=in0, in1=in1, op=op), reads, writes,
                    stream=(eng == DVE) and big(out) and big(in0) and big(in1))

    def TS(out, in0, s1, s2, op0, op1, reads, writes, eng=DVE):
        if s2 is None:
            return P.op(eng, lambda e: e.tensor_scalar(out, in0, s1, None, op0=op0), reads, writes, stream=(eng == DVE) and big(out) and big(in0))
        return P.op(eng, lambda e: e.tensor_scalar(out, in0, s1, s2, op0=op0, op1=op1), reads, writes, stream=(eng == DVE) and big(out) and big(in0))

    def STT(out, in0, scalar, in1, op0, op1, reads, writes, eng=DVE):
        return P.op(eng, lambda e: e.scalar_tensor_tensor(out=out, in0=in0, scalar=scalar, in1=in1, op0=op0, op1=op1), reads, writes,
                    stream=(eng == DVE) and big(out) and big(in0) and big(in1))

    def CP(out, in_, reads, writes, eng=DVE):
        return P.op(eng, lambda e: e.tensor_copy(out, in_), reads, writes, stream=(eng == DVE) and big(out) and big(in_))

    def RCP(out, in_, reads, writes):
        return P.op(DVE, lambda e: e.reciprocal(out=out, in_=in_), reads, writes, stream=big(out) and big(in_))

    def MSET(ap, val, writes, eng=POOL):
        return P.op(eng, lambda e: e.memset(ap, val), (), writes)

    def LD(eng, out, in_, reads, writes):
        return P.dma(eng, lambda e: e.dma_start(out=out, in_=in_), reads, writes)

    LD(POOL, CM[:, :], cmats[:, 0:512], [], [CMb])
    LD(SP, IDF[:, :], cmats[:, 0:128], [], [IDFb])
    LD(SP, ONEF[:, :], cmats[0:16, 128:144], [], [ONEFb])
    LD(SP, VEC[:, :], vecs[:, :], [], [VECb])
    LD(SP, PFIX[:, :], pool_fix[:, :], [], [PFIXb])
    def zero_mo(reads):
        zsrc = zeros_d[0:NT // 4, :].rearrange("(a b) d -> a (b d)", a=16)
        for q in range(4):
            LD(SP, MOQ[q][:, :].rearrange("(a b) d -> a (b d)", a=16), zsrc, [MOb[q]] if reads else [], [MOb[q]])
    zero_mo(False)
    for i in range(NCT):
        MSET(ct(i, 0, CTW), 0.0, [b for j in range(9) for b in cb(i, j)])
    for k in range(12):
        MSET(wt(k), 0.0, [WTb[k]])

    A(WT[:, 0:16], VEC[:, V_CC:V_CC + 16], AF.Silu, [VECb, WTb[0]], [WTb[0]])
    CP(SCC[:, :], WT[:, 0:16], [WTb[0]], [SCCb])
    scc3 = SCC[:, :].rearrange("p (c v) -> p c v", v=2)
    pcount = [0]

    def mods_mm(i):
        for n in range(12):
            s_ = pcount[0] % NRING
            pcount[0] += 1
            LD(POOL, ring3(s_, 8), w_mod[i].rearrange("(c p) n -> p c n", p=128)[:, :, n * 512:(n + 1) * 512], [], [RINGb[s_]])
            r3 = ring3(s_, 8)
            for oc in range(4):
                k = n * 4 + oc
                MM(PS[0][:, k * 2:k * 2 + 2], [(r3[:, c, oc * 128:(oc + 1) * 128], scc3[:, c, :]) for c in range(8)],
                   [RINGb[s_], SCCb], [PSb[0]])

    def mods_fin(i):
        m3 = MODS[:, i * 96:(i + 1) * 96].rearrange("p (k v) -> p k v", v=2)
        p3 = PS[0][:, 0:96].rearrange("p (k v) -> p k v", v=2)
        for v in range(2):
            TT(m3[:, :, v], p3[:, :, v], VEC[:, V_BMOD + i * 48: V_BMOD + (i + 1) * 48], ALU.add, [PSb[0], VECb], [MODSb])

    mods_mm(start_layer)
    mods_fin(start_layer)

    def mod(i, grp, c, v):
        k = grp * 8 + c
        return MODS[:, i * 96 + k * 2 + v: i * 96 + k * 2 + v + 1]

    def der_setup(i):
        d3 = DER[:, 0:32].rearrange("p (g v c) -> p g v c", g=2, v=2)
        m4 = MODS[:, i * 96:(i + 1) * 96].rearrange("p (g c v) -> p g c v", g=6, v=2)
        for v in range(2):
            STT(d3[:, 0, v, :], m4[:, 1, :, v], 1.0, VEC[:, V_NMG + i * 8: V_NMG + i * 8 + 8], ALU.add, ALU.mult, [MODSb, VECb], [DERb])
            STT(d3[:, 1, v, :], m4[:, 4, :, v], 1.0, VEC[:, V_NFG + i * 8: V_NFG + i * 8 + 8], ALU.add, ALU.mult, [MODSb, VECb], [DERb])

    def G1(c, v):
        return DER[:, v * 8 + c: v * 8 + c + 1]

    def G2(c, v):
        return DER[:, 16 + v * 8 + c: 16 + v * 8 + c + 1]

    SS = 7

    sqrr = [0]

    def stat_chunk(j, c, xs_ap, xs_buf, w):
        k = 3 + (sqrr[0] % 2)
        sqrr[0] += 1
        A(wth(k, 0, w), xs_ap, AF.Square, [xs_buf], [WTb[k]])
        return k

    def stat_mm(j, c, k, w):
        P.op(PE, lambda e: e.matmul(PS[SS][:, 0:w], lhsT=cm(ONESB), rhs=wth(k, 0, w), start=(c == 0), stop=(c == 7)),
             [CMb, WTb[k]], [PSb[SS]])

    def stat_fin(j, w):
        xcol = BLKS[j][1]
        A(wt(5, w), PS[SS][:, 0:w], AF.Sqrt, [PSb[SS]], [WTb[5]], scale=1.0 / D, bias=EPS)
        RCP(RSTD[:, xcol:xcol + w], wt(5, w), [WTb[5]], [RSTDb[j]])

    def blocks_for(use_ctx):
        return list(range(9)) if use_ctx else list(range(8))

    xsrr = [0]

    def next_xs():
        k = xsrr[0] % 3
        xsrr[0] += 1
        return k

    for j in range(9):
        cs, xc, w = BLKS[j]
        pend = None
        for c in range(8):
            k = next_xs()
            src = xT[c * 128:(c + 1) * 128, xc:xc + w] if j < 8 else ctxT[c * 128:(c + 1) * 128, 0:w]
            LD(SP, wt(k, w), src, [], [WTb[k]])
            LD(SP, XA[c * 128:(c + 1) * 128, xc:xc + w], wt(k, w), [WTb[k]], [XAb[c][j]])
            kk = stat_chunk(j, c, wt(k, w), WTb[k], w)
            if pend is not None:
                stat_mm(j, *pend, w)
            pend = (c, kk)
        stat_mm(j, *pend, w)
        stat_fin(j, w)

    def chk(name):
        if stop == name:
            raise _Stop()

    HB = [WTH[:, 8 * 1024: 12 * 1024], RING[:, 6 * 4096: 7 * 4096]]
    HBb = [[WTb[8], WTb[9], WTb[10], WTb[11]], [RINGb[6]]]
    hbrr = [0]

    def build_h(i, j, which):
        cs, xc, w = BLKS[j]
        v = 1 if j == 8 else 0
        hb_i = hbrr[0] % 2 if "hb0" not in KDBG else 0
        hbrr[0] += 1
        hb3 = HB[hb_i].rearrange("p (c n) -> p c n", c=8)
        for c in range(8):
            k = next_xs()
            LD(SP, wt(k, w), XA[c * 128:(c + 1) * 128, xc:xc + w], [XAb[c][j]], [WTb[k]])
            TT(wt(k, w), wt(k, w), RSTD[:, xc:xc + w], ALU.mult, [WTb[k], RSTDb[j]], [WTb[k]])
            if which == 1:
                A(hb3[:, c, 0:w], wt(k, w), AF.Identity, [WTb[k], DERb, MODSb] + HBb[hb_i], HBb[hb_i],
                  scale=G1(c, v), bias=mod(i, 0, c, v))
            else:
                A(hb3[:, c, 0:w], wt(k, w), AF.Identity, [WTb[k], DERb, MODSb] + HBb[hb_i], HBb[hb_i],
                  scale=G2(c, v), bias=mod(i, 3, c, v))
        return hb3, HBb[hb_i]

    def load_w_slot(slot, src3, ncols=512, dst_off=0):
        LD(POOL, ring3(slot, 8)[:, :, dst_off:dst_off + ncols], src3, [], [RINGb[slot]])

    def w3(wh):
        return wh.rearrange("(c p) n -> p c n", p=128)

    def outproj_residual(i, mix_tiles, wslots, use_ctx, gate_grp):
        for j in blocks_for(use_ctx):
            cs, xc, w = BLKS[j]
            v = 1 if j == 8 else 0
            pend = None
            for c in range(8):
                slot = wslots[c // 4]
                r3 = ring3(slot, 8)
                pb = c % 2
                MM(PS[pb][:, 0:w], [(r3[:, k, (c % 4) * 128:(c % 4 + 1) * 128], ct(mix_tiles[k], cs, cs + w)) for k in range(8)],
                   [RINGb[slot]] + [b for k in range(8) for b in cb(mix_tiles[k], j)], [PSb[pb]])
                kx = next_xs()
                LD(SP, wt(kx, w), XA[c * 128:(c + 1) * 128, xc:xc + w], [XAb[c][j]], [WTb[kx]])
                STT(wt(kx, w), PS[pb][:, 0:w], mod(i, gate_grp, c, v), wt(kx, w), ALU.mult, ALU.add,
                    [PSb[pb], WTb[kx], MODSb], [WTb[kx]])
                LD(SP, XA[c * 128:(c + 1) * 128, xc:xc + w], wt(kx, w), [WTb[kx]], [XAb[c][j]])
                kk = stat_chunk(j, c, wt(kx, w), WTb[kx], w)
                if pend is not None:
                    stat_mm(j, *pend, w)
                pend = (c, kk)
            stat_mm(j, *pend, w)
            stat_fin(j, w)

    U_T = [0, 1, 2, 3]
    CO_T = [4, 5, 6, 7]
    Q_T = [0, 1, 2, 3]
    K_T = 8
    V_T = 9
    MISC_T = 11

    def vtok(kt, a, b):
        base = V_T * CTW + kt * 256
        return CT[:, base + a: base + b]

    VTb = [P.buf("vt%d" % kt) for kt in range(34)]

    RINGF = RING.bitcast(F32)
    COS_AP = RINGF[:, 4 * 2048: 4 * 2048 + 512]
    SIN_AP = RINGF[:, 4 * 2048 + 512: 4 * 2048 + 1024]
    KP_T = [K_T, MISC_T]

    def zero_pads(tiles):
        for t in tiles:
            MSET(ct(t, 0, PAD), 0.0, cb(t, 0))
            MSET(ct(t, L0 + NL, C0), 0.0, cb(t, 7) + cb(t, 8))
            MSET(ct(t, C0 + NCX, CTW), 0.0, cb(t, 8))

    def even_mixer(i):
        jj = i // 2
        ctx_full = i < 2
        ve = V_EV + jj * 138
        zero_pads(U_T)
        wi = w3(ev_w_in[jj])
        load_w_slot(0, wi[:, :, 0:512])
        load_w_slot(1, wi[:, :, 512:1024])
        for cq in range(4):
            for half in range(2):
                hq = half * 4 + cq
                load_w_slot(2, wi[:, :, 1024 + hq * 64: 1024 + (hq + 1) * 64], 64, cq * 128 + half * 64)
        load_w_slot(3, wi[:, :, 1536:1792], 256, 0)
        for j in blocks_for(ctx_full):
            cs, xc, w = BLKS[j]
            hb3, hbb = build_h(i, j, 1)
            for c in range(4):
                MM(PS[0][:, 0:w], [(ring3(0, 8)[:, k, c * 128:(c + 1) * 128], hb3[:, k, 0:w]) for k in range(8)],
                   [RINGb[0]] + hbb, [PSb[0]])
                MM(PS[1][:, 0:w], [(ring3(1, 8)[:, k, c * 128:(c + 1) * 128], hb3[:, k, 0:w]) for k in range(8)],
                   [RINGb[1]] + hbb, [PSb[1]])
                A(wt(5, w), PS[1][:, 0:w], AF.Sigmoid, [PSb[1]], [WTb[5]])
                TT(ct(U_T[c], cs, cs + w), PS[0][:, 0:w], wt(5, w), ALU.mult, [PSb[0], WTb[5]], cb(U_T[c], j))
        chk("passA")
        DS = [4, 5, 6, 0]
        for c in range(4 if "nodiag" not in KDBG else 0):
            for t in range(31):
                TS(ring(DS[c], t * 128, (t + 1) * 128), cm(IDB), VEC[:, ve + 14 + c * 31 + t: ve + 14 + c * 31 + t + 1], None,
                   ALU.mult, None, [CMb, VECb], [RINGb[DS[c]]], eng=POOL if (t % 2) else DVE)
        wo = w3(ev_w_out[jj])
        def load_wout(slot, col0):
            LD(POOL, ring3(slot, 8)[:, 0:4, :], wo[:, 0:4, col0:col0 + 512], [], [RINGb[slot]])
            woh = ev_w_out[jj][512:1024, col0:col0 + 512].rearrange("(h p) n -> p h n", p=64)
            for half in range(2):
                LD(POOL, RING[half * 64:(half + 1) * 64, slot * 4096:(slot + 1) * 4096].rearrange("p (c n) -> p c n", c=8)[:, 4:8, :],
                   woh[:, half * 4: half * 4 + 4, :], [], [RINGb[slot]])
        load_wout(1, 0)
        for j in (blocks_for(ctx_full) if "noconv" not in KDBG else []):
            cs, xc, w = BLKS[j]
            for c in range(4):
                MM(PS[c % 2][:, 0:w], [(ring(DS[c], t * 128, (t + 1) * 128), ct(U_T[c], cs + t - 15, cs + t - 15 + w)) for t in range(31)],
                   [RINGb[DS[c]]] + [b for jn in ((8,) if j == 8 else (max(j - 1, 0), j, min(j + 1, 7))) for b in cb(U_T[c], jn)], [PSb[c % 2]])
                A(wt(5 + c, w), PS[c % 2][:, 0:w], AF.Identity, [PSb[c % 2], VECb], [WTb[5 + c]], bias=VEC[:, ve + c: ve + c + 1])
                A(wth(3, 0, w), wt(5 + c, w), AF.Square, [WTb[5 + c]], [WTb[3]])
                CP(wth(4, 0, w), wt(5 + c, w), [WTb[5 + c]], [WTb[4]])
                P.op(PE, lambda e, c=c, w=w: e.matmul(PS[2][:, 0:w], lhsT=cm(ONESB), rhs=wth(4, 0, w), start=(c == 0), stop=(c == 3)),
                     [CMb, WTb[4]], [PSb[2]])
                P.op(PE, lambda e, c=c, w=w: e.matmul(PS[3][:, 0:w], lhsT=cm(ONESB), rhs=wth(3, 0, w), start=(c == 0), stop=(c == 3)),
                     [CMb, WTb[3]], [PSb[3]])
            TS(wt(9, w), PS[2][:, 0:w], 1.0 / 512, None, ALU.mult, None, [PSb[2]], [WTb[9]])
            TT(wt(10, w), wt(9, w), wt(9, w), ALU.mult, [WTb[9]], [WTb[10]])
            STT(wt(10, w), PS[3][:, 0:w], 1.0 / 512, wt(10, w), ALU.mult, ALU.subtract, [PSb[3], WTb[10]], [WTb[10]])
            A(wt(10, w), wt(10, w), AF.Sqrt, [WTb[10]], [WTb[10]], bias=EPS)
            RCP(wt(10, w), wt(10, w), [WTb[10]], [WTb[10]])
            for c in range(4):
                TT(wt(5 + c, w), wt(5 + c, w), wt(9, w), ALU.subtract, [WTb[5 + c], WTb[9]], [WTb[5 + c]])
                TT(wt(5 + c, w), wt(5 + c, w), wt(10, w), ALU.mult, [WTb[5 + c], WTb[10]], [WTb[5 + c]])
                A(ct(CO_T[c], cs, cs + w), wt(5 + c, w), AF.Silu, [WTb[5 + c], VECb], cb(CO_T[c], j),
                  scale=VEC[:, ve + 4 + c: ve + 5 + c], bias=VEC[:, ve + 8 + c: ve + 9 + c])
        chk("conv")
        TS(DER[:, 40:41], VEC[:, ve + 12: ve + 13], 0.125, None, ALU.mult, None, [VECb], [DERb])
        CP(DER[:, 41:42], VEC[:, ve + 13: ve + 14], [VECb], [DERb])
        MSET(ct(MISC_T, 0, CTW), 0.0, [b for jx in range(9) for b in cb(MISC_T, jx)], eng=DVE)
        for kt in range(34 if "noones" not in KDBG else 0):
            MSET(CT[:, V_T * CTW + kt * 256: V_T * CTW + (kt + 1) * 256].rearrange("p (h n) -> p h n", h=2)[:, :, 64:128], 1.0, [VTb[kt]], eng=DVE)
        for j in range(int(os.environ.get("PBN", "9"))):
            cs, xc, w = BLKS[j]
            is_ctx = (j == 8)
            hb3, hbb = build_h(i, j, 1)
            if "nochunks" in KDBG:
                continue
            if not is_ctx and "noropeld" not in KDBG:
                LD(SP, COS_AP[:, 0:w], rope_cs[:, xc:xc + w], [], [RINGb[4]])
                LD(SP, SIN_AP[:, 0:w], rope_cs[:, NL + xc: NL + xc + w], [], [RINGb[4]])
            chunks = []
            if (not is_ctx) or ctx_full:
                chunks += [("q", cq) for cq in range(4)]
            chunks += [("k", 0)]
            for kind, cq in chunks:
                if kind == "q":
                    lw = [(ring3(2, 8)[:, k, cq * 128:(cq + 1) * 128], hb3[:, k, 0:w]) for k in range(8)]
                    dst, dstb, gcol = ct(Q_T[cq], cs, cs + w), cb(Q_T[cq], j), 40
                    rb = [RINGb[2]]
                else:
                    lw = [(ring3(3, 8)[:, k, 0:128], hb3[:, k, 0:w]) for k in range(8)]
                    dst, dstb, gcol = ct(K_T, cs, cs + w), cb(K_T, j), 41
                    rb = [RINGb[3]]
                MM(PS[0][:, 0:w], lw, rb + hbb, [PSb[0]])
                CP(wt(5, w), PS[0][:, 0:w], [PSb[0]], [WTb[5]])
                A(wth(3, 0, w), wt(5, w), AF.Square, [WTb[5]], [WTb[3]])
                MM(PS[1][:, 0:w], [(cm(BLK1), wth(3, 0, w))], [CMb, WTb[3]], [PSb[1]])
                A(wt(6, w), PS[1][:, 0:w], AF.Sqrt, [PSb[1]], [WTb[6]], scale=1.0 / 64, bias=EPS)
                RCP(wt(6, w), wt(6, w), [WTb[6]], [WTb[6]])
                if "noqk" in KDBG:
                    continue
                if is_ctx or "norope" in KDBG:
                    STT(dst, wt(5, w), DER[:, gcol:gcol + 1], wt(6, w), ALU.mult, ALU.mult, [WTb[5], WTb[6], DERb], dstb)
                else:
                    STT(wt(5, w), wt(5, w), DER[:, gcol:gcol + 1], wt(6, w), ALU.mult, ALU.mult, [WTb[5], WTb[6], DERb], [WTb[5]])
                    A(wth(4, 0, w), wt(5, w), AF.Copy, [WTb[5]], [WTb[4]])
                    MM(PS[2][:, 0:w], [(cm(ROT), wth(4, 0, w))], [CMb, WTb[4]], [PSb[2]])
                    TT(wt(5, w), wt(5, w), COS_AP[:, 0:w], ALU.mult, [WTb[5], RINGb[4]], [WTb[5]])
                    TT(wt(6, w), PS[2][:, 0:w], SIN_AP[:, 0:w], ALU.mult, [PSb[2], RINGb[4]], [WTb[6]])
                    TT(dst, wt(5, w), wt(6, w), ALU.add, [WTb[5], WTb[6]], dstb)
                if kind == "k":
                    CP(ctp(MISC_T, cs, cs + w, 64, 128), ctp(K_T, cs, cs + w, 64, 128), [CTb[K_T][j][1]], [CTb[MISC_T][j][1]])
                    MSET(ctp(K_T, cs, cs + w, 64, 128), 0.0, [CTb[K_T][j][1]], eng=DVE)
            for tt in range(w // 128 if "nov" not in KDBG else 0):
                kt = (32 + tt) if is_ctx else (j * 4 + tt)
                pb = 4 + (tt % 2)
                MM(PS[pb][:, 0:128], [(hb3[:, k, tt * 128:(tt + 1) * 128], ring3(3, 8)[:, k, 128:256]) for k in range(8)],
                   [RINGb[3]] + hbb, [PSb[pb]])
                CP(vtok(kt, 0, 256).rearrange("p (h n) -> p h n", h=2)[:, :, 0:64],
                   PS[pb][:, 0:128].rearrange("p (h n) -> p h n", h=2), [PSb[pb]], [VTb[kt]])
        chk("passB")
        load_wout(2, 512)
        items = []
        for j in blocks_for(ctx_full):
            is_ctx = (j == 8)
            kts = [32, 33] if is_ctx else [32, 33] + list(range(32))
            prs = [(kts[n], kts[n + 1]) for n in range(0, len(kts), 2)]
            for cq in range(4):
                for half in range(2):
                    for n, pr in enumerate(prs):
                        items.append((j, cq, half, pr, n == 0, n == len(prs) - 1))
        LA = 3
        nit = len(items)
        for step in range(nit + LA):
            if step < nit:
                j, cq, half, pr, first, last = items[step]
                cs, xc, w = BLKS[j]
                sd = step % 3
                sbufs = [PSb[2 * sd], PSb[2 * sd + 1]]
                pairs_mm = []
                rd = list(cb(Q_T[cq], j))
                for n, kt in enumerate(pr):
                    kcol = (C0 + (kt - 32) * 128) if kt >= 32 else (L0 + kt * 128)
                    kj = 8 if kt >= 32 else kt // 4
                    pairs_mm.append((PSD[sd][:, n * w:(n + 1) * w], ct(KP_T[half], kcol, kcol + 128)))
                    rd += cb(KP_T[half], kj)
                qap = ct(Q_T[cq], cs, cs + w)

                def smm(e, pairs_mm=pairs_mm, qap=qap):
                    last_i = None
                    for (o, kap) in pairs_mm:
                        last_i = e.matmul(o, lhsT=kap, rhs=qap, start=True, stop=True)
                    return last_i
                P.op(PE, smm, rd, sbufs)
                pk = step % 4
                A(wth(pk, 0, 2 * w), PSD[sd][:, 0:2 * w], AF.Exp, sbufs, [WTb[pk]])
            if step >= LA:
                j, cq, half, pr, first, last = items[step - LA]
                cs, xc, w = BLKS[j]
                pk = (step - LA) % 4
                ob = 6 + half

                def pvmm(e, ob=ob, w=w, pr=pr, half=half, pk=pk, first=first, last=last):
                    last_i = None
                    for n, kt in enumerate(pr):
                        last_i = e.matmul(PS[ob][:, 0:w], lhsT=vtok(kt, half * 128, half * 128 + 128), rhs=wth(pk, n * w, (n + 1) * w),
                                          start=(first and n == 0), stop=(last and n == len(pr) - 1))
                    return last_i
                P.op(PE, pvmm, [VTb[kt] for kt in pr] + [WTb[pk]], [PSb[ob]])
                if last:
                    RCP(WT[64:128, 5 * 512: 5 * 512 + w], PS[ob][64:128, 0:w], [PSb[ob]], [WTb[5]])
                    TT(ctp(Q_T[cq], cs, cs + w, half * 64, half * 64 + 64), PS[ob][0:64, 0:w], WT[64:128, 5 * 512: 5 * 512 + w], ALU.mult,
                       [PSb[ob], WTb[5]], [CTb[Q_T[cq]][j][half]])
        chk("attn")
        outproj_residual(i, CO_T + Q_T, [1, 2], ctx_full, 2)


    def odd_mixer(i):
        jj = i // 2
        ctx_full = i < 2
        vo = V_OD + jj * 16
        wi = w3(od_w_in[jj])
        PIN_T = [0, 1, 2, 3]
        PO_T = [4, 5, 6, 7]
        SC_T = [0, 1, 2, 3]
        GB_T = [8, 9, 10, 11]
        zero_pads(PIN_T)
        load_w_slot(3, wi[:, :, 1536:2048])
        load_w_slot(0, wi[:, :, 0:512])
        load_w_slot(1, wi[:, :, 512:1024])
        load_w_slot(2, wi[:, :, 1024:1536])
        PWT = ring(4, 20 * 128, 24 * 128)
        PWTb = RINGb[4]
        LD(POOL, PWT.rearrange("p (g n) -> p g n", g=4), od_pool_w[jj].rearrange("g p n -> p g n"), [], [PWTb])
        WIN = [2, 4, 8, 16]
        tapmat = {}
        tcount = 0
        for gi, wv in enumerate(WIN):
            for val in (1.0 / wv, 1.0 / wv - 1.0):
                TS(ring(4, tcount * 128, (tcount + 1) * 128), cm(IDB), float(val), None, ALU.mult, None, [CMb], [RINGb[4]])
                tapmat[(gi, val == 1.0 / wv)] = tcount
                tcount += 1
        for c in range(4):
            for t in range(3):
                TS(ring(4, (8 + c * 3 + t) * 128, (9 + c * 3 + t) * 128), cm(IDB), VEC[:, vo + c * 3 + t: vo + c * 3 + t + 1], None,
                   ALU.mult, None, [CMb, VECb], [RINGb[4]])
        for j in blocks_for(ctx_full):
            cs, xc, w = BLKS[j]
            hb3, hbb = build_h(i, j, 1)
            for c in range(4):
                MM(PS[c % 2][:, 0:w], [(ring3(3, 8)[:, k, c * 128:(c + 1) * 128], hb3[:, k, 0:w]) for k in range(8)],
                   [RINGb[3]] + hbb, [PSb[c % 2]])
                A(ct(PIN_T[c], cs, cs + w), PS[c % 2][:, 0:w], AF.Copy, [PSb[c % 2]], cb(PIN_T[c], j))
        wo = w3(od_w_out[jj])
        load_w_slot(5, wo[:, :, 0:512])
        load_w_slot(3, wo[:, :, 512:1024])
        for j in blocks_for(ctx_full):
            cs, xc, w = BLKS[j]
            seq0, seqn = (C0, NCX) if j == 8 else (L0, NL)
            for gi, wv in enumerate(WIN):
                taps = list(range(-wv // 2, wv // 2))
                nb = [b for jn in ((8,) if j == 8 else (max(j - 1, 0), j, min(j + 1, 7))) for b in cb(PIN_T[gi], jn)]
                MM(PS[gi % 2][:, 0:w], [(ring(4, tapmat[(gi, t != 0)] * 128, (tapmat[(gi, t != 0)] + 1) * 128),
                                         ct(PIN_T[gi], cs + t, cs + t + w)) for t in taps], [RINGb[4]] + nb, [PSb[gi % 2]])
                dk = 5 + (gi % 2)
                CP(wth(dk, 0, w), PS[gi % 2][:, 0:w], [PSb[gi % 2]], [WTb[dk]])
                h2 = wv // 2
                fix = []
                if cs == seq0:
                    fix.append((0, h2, gi * 16))
                if cs + w == seq0 + seqn:
                    fix.append((w - (h2 - 1), h2 - 1, gi * 16 + 8))
                for (o, n, fc) in fix:
                    if n <= 0:
                        continue
                    TT(wt(7, 16)[:, 0:n], wth(dk, o, o + n), ct(PIN_T[gi], cs + o, cs + o + n), ALU.add, [WTb[dk]] + cb(PIN_T[gi], j), [WTb[7]])
                    TT(wt(7, 16)[:, 0:n], wt(7, 16)[:, 0:n], PFIX[:, fc:fc + n], ALU.mult, [WTb[7], PFIXb], [WTb[7]])
                    TT(wth(dk, o, o + n), wt(7, 16)[:, 0:n], ct(PIN_T[gi], cs + o, cs + o + n), ALU.subtract, [WTb[7]] + cb(PIN_T[gi], j), [WTb[dk]])
                MM(PS[2 + gi % 2][:, 0:w], [(PWT[:, gi * 128:(gi + 1) * 128], wth(dk, 0, w))], [PWTb, WTb[dk]], [PSb[2 + gi % 2]])
                A(ct(PO_T[gi], cs, cs + w), PS[2 + gi % 2][:, 0:w], AF.Identity, [PSb[2 + gi % 2], VECb], cb(PO_T[gi], j),
                  scale=VEC[:, vo + 12 + gi: vo + 13 + gi])
        for j in blocks_for(ctx_full):
            cs, xc, w = BLKS[j]
            hb3, hbb = build_h(i, j, 1)
            for c in range(4):
                MM(PS[0][:, 0:w], [(ring3(0, 8)[:, k, c * 128:(c + 1) * 128], hb3[:, k, 0:w]) for k in range(8)], [RINGb[0]] + hbb, [PSb[0]])
                MM(PS[1][:, 0:w], [(ring3(2, 8)[:, k, c * 128:(c + 1) * 128], hb3[:, k, 0:w]) for k in range(8)], [RINGb[2]] + hbb, [PSb[1]])
                MM(PS[2][:, 0:w], [(ring3(1, 8)[:, k, c * 128:(c + 1) * 128], hb3[:, k, 0:w]) for k in range(8)], [RINGb[1]] + hbb, [PSb[2]])
                A(wt(5, w), PS[0][:, 0:w], AF.Copy, [PSb[0]], [WTb[5]])
                TT(ct(SC_T[c], cs, cs + w), PS[1][:, 0:w], wt(5, w), ALU.mult, [PSb[1], WTb[5]], cb(SC_T[c], j))
                A(ct(GB_T[c], cs, cs + w), PS[2][:, 0:w], AF.Copy, [PSb[2]], cb(GB_T[c], j))
        for j in blocks_for(ctx_full):
            cs, xc, w = BLKS[j]
            for c in range(4):
                nb = [b for jn in ((8,) if j == 8 else (max(j - 1, 0), j, min(j + 1, 7))) for b in cb(SC_T[c], jn)]
                MM(PS[c % 2][:, 0:w], [(ring(4, (8 + c * 3 + t) * 128, (9 + c * 3 + t) * 128), ct(SC_T[c], cs + t - 1, cs + t - 1 + w)) for t in range(3)],
                   [RINGb[4]] + nb, [PSb[c % 2]])
                TT(ct(GB_T[c], cs, cs + w), PS[c % 2][:, 0:w], ct(GB_T[c], cs, cs + w), ALU.mult, [PSb[c % 2]] + cb(GB_T[c], j), cb(GB_T[c], j))
        outproj_residual(i, GB_T + PO_T, [5, 3], ctx_full, 2)

    AFF_T = 0
    AFW_T = 2
    XE_T = [4, 5]
    ACT_T = 6

    def ctf(i, a, b, p=16):
        base = (i * CTW) // 2
        return CTF[0:p, base + a: base + b]

    def ctfb(i):
        return [b for t in (i, i + 1) for j in range(9) for b in cb(t, j)]

    TVb, TIb, TFb = [WTb[0], WTb[1]], [WTb[2], WTb[3]], [WTb[4], WTb[5]]

    def moe(i, last_layer):
        ctx_full = i < 2
        ntile = 5 if ctx_full else 4
        LD(SP, WR[:, :].rearrange("p (c n) -> p c n", c=8), w_router[i].rearrange("(c p) n -> p c n", p=128), [], [WRb])
        wr3 = WR[:, :].rearrange("p (c n) -> p c n", c=8)
        affb, afwb = ctfb(AFF_T), ctfb(AFW_T)
        for j in blocks_for(ctx_full):
            cs, xc, w = BLKS[j]
            v = 1 if j == 8 else 0
            hb_i = hbrr[0] % 2
            hbrr[0] += 1
            hb3 = HB[hb_i].rearrange("p (c n) -> p c n", c=8)
            hbb = HBb[hb_i]
            for c in range(8):
                k = next_xs()
                LD(SP, wt(k, w), XA[c * 128:(c + 1) * 128, xc:xc + w], [XAb[c][j]], [WTb[k]])
                TT(wt(k, w), wt(k, w), RSTD[:, xc:xc + w], ALU.mult, [WTb[k], RSTDb[j]], [WTb[k]])
                A(wt(k, w), wt(k, w), AF.Identity, [WTb[k], DERb, MODSb], [WTb[k]], scale=G2(c, v), bias=mod(i, 3, c, v))
                P.op(PE, lambda e, c=c, k=k, w=w: e.matmul(PS[0][0:16, 0:w], lhsT=wr3[:, c, :], rhs=wt(k, w), start=(c == 0), stop=(c == 7)),
                     [WRb, WTb[k]], [PSb[0]])
                CP(hb3[:, c, 0:w], wt(k, w), [WTb[k]] + hbb, hbb, eng=POOL)
            A(wt(5, w)[0:16, :], PS[0][0:16, 0:w], AF.Exp, [PSb[0]], [WTb[5]])
            MM(PS[1][0:16, 0:w], [(ONEF[:, :], wt(5, w)[0:16, :])], [ONEFb, WTb[5]], [PSb[1]])
            RCP(wt(6, w)[0:16, :], PS[1][0:16, 0:w], [PSb[1]], [WTb[6]])
            TT(ctf(AFF_T, xc, xc + w), wt(5, w)[0:16, :], wt(6, w)[0:16, :], ALU.mult, [WTb[5], WTb[6]], affb)
            for tt in range(w // 128):
                pb = 2 + (tt % 2)
                TRS([(PSH[pb][:, c * 128:(c + 1) * 128], hb3[:, c, tt * 128:(tt + 1) * 128], cm(IDB)) for c in range(8)],
                    [CMb] + hbb, [PSb[pb]])
                gk = 3 + (tt % 2)
                if tt % 2 == 0:
                    CP(wth(gk), PSH[pb][:, :], [PSb[pb]], [WTb[gk]])
                else:
                    A(wth(gk), PSH[pb][:, :], AF.Copy, [PSb[pb]], [WTb[gk]])
                trow = xc + tt * 128
                LD(SP, HFTOK[trow:trow + 128, :], wth(gk), [WTb[gk]], [HFb[trow // 128]])
        chk("moe_prep")
        if i + 1 < n_layers:
            mods_mm(i + 1)
        TV = WT[0:16, 0:NSLOT]
        TI = WTU[0:16, 1024:1024 + NSLOT]
        TF = WT[0:16, 2048:2048 + NSLOT]
        for (col0, ncol, nround, s0) in ([(0, NL, CAP_L // 8, 0)] + ([(NL, NCX, CAP_C // 8, CAP_L)] if ctx_full else [])):
            for r in range(nround):
                src = ctf(AFF_T, col0, col0 + ncol) if r == 0 else ctf(AFW_T, col0, col0 + ncol)
                srcb = affb if r == 0 else afwb
                sl = slice(s0 + 8 * r, s0 + 8 * r + 8)
                P.op(DVE, lambda e, sl=sl, src=src: e.max(out=TV[:, sl], in_=src), srcb, TVb)
                P.op(DVE, lambda e, sl=sl, src=src: e.max_index(out=TI[:, sl], in_max=TV[:, sl], in_values=src), srcb + TVb, TIb)
                if r < nround - 1:
                    P.op(DVE, lambda e, sl=sl, src=src, col0=col0, ncol=ncol: e.match_replace(out=ctf(AFW_T, col0, col0 + ncol), in_to_replace=TV[:, sl],
                                                                                              in_values=src, imm_value=-1.0), srcb + TVb, afwb)
        CP(TF, TI, TIb, TFb)
        if ctx_full:
            TS(TF[:, CAP_L:NSLOT], TF[:, CAP_L:NSLOT], float(NL), None, ALU.add, None, TFb, TFb)
        for t in range(ntile):
            n = 128 if t < 4 else CAP_C
            TR(PS[4][0:n, 0:16], TV[:, t * 128: t * 128 + n], IDF[0:16, 0:16], TVb + [IDFb], [PSb[4]])
            CP(AFFT[0:n, t * 16:(t + 1) * 16], PS[4][0:n, 0:16], [PSb[4]], [AFFTb])
            TR(PS[5][0:n, 0:16], TF[:, t * 128: t * 128 + n], IDF[0:16, 0:16], TFb + [IDFb], [PSb[5]])
            CP(IDXT[0:n, t * 16:(t + 1) * 16], PS[5][0:n, 0:16], [PSb[5]], [IDXTb])
        if i + 1 < n_layers:
            mods_fin(i + 1)
        chk("moe_topk")
        xe3 = [CT[:, XE_T[k] * CTW: XE_T[k] * CTW + 8 * NSLOT].rearrange("p (c s) -> p c s", c=8) for k in range(2)]
        xeb = [[b for j in range(9) for b in cb(XE_T[k], j)] for k in range(2)]
        act3 = CT[:, ACT_T * CTW: ACT_T * CTW + 16 * NSLOT].rearrange("p (f s) -> p f s", f=16)
        actb = [b for t in (ACT_T, ACT_T + 1) for j in range(9) for b in cb(t, j)]
        GTK = [9, 10, 11, 0, 1]

        pieces = []
        for e in range(NEXP):
            for q in range(4):
                pieces.append(("g", e, q))
                pieces.append(("u", e, q))
            for dq in range(4):
                pieces.append(("d", e, dq))
        slot_of = {}
        loaded = [0]

        def ensure_loaded(upto):
            while loaded[0] <= min(upto, len(pieces) - 1):
                kind, e, q = pieces[loaded[0]]
                s = pcount[0] % NRING
                pcount[0] += 1
                slot_of[loaded[0]] = s
                if kind == "g":
                    LD(POOL, ring3(s, 8), w3(w_gate[mmap[i], e])[:, :, q * 512:(q + 1) * 512], [], [RINGb[s]])
                elif kind == "u":
                    LD(POOL, ring3(s, 8), w3(w_up[mmap[i], e])[:, :, q * 512:(q + 1) * 512], [], [RINGb[s]])
                else:
                    LD(POOL, ring3(s, 16), w_down[mmap[i], e].rearrange("(f p) d -> p f d", p=128)[:, :, q * 256:(q + 1) * 256], [], [RINGb[s]])
                loaded[0] += 1

        def gather(e):
            for t in range(ntile):
                n = 128 if t < 4 else CAP_C
                gk = GTK[t]
                P.dma(POOL, lambda eng, gk=gk, n=n, t=t, e=e: eng.indirect_dma_start(
                    out=WTH[0:n, gk * 1024:(gk + 1) * 1024], out_offset=None, in_=HFTOK[:, :],
                    in_offset=bass.IndirectOffsetOnAxis(ap=IDXT[0:n, t * 16 + e: t * 16 + e + 1], axis=0)),
                    [IDXTb] + HFb, [WTb[gk]])

        def xe_transposes(e):
            k = e % 2
            for t in range(ntile):
                n = 128 if t < 4 else CAP_C
                gk = GTK[t]
                tb = 6 if (ctx_full or t % 2 == 0) else 4
                TRS([(PSH[tb][:, c * 128: c * 128 + n], WTH[0:n, gk * 1024 + c * 128: gk * 1024 + (c + 1) * 128], CM[0:n, 0:n]) for c in range(8)],
                    [CMb, WTb[gk]], [PSb[tb]])
                src = PSH[tb][:, :].rearrange("p (c s) -> p c s", c=8)[:, :, 0:n]
                if t % 2 == 0:
                    CP(xe3[k][:, :, t * 128: t * 128 + n], src, [PSb[tb]], xeb[k])
                else:
                    P.op(ACT, lambda eng, k=k, t=t, n=n, src=src: eng.activation(out=xe3[k][:, :, t * 128: t * 128 + n], in_=src, func=AF.Copy),
                         [PSb[tb]], xeb[k])

        PYb = [PSb[5], PSb[7]]
        YSb = [P.buf("ys%d_%d" % (i, k)) for k in range(16)]
        gather(0)
        ensure_loaded(4)
        xe_transposes(0)
        pidx = 0
        ysr = [0]
        for e in range(NEXP):
            k = e % 2
            if e + 1 < NEXP:
                gather(e + 1)
            for q in range(4):
                ensure_loaded(pidx + 5)
                sg_, su_ = slot_of[pidx], slot_of[pidx + 1]
                pidx += 2
                for fl in range(4):
                    fc = q * 4 + fl
                    pg, pu = (fc % 2), 2 + (fc % 2)
                    MM(PS[pg][:, 0:512], [(ring3(sg_, 8)[:, c, fl * 128:(fl + 1) * 128], xe3[k][:, c, 0:512]) for c in range(8)],
                       [RINGb[sg_]] + xeb[k], [PSb[pg]])
                    MM(PS[pu][:, 0:512], [(ring3(su_, 8)[:, c, fl * 128:(fl + 1) * 128], xe3[k][:, c, 0:512]) for c in range(8)],
                       [RINGb[su_]] + xeb[k], [PSb[pu]])
                    if ctx_full:
                        co = (fc % 8) * 64
                        MM(PS[4][:, co:co + 32], [(ring3(sg_, 8)[:, c, fl * 128:(fl + 1) * 128], xe3[k][:, c, 512:544]) for c in range(8)],
                           [RINGb[sg_]] + xeb[k], [PSb[4]])
                        MM(PS[4][:, co + 32:co + 64], [(ring3(su_, 8)[:, c, fl * 128:(fl + 1) * 128], xe3[k][:, c, 512:544]) for c in range(8)],
                           [RINGb[su_]] + xeb[k], [PSb[4]])
                    sk = 5 + (fc % 2)
                    A(wth(sk, 0, 512), PS[pg][:, 0:512], AF.Silu, [PSb[pg]], [WTb[sk]])
                    TT(act3[:, fc, 0:512], wth(sk, 0, 512), PS[pu][:, 0:512], ALU.mult, [WTb[sk], PSb[pu]], actb)
                    if ctx_full and fc % 8 == 7:
                        p4 = PS[4][:, :].rearrange("p (f g s) -> p f g s", f=8, g=2)
                        s3 = wth(7, 0, 256).rearrange("p (f s) -> p f s", f=8)
                        A(s3, p4[:, :, 0, :], AF.Silu, [PSb[4]], [WTb[7]])
                        TT(act3[:, fc - 7: fc + 1, 512:544], s3, p4[:, :, 1, :], ALU.mult, [WTb[7], PSb[4]], actb)
            if e + 1 < NEXP:
                xe_transposes(e + 1)
            for dq in range(4):
                ensure_loaded(pidx + 5)
                sd_ = slot_of[pidx]
                pidx += 1
                d3 = ring3(sd_, 16)
                ys_list = []
                for t in range(ntile):
                    n = 128 if t < 4 else CAP_C
                    yb = ysr[0] % 2
                    ysr[0] += 1
                    pyb, pyo = 5 + yb * 2, 0
                    py = PS[pyb][0:n, pyo:pyo + 256]
                    MM(py, [(act3[:, fc, t * 128: t * 128 + n], d3[:, fc, :]) for fc in range(16)], [RINGb[sd_]] + actb, [PYb[yb]])
                    yk = (ysr[0] - 1) % 16
                    ybase = ((8 + yk // 8) * CTW) // 2 + (yk % 8) * 256
                    ysl = CTF[0:n, ybase: ybase + 256]
                    ysb = YSb[yk]
                    if yb == 0:
                        A(ysl, py, AF.Identity, [PYb[yb], AFFTb], [ysb], scale=AFFT[0:n, t * 16 + e: t * 16 + e + 1])
                    else:
                        TS(ysl, py, AFFT[0:n, t * 16 + e: t * 16 + e + 1], None, ALU.mult, None, [PYb[yb], AFFTb], [ysb])
                    ys_list.append((ysl, ysb, n, t))
                fns = []
                for (ysl, ysb, n, t) in ys_list:
                    fns.append(lambda eng, ysl=ysl, n=n, t=t, e=e, dq=dq: eng.indirect_dma_start(
                        out=MOQ[dq][:, :], out_offset=bass.IndirectOffsetOnAxis(ap=IDXT[0:n, t * 16 + e: t * 16 + e + 1], axis=0),
                        in_=ysl, in_offset=None, compute_op=ALU.add))
                P.dma(POOL, fns, [IDXTb, MOb[dq]] + [ysb for (_, ysb, _, _) in ys_list], [MOb[dq]])
        chk("moe_exp")
        for j in blocks_for(ctx_full):
            cs, xc, w = BLKS[j]
            v = 1 if j == 8 else 0
            pend = None
            for half in range(2):
                for tt in range(w // 128):
                    gk = 9 + (tt % 2)
                    trow = xc + tt * 128
                    P.dma(SP, [lambda e_, gk=gk, trow=trow, q=q: e_.dma_start(out=wt(gk, 512)[:, (q % 2) * 256:(q % 2 + 1) * 256], in_=MOQ[q][trow:trow + 128, :])
                               for q in (2 * half, 2 * half + 1)], [MOb[2 * half], MOb[2 * half + 1]], [WTb[gk]])
                    for cl in range(4):
                        TR(PS[cl][:, tt * 128:(tt + 1) * 128], wt(gk, 512)[:, cl * 128:(cl + 1) * 128], IDF[:, :], [WTb[gk], IDFb], [PSb[cl]])
                for cl in range(4):
                    c = half * 4 + cl
                    kx = next_xs()
                    LD(SP, wt(kx, w), XA[c * 128:(c + 1) * 128, xc:xc + w], [XAb[c][j]], [WTb[kx]])
                    STT(wt(kx, w), PS[cl][:, 0:w], mod(i, 5, c, v), wt(kx, w), ALU.mult, ALU.add, [PSb[cl], WTb[kx], MODSb], [WTb[kx]])
                    if last_layer:
                        LD(SP, outT[c * 128:(c + 1) * 128, xc:xc + w], wt(kx, w), [WTb[kx]], [OUTb])
                    else:
                        LD(SP, XA[c * 128:(c + 1) * 128, xc:xc + w], wt(kx, w), [WTb[kx]], [XAb[c][j]])
                        kk = stat_chunk(j, c, wt(kx, w), WTb[kx], w)
                        if pend is not None:
                            stat_mm(j, *pend, w)
                        pend = (c, kk)
            if not last_layer:
                stat_mm(j, *pend, w)
                stat_fin(j, w)
        if not last_layer:
            zero_mo(True)

    try:
        chk("pre")
        for i in range(start_layer, n_layers):
            der_setup(i)
            if i % 2 == 0:
                even_mixer(i)
            else:
                odd_mixer(i)
            if stop == "mix%d" % i:
                break
            moe(i, i == DEPTH - 1)
    except _Stop:
        pass
    if debug:
        for c in range(8):
            LD(SP, dbg["xa"][c * 128:(c + 1) * 128, :], XA[c * 128:(c + 1) * 128, :], [b for b in XAb[c]], [OUTb])
    P.finish()
    return nc, P


def _rope_tables():
    n_freq = 16
    inv = (10000.0 ** (-np.arange(n_freq, dtype=np.float32) / n_freq)).astype(np.float32)
    t = np.arange(NL)
    row = (t // 64).astype(np.float32)
    col = (t % 64).astype(np.float32)
    ang_r = row[:, None] * inv
    ang_c = col[:, None] * inv
    cos = np.zeros((64, NL), np.float32)
    sin = np.zeros((64, NL), np.float32)
    cos[0:16] = np.cos(ang_r).T
    cos[16:32] = np.cos(ang_r).T
    cos[32:48] = np.cos(ang_c).T
    cos[48:64] = np.cos(ang_c).T
    sin[0:16] = np.sin(ang_r).T
    sin[16:32] = np.sin(ang_r).T
    sin[32:48] = np.sin(ang_c).T
    sin[48:64] = np.sin(ang_c).T
    cos = np.concatenate([cos, cos], 0)
    sin = np.concatenate([sin, sin], 0)
    return np.concatenate([cos, sin], 1).astype(np.float32)


def _const_mats():
    m = np.zeros((128, 5 * 128), np.float32)
    m[:, 0:128] = np.eye(128)
    m[:, 128:256] = 1.0
    blk = np.zeros((128, 128), np.float32)
    blk[0:64, 0:64] = 1.0
    blk[64:128, 64:128] = 1.0
    m[:, 256:384] = blk
    rot = np.zeros((128, 128), np.float32)
    for base in range(0, 128, 32):
        for d in range(16):
            rot[base + 16 + d, base + d] = -1.0
            rot[base + d, base + 16 + d] = 1.0
    m[:, 384:512] = rot
    return m


def _pool_fix():
    f = np.ones((128, 64), np.float32)
    for gi, w in enumerate((2, 4, 8, 16)):
        h = w // 2
        for t in range(h):
            f[:, gi * 16 + t] = w / float(t + h)
        for m_ in range(h - 1):
            f[:, gi * 16 + 8 + m_] = w / float((h - 1 - m_) + h)
    return f


def _pack_vecs(inp, b):
    v = np.zeros((128, NV), np.float32)

    def col(a):
        a = np.asarray(a, np.float32)
        return a.reshape(-1, 128).T

    for i in range(DEPTH):
        v[:, V_NMG + i * 8: V_NMG + (i + 1) * 8] = col(inp["norm_mix_g"][i])
        v[:, V_NFG + i * 8: V_NFG + (i + 1) * 8] = col(inp["norm_ffn_g"][i])
        v[:, V_BMOD + i * 48: V_BMOD + (i + 1) * 48] = col(inp["b_mod"][i])
    for j in range(2):
        o = V_EV + j * 138
        v[:, o: o + 4] = col(inp["ev_conv_b"][j])
        v[:, o + 4: o + 8] = col(inp["ev_ln_g"][j])
        v[:, o + 8: o + 12] = col(inp["ev_ln_b"][j])
        v[:, o + 12] = np.tile(np.asarray(inp["ev_q_norm_g"][j], np.float32), 2)
        v[:, o + 13] = np.tile(np.asarray(inp["ev_k_norm_g"][j], np.float32), 2)
        cw = np.asarray(inp["ev_conv_w"][j], np.float32)
        for c in range(4):
            v[:, o + 14 + c * 31: o + 14 + (c + 1) * 31] = cw[:, c * 128:(c + 1) * 128].T
        o2 = V_OD + j * 16
        ow = np.asarray(inp["od_conv_w"][j], np.float32)
        for c in range(4):
            v[:, o2 + c * 3: o2 + (c + 1) * 3] = ow[:, c * 128:(c + 1) * 128].T
        v[:, o2 + 12: o2 + 16] = col(inp["od_pool_scale"][j])
    cc = np.stack([col(inp["c"][b]), col(inp["c_ctx"])], axis=-1)
    v[:, V_CC: V_CC + 16] = cc.reshape(128, 16)
    return v


_CACHE = {}


def kernel(**inputs):
    inp = {k: np.asarray(v) for k, v in inputs.items()}
    n = 8
    if "nc" not in _CACHE:
        _CACHE["nc"] = build_program()[0]
    nc = _CACHE["nc"]
    rope = _rope_tables()
    cm = _const_mats()
    pf = _pool_fix()
    zeros = np.zeros((NT, D), np.float32)
    shared = {k: np.ascontiguousarray(inp[k], dtype=np.float32) for k in
              ("w_mod", "ev_w_in", "ev_w_out", "od_w_in", "od_w_out", "od_pool_w", "w_router", "w_gate", "w_up", "w_down")}
    in_maps = []
    for b in range(n):
        m = dict(shared)
        m["xT"] = np.ascontiguousarray(inp["x"][b].T, dtype=np.float32)
        m["ctxT"] = np.ascontiguousarray(inp["ctx"][b].T, dtype=np.float32)
        m["vecs"] = _pack_vecs(inp, b)
        m["cmats"] = cm
        m["rope_cs"] = rope
        m["pool_fix"] = pf
        m["zeros_d"] = zeros
        in_maps.append(m)
    res = run_bass_kernel_spmd(nc, in_maps, core_ids=list(range(n)))
    out = np.stack([np.ascontiguousarray(res.results[b]["outT"].T) for b in range(n)], axis=0)
    return out.astype(np.float32)
```
